# Optimizing a Trainium2 kernel written in Bass

```python
import math
import jax, jax.numpy as jnp
from jax import lax
import numpy as np

D_MODEL = 1024
BATCH = 8
SEQ = 4096
DEPTH = 1

CONV_CH = 512
CONV_WIDTH = 31
ATTN_HEADS = 8
HEAD_DIM = 64
ATTN_W = ATTN_HEADS * HEAD_DIM
MOBA_BLOCK = 256
MOBA_TOPK = 3
Q_CHUNK = 128
PEER_HEADS = 8
PEER_NKEYS = 128
PEER_EXPERTS = PEER_NKEYS * PEER_NKEYS
PEER_QDIM = 256
PEER_TOPK = 16
PEER_TOK_CHUNK = 128
NORM_EPS = 1e-6
IN_SPLITS = (2 * CONV_CH, 2 * CONV_CH + ATTN_W, 2 * CONV_CH + 2 * ATTN_W,
             2 * CONV_CH + 3 * ATTN_W, 2 * CONV_CH + 3 * ATTN_W + D_MODEL)
IN_COLS = 2 * CONV_CH + 3 * ATTN_W + 2 * D_MODEL

kernel_name = 'hybrid_conformer_moba_peer_block'


def rms_norm(x, g):
    xf = x.astype(jnp.float32)
    y = xf * lax.rsqrt(jnp.mean(xf * xf, axis=-1, keepdims=True) + NORM_EPS)
    return (y * g.astype(jnp.float32)).astype(x.dtype)


def layer_norm(x, g, b):
    xf = x.astype(jnp.float32)
    mu = jnp.mean(xf, axis=-1, keepdims=True)
    xc = xf - mu
    var = jnp.mean(xc * xc, axis=-1, keepdims=True)
    y = xc * lax.rsqrt(var + NORM_EPS) * g.astype(jnp.float32) + b.astype(jnp.float32)
    return y.astype(x.dtype)


def alibi_slopes(n_heads):
    return jnp.asarray([2.0 ** (-8.0 * (i + 1) / n_heads) for i in range(n_heads)], dtype=jnp.float32)


def conformer_conv(u, conv_w, conv_b, ln_g, ln_b, w_pw, b_pw):
    a, gte = jnp.split(u, 2, axis=-1)
    z = a * jax.nn.sigmoid(gte)
    z = jnp.pad(z, ((0, 0), (CONV_WIDTH - 1, 0), (0, 0)))
    z = lax.conv_general_dilated(z, conv_w[:, None, :].astype(z.dtype), window_strides=(1,),
                                 padding='VALID', dimension_numbers=('NWC', 'WIO', 'NWC'),
                                 feature_group_count=CONV_CH) + conv_b
    z = jax.nn.silu(layer_norm(z, ln_g, ln_b))
    return z @ w_pw + b_pw


def moba_attention(q, k, v):
    B, H, S, dh = q.shape
    nb = -(-S // MOBA_BLOCK)
    s_pad = nb * MOBA_BLOCK
    pad = ((0, 0), (0, 0), (0, s_pad - S), (0, 0))
    kb = jnp.pad(k, pad).reshape(B, H, nb, MOBA_BLOCK, dh)
    vb = jnp.pad(v, pad).reshape(B, H, nb, MOBA_BLOCK, dh)
    kmean = jnp.mean(kb.astype(jnp.float32), axis=3)
    n_sel = min(MOBA_TOPK, nb)
    nc = S // Q_CHUNK
    slopes = alibi_slopes(H)
    scale = dh ** -0.5
    offs = jnp.arange(MOBA_BLOCK)
    qloc = jnp.arange(Q_CHUNK)
    hidx = jnp.arange(H)[:, None, None]

    def chunk(n):
        b = n // nc
        c = n % nc
        t0 = c * Q_CHUNK
        blk = t0 // MOBA_BLOCK
        qc = lax.dynamic_slice_in_dim(lax.dynamic_index_in_dim(q, b, 0, keepdims=False), t0, Q_CHUNK, axis=1)
        qf = qc.astype(jnp.float32) * scale
        kb_b = lax.dynamic_index_in_dim(kb, b, 0, keepdims=False)
        vb_b = lax.dynamic_index_in_dim(vb, b, 0, keepdims=False)
        km_b = lax.dynamic_index_in_dim(kmean, b, 0, keepdims=False)
        tpos = t0 + qloc
        bscore = jnp.einsum('hqd,hnd->hqn', qf, km_b)
        bscore = jnp.where(jnp.arange(nb) < blk, bscore, -jnp.inf)
        _, sel = lax.top_k(bscore, n_sel)
        sel_ok = sel < blk
        ksel = kb_b[hidx, sel]
        vsel = vb_b[hidx, sel]
        spos = sel[..., None] * MOBA_BLOCK + offs
        dist = (tpos[None, :, None, None] - spos).astype(jnp.float32)
        lsel = jnp.einsum('hqd,hqnkd->hqnk', qf, ksel.astype(jnp.float32)) - slopes[:, None, None, None] * dist
        lsel = jnp.where(sel_ok[..., None], lsel, -jnp.inf).reshape(H, Q_CHUNK, n_sel * MOBA_BLOCK)
        kown = lax.dynamic_index_in_dim(kb_b, blk, 1, keepdims=False)
        vown = lax.dynamic_index_in_dim(vb_b, blk, 1, keepdims=False)
        opos = blk * MOBA_BLOCK + offs
        odist = (tpos[:, None] - opos[None, :]).astype(jnp.float32)
        lown = jnp.einsum('hqd,hkd->hqk', qf, kown.astype(jnp.float32)) - slopes[:, None, None] * odist
        lown = jnp.where((opos[None, :] <= tpos[:, None])[None], lown, -jnp.inf)
        p = jax.nn.softmax(jnp.concatenate([lsel, lown], axis=-1), axis=-1)
        p_sel = p[..., :n_sel * MOBA_BLOCK]
        p_own = p[..., n_sel * MOBA_BLOCK:]
        out = (jnp.einsum('hqk,hqkd->hqd', p_sel, vsel.reshape(H, Q_CHUNK, n_sel * MOBA_BLOCK, dh).astype(jnp.float32))
               + jnp.einsum('hqk,hkd->hqd', p_own, vown.astype(jnp.float32)))
        return out.astype(q.dtype)

    o = lax.map(chunk, jnp.arange(B * nc))
    o = o.reshape(B, nc, H, Q_CHUNK, dh).transpose(0, 1, 3, 2, 4)
    return o.reshape(B, S, H * dh)


def peer_ffn(h, w_q, keys1, keys2, u_tab, v_tab):
    B, S, D = h.shape
    T = B * S
    ht = h.reshape(T, D)
    q = (ht @ w_q).reshape(T, PEER_HEADS, 2, PEER_QDIM // 2).astype(jnp.float32)
    s1 = jnp.einsum('thc,nc->thn', q[:, :, 0], keys1.astype(jnp.float32))
    s2 = jnp.einsum('thc,nc->thn', q[:, :, 1], keys2.astype(jnp.float32))
    v1, i1 = lax.top_k(s1, PEER_TOPK)
    v2, i2 = lax.top_k(s2, PEER_TOPK)
    cand = (v1[..., :, None] + v2[..., None, :]).reshape(T, PEER_HEADS, PEER_TOPK * PEER_TOPK)
    cidx = (i1[..., :, None] * PEER_NKEYS + i2[..., None, :]).reshape(T, PEER_HEADS, PEER_TOPK * PEER_TOPK)
    sc, pos = lax.top_k(cand, PEER_TOPK)
    eidx = jnp.take_along_axis(cidx, pos, axis=-1)
    g = jax.nn.softmax(sc, axis=-1)
    n_ch = T // PEER_TOK_CHUNK
    E = PEER_HEADS * PEER_TOPK
    xs = (ht.reshape(n_ch, PEER_TOK_CHUNK, D),
          eidx.reshape(n_ch, PEER_TOK_CHUNK, E),
          g.reshape(n_ch, PEER_TOK_CHUNK, E))

    def body(args):
        xc, ec, gc = args
        uc = u_tab[ec]
        a = jax.nn.gelu(jnp.einsum('td,ted->te', xc, uc, preferred_element_type=jnp.float32), approximate=False)
        vc = v_tab[ec]
        y = jnp.einsum('te,ted->td', (gc * a).astype(vc.dtype), vc, preferred_element_type=jnp.float32)
        return y.astype(h.dtype)

    return lax.map(body, xs).reshape(B, S, D)


def setup_inputs(seed: int = 0) -> dict:
    key = jax.random.key(seed)
    ks = jax.random.split(key, 20)
    f32 = jnp.float32
    L = DEPTH
    nrm = lambda k, shape, s: jax.random.normal(k, shape, f32) * s
    return {
        'x': jax.random.normal(ks[0], (BATCH, SEQ, D_MODEL), f32),
        'g_norm1': 1.0 + nrm(ks[1], (L, D_MODEL), 0.02),
        'w_in': nrm(ks[2], (L, D_MODEL, IN_COLS), D_MODEL ** -0.5),
        'conv_w': nrm(ks[3], (L, CONV_WIDTH, CONV_CH), CONV_WIDTH ** -0.5),
        'conv_b': nrm(ks[4], (L, CONV_CH), 0.02),
        'conv_ln_g': 1.0 + nrm(ks[5], (L, CONV_CH), 0.02),
        'conv_ln_b': nrm(ks[6], (L, CONV_CH), 0.02),
        'w_conv_pw': nrm(ks[7], (L, CONV_CH, D_MODEL), CONV_CH ** -0.5),
        'b_conv_pw': nrm(ks[8], (L, D_MODEL), 0.02),
        'w_attn_out': nrm(ks[9], (L, ATTN_W, D_MODEL), ATTN_W ** -0.5),
        'w_out': nrm(ks[10], (L, D_MODEL, D_MODEL), D_MODEL ** -0.5),
        'g_norm2': 1.0 + nrm(ks[11], (L, D_MODEL), 0.02),
        'w_peer_q': nrm(ks[12], (L, D_MODEL, PEER_HEADS * PEER_QDIM), D_MODEL ** -0.5),
        'peer_keys1': nrm(ks[13], (L, PEER_NKEYS, PEER_QDIM // 2), (PEER_QDIM // 2) ** -0.5),
        'peer_keys2': nrm(ks[14], (L, PEER_NKEYS, PEER_QDIM // 2), (PEER_QDIM // 2) ** -0.5),
        'peer_u': nrm(ks[15], (L, PEER_EXPERTS, D_MODEL), D_MODEL ** -0.5),
        'peer_v': nrm(ks[16], (L, PEER_EXPERTS, D_MODEL), 0.25),
        'g_final': 1.0 + nrm(ks[17], (D_MODEL,), 0.02),
    }


def reference(x, g_norm1, w_in, conv_w, conv_b, conv_ln_g, conv_ln_b, w_conv_pw, b_conv_pw,
              w_attn_out, w_out, g_norm2, w_peer_q, peer_keys1, peer_keys2, peer_u, peer_v, g_final):
    B, S, D = x.shape
    for l in range(DEPTH):
        h = rms_norm(x, g_norm1[l])
        z = h @ w_in[l]
        u_conv, q, k, v, gate_c, gate_a = jnp.split(z, IN_SPLITS, axis=-1)
        y_conv = conformer_conv(u_conv, conv_w[l], conv_b[l], conv_ln_g[l], conv_ln_b[l], w_conv_pw[l], b_conv_pw[l])
        to_heads = lambda t: t.reshape(B, S, ATTN_HEADS, HEAD_DIM).transpose(0, 2, 1, 3)
        y_attn = moba_attention(to_heads(q), to_heads(k), to_heads(v)) @ w_attn_out[l]
        mix = jax.nn.sigmoid(gate_c) * y_conv + jax.nn.sigmoid(gate_a) * y_attn
        x = x + mix @ w_out[l]
        x = x + peer_ffn(rms_norm(x, g_norm2[l]), w_peer_q[l], peer_keys1[l], peer_keys2[l], peer_u[l], peer_v[l])
    return rms_norm(x, g_final)
```

```python
import numpy as np
import ml_dtypes
from contextlib import ExitStack
import concourse.bass as bass
import concourse.mybir as mybir
from concourse.bass_utils import run_bass_kernel_spmd

F32 = mybir.dt.float32
BF16 = mybir.dt.bfloat16
U32 = mybir.dt.uint32
I32 = mybir.dt.int32
ALU = mybir.AluOpType
AF = mybir.ActivationFunctionType
AX = mybir.AxisListType

D = 1024
NCOL = 4608
NH = 8
DH = 64
CC = 512
CW = 31
MB = 256
PH = 8
NK = 128
TOPK = 16
EPS = 1e-6
BIG = 30000.0
NEG = -1.0e30


class Prog:
    ENG = ("pe", "dve", "act", "pool", "sp")
    DQ = ("sp", "pool", "act")

    def __init__(self, nc, stack, kdma=8):
        self.nc = nc
        self.eng = {"pe": nc.tensor, "dve": nc.vector, "act": nc.scalar, "pool": nc.gpsimd, "sp": nc.sync}
        self.sems = {}
        for e in self.ENG:
            self.sems[("c", e)] = stack.enter_context(nc.semaphore(f"c_{e}"))
        self.kdma = {"sp": kdma, "pool": 16, "act": 4}
        for q in self.DQ:
            for j in range(self.kdma[q]):
                self.sems[("d", q, j)] = stack.enter_context(nc.semaphore(f"d_{q}{j}"))
        self.latest = {k: 0 for k in self.sems}
        self.dnext = {q: 0 for q in self.DQ}
        self.ops = {e: [] for e in self.ENG}
        self.known = {e: {} for e in self.ENG}
        self.lastw = {}
        self.readers = {}
        self.nins = 0

    def _need(self, eng, key, val):
        if self.known[eng].get(key, 0) < val:
            self.known[eng][key] = val
            self.ops[eng].append(("w", key, val))

    def _deps(self, eng, reads, writes, is_dma):
        for b in reads:
            lw = self.lastw.get(b)
            if lw is not None:
                key, val = lw
                if (not is_dma) and key == ("c", eng) and eng == "pe":
                    continue
                self._need(eng, key, val)
        for b in writes:
            lw = self.lastw.get(b)
            if lw is not None:
                key, val = lw
                if is_dma or key != ("c", eng):
                    self._need(eng, key, val)
            for key, val in self.readers.get(b, {}).items():
                if is_dma or key != ("c", eng):
                    self._need(eng, key, val)

    def _commit(self, key, val, reads, writes):
        for b in writes:
            self.lastw[b] = (key, val)
            self.readers[b] = {}
        for b in reads:
            r = self.readers.setdefault(b, {})
            r[key] = max(r.get(key, 0), val)

    def op(self, eng, fn, reads=(), writes=()):
        self._deps(eng, reads, writes, False)
        key = ("c", eng)
        self.latest[key] += 1
        val = self.latest[key]
        self.ops[eng].append(("i", fn, key, 1))
        self._commit(key, val, reads, writes)
        self.nins += 1

    def dma(self, q, fn, reads=(), writes=(), reuse_wait=True):
        self._deps(q, reads, writes, True)
        j = self.dnext[q]
        self.dnext[q] = (j + 1) % self.kdma[q]
        key = ("d", q, j)
        if self.latest[key] > 0 and reuse_wait:
            self._need(q, key, self.latest[key])
        self.latest[key] += 16
        val = self.latest[key]
        self.ops[q].append(("i", fn, key, 16))
        self._commit(key, val, reads, writes)
        self.nins += 1

    def barrier(self):
        for e in self.ENG:
            for key, val in self.latest.items():
                if val > 0:
                    self._need(e, key, val)
        self.lastw = {}
        self.readers = {}

    def emit(self, name=None):
        ops = self.ops
        self.ops = {e: [] for e in self.ENG}
        sems = self.sems
        nc = self.nc

        def replay(lst):
            def f(e):
                for it in lst:
                    if it[0] == "w":
                        e.wait_ge(sems[it[1]], it[2])
                    else:
                        ins = it[1](e)
                        ins.then_inc(sems[it[2]], it[3])
            return f

        with nc.Block() as block:
            block.tensor(replay(ops["pe"]))
            block.vector(replay(ops["dve"]))
            block.scalar(replay(ops["act"]))
            block.gpsimd(replay(ops["pool"]))
            block.sync(replay(ops["sp"]))


class Rot:
    def __init__(self, n):
        self.n = n
        self.i = -1

    def next(self):
        self.i = (self.i + 1) % self.n
        return self.i


def _bf(a):
    return np.ascontiguousarray(a).astype(ml_dtypes.bfloat16)


def make_consts(S):
    NT = S // 128
    NB = S // MB
    c = {}
    c["ident_bf"] = _bf(np.eye(128, dtype=np.float32))
    c["ident_f"] = np.eye(128, dtype=np.float32)
    ka = np.zeros((17, S), np.float32)
    ka[0, :] = 1.0
    for n in range(NB):
        ka[1 + n, n * MB:(n + 1) * MB] = 1.0
    c["kaug_static"] = _bf(ka)
    mt = np.zeros((4, 128, 512), np.float32)
    for i in range(4):
        for j in range(4):
            if i // 2 != j // 2:
                continue
            blk = mt[i, :, j * 128:(j + 1) * 128]
            if i > j:
                blk[:] = -BIG
            elif i == j:
                kk = np.arange(128)[:, None]
                qq = np.arange(128)[None, :]
                blk[kk > qq] = -BIG
    c["masktri"] = _bf(mt)
    tile = np.arange(NT)[:, None]
    n = np.arange(16)[None, :]
    valid = (n < tile // 2).astype(np.float32)
    own = (n == tile // 2).astype(np.float32)
    c["valid01"] = np.broadcast_to(valid[None], (128, NT, 16)).astype(np.float32).copy()
    c["own01"] = np.broadcast_to(own[None], (128, NT, 16)).astype(np.float32).copy()
    c["maskv"] = ((c["valid01"] - 1.0) * 1.0e30).astype(np.float32)
    slopes = np.array([2.0 ** (-8.0 * (i + 1) / NH) for i in range(NH)], np.float32)
    stat = np.zeros((NH, 128, NT, 16), np.float32)
    for h in range(NH):
        st = -slopes[h] * (128.0 * tile - 256.0 * n)
        st = np.where(n <= tile // 2, st, 0.0)
        stat[h] = st[None]
    c["stat"] = stat
    p = np.arange(128, dtype=np.float32)
    c["qlo"] = _bf(np.stack([-slopes[h] * p for h in range(NH)], axis=1))
    kb = np.zeros((128, NH, 2), np.float32)
    for h in range(NH):
        for half in range(2):
            kb[:, h, half] = slopes[h] * (p + 128.0 * half)
    c["kbias"] = kb
    c["iota16"] = np.broadcast_to(np.arange(16, dtype=np.float32)[None], (128, 16)).copy()
    c["epsc"] = np.full((128, 1), EPS, np.float32)
    return c


def act(P, out, in_, func, r, w, **kw):
    P.op("act", lambda e: e.activation(out=out, in_=in_, func=func, **kw), r, w)


def tt(P, eng, out, in0, in1, op, r, w):
    P.op(eng, lambda e: e.tensor_tensor(out=out, in0=in0, in1=in1, op=op), r, w)


def ts(P, eng, out, in0, s1, s2, op0, op1, r, w, **kw):
    if s2 is None:
        P.op(eng, lambda e: e.tensor_scalar(out=out, in0=in0, scalar1=s1, scalar2=None, op0=op0, **kw), r, w)
    else:
        P.op(eng, lambda e: e.tensor_scalar(out=out, in0=in0, scalar1=s1, scalar2=s2, op0=op0, op1=op1, **kw), r, w)


def cp(P, eng, out, in_, r, w):
    if eng == "act":
        P.op("act", lambda e: e.copy(out=out, in_=in_), r, w)
    else:
        P.op(eng, lambda e: e.tensor_copy(out=out, in_=in_), r, w)


def mm(P, out, lhsT, rhs, start, stop, r, w):
    P.op("pe", lambda e: e.matmul(out, lhsT, rhs, start=start, stop=stop), r, w)


def tr(P, out, in_, ident, r, w):
    P.op("pe", lambda e: e.transpose(out, in_, ident), r, w)


def dma(P, q, out, in_, r, w):
    P.dma(q, lambda e: e.dma_start(out=out, in_=in_), r, w)


def rmsnorm_stats(P, xt, junk, ss, std, rstd, epsc, tag):
    act(P, junk, xt, AF.Square, [tag + "x"], [tag + "junk", tag + "ss"], accum_out=ss)
    act(P, std, ss, AF.Sqrt, [tag + "ss", "epsc"], [tag + "std"], bias=epsc, scale=1.0 / D)
    P.op("dve", lambda e: e.reciprocal(out=rstd, in_=std), [tag + "std"], [tag + "rstd"])


class Ctx:
    pass


def declare_io(nc, S, debug=False):
    c = Ctx()
    c.S = S

    def inp(name, shape, dt=F32):
        return nc.dram_tensor(name, list(shape), dt, kind="ExternalInput").ap()

    def scr(name, shape, dt):
        return nc.dram_tensor(name, list(shape), dt, kind="ExternalOutput" if debug else "Internal").ap()

    NT = S // 128
    c.x = inp("x", [S, D])
    c.w_in = inp("w_in", [D, NCOL])
    c.g1c = inp("g1c", [128, 8])
    c.conv_wT = inp("conv_wT", [128, 4, CW])
    c.conv_bc = inp("conv_bc", [128, 4])
    c.ln_gc = inp("ln_gc", [128, 4])
    c.ln_bc = inp("ln_bc", [128, 4])
    c.b_pwc = inp("b_pwc", [128, 8])
    c.w_pw = inp("w_conv_pw", [CC, D])
    c.w_ao = inp("w_attn_out", [NH * DH, D])
    c.w_out = inp("w_out", [D, D])
    c.w_pq = inp("w_peer_q", [D, PH * 256])
    c.g2rep = inp("g2rep", [128, D])
    c.gfrep = inp("gfrep", [128, D])
    c.keys1T = inp("keys1T", [128, NK])
    c.keys2T = inp("keys2T", [128, NK])
    c.peer_u = inp("peer_u", [NK * NK, D])
    c.peer_v = inp("peer_v", [NK * NK, D])
    c.ident_bf = inp("ident_bf", [128, 128], BF16)
    c.ident_f = inp("ident_f", [128, 128])
    c.kaug_static = inp("kaug_static", [17, S], BF16)
    c.masktri = inp("masktri", [4, 128, 512], BF16)
    c.valid01 = inp("valid01", [128, NT, 16])
    c.own01 = inp("own01", [128, NT, 16])
    c.maskv = inp("maskv", [128, NT, 16])
    c.stat = inp("stat", [NH, 128, NT, 16])
    c.qlo = inp("qlo", [128, NH], BF16)
    c.kbias = inp("kbias", [128, NH, 2])
    c.iota16 = inp("iota16", [128, 16])
    c.epsc = inp("epsc", [128, 1])
    c.out = nc.dram_tensor("out", [S, D], F32, kind="ExternalOutput").ap()
    c.zcT = scr("s_zcT", [CC, S], BF16)
    c.qT = scr("s_qT", [NH * DH, S], BF16)
    c.kT = scr("s_kT", [NH * DH, S], BF16)
    c.vS = scr("s_v", [S, NH * DH], BF16)
    c.sgc = scr("s_sgc", [D, S], BF16)
    c.sga = scr("s_sga", [D, S], BF16)
    c.oT = scr("s_oT", [NH * DH, S], BF16)
    c.x1 = scr("s_x1", [S, D], F32)
    c.uv = nc.dram_tensor("s_uv", [NK * NK, 2, D], BF16, kind="Internal").ap()
    c.dbg = scr("s_dbg", [4, 128, 2048], F32) if debug else None
    return c


def phase_a(nc, P, c):
    S = c.S
    NBLK = S // 512
    with ExitStack() as st:
        sb = lambda name, shape, dt: st.enter_context(nc.sbuf_tensor(name, list(shape), dt))
        ps = lambda name, shape, dt: st.enter_context(nc.psum_tensor(name, list(shape), dt))
        wbf = sb("a_wbf", [128, 8, NCOL], BF16)
        wst = [sb(f"a_wst{i}", [128, NCOL], F32) for i in range(2)]
        g1c = sb("a_g1c", [128, 8], F32)
        epsc = sb("a_eps", [128, 1], F32)
        ident = sb("a_ident", [128, 128], BF16)
        xt = [sb(f"a_xt{i}", [128, D], F32) for i in range(3)]
        junk = sb("a_junk", [128, D], F32)
        hn = [sb(f"a_hn{i}", [128, D], BF16) for i in range(2)]
        ss = [sb(f"a_ss{i}", [128, 1], F32) for i in range(2)]
        std = [sb(f"a_std{i}", [128, 1], F32) for i in range(2)]
        rstd = [sb(f"a_rstd{i}", [128, 1], F32) for i in range(2)]
        hnT = [sb(f"a_hnT{i}", [128, 8, 512], BF16) for i in range(2)]
        sig = [sb(f"a_sig{i}", [128, 512], F32) for i in range(2)]
        stg = [sb(f"a_stg{i}", [128, 512], BF16) for i in range(8)]
        tp = ps("a_tp", [128, 8, 128], BF16)
        acc = [ps(f"a_acc{i}", [128, 512], F32) for i in range(6)]

        dma(P, "sp", g1c[:], c.g1c, [], ["g1c"])
        dma(P, "sp", epsc[:], c.epsc, [], ["epsc"])
        dma(P, "sp", ident[:], c.ident_bf, [], ["ident"])
        w_in_v = c.w_in.rearrange("(k p) n -> k p n", p=128)
        for k in range(8):
            dma(P, "sp", wst[k % 2][:], w_in_v[k], [], [f"wst{k%2}"])
            act(P, wbf[:, k, :], wst[k % 2][:], AF.Copy, [f"wst{k%2}", "g1c"], [f"wbf{k}"], scale=g1c[:, k:k + 1])
        wall = [f"wbf{k}" for k in range(8)]

        xr, hr, sr, ar, gr = Rot(3), Rot(2), Rot(2), Rot(6), Rot(8)

        CH = 1024
        conv_list = [(ti, r0) for r0 in range(0, NK * NK, CH) for ti in range(2)]
        nstores = [0]
        per = max(1, (NBLK * 36) // len(conv_list))

        def conv_tick(force=False):
            nstores[0] += 1
            if conv_list and (force or nstores[0] % per == 0):
                ti, r0 = conv_list.pop(0)
                tab = (c.peer_u, c.peer_v)[ti]
                dma(P, "pool", c.uv[r0:r0 + CH, ti, :], tab[r0:r0 + CH, :], [], [])
        x_v = c.x.rearrange("(t p) d -> t p d", p=128)

        def store(dst, src_ps, tok, kind, scale=None):
            g = gr.next()
            npart = dst.shape[0]
            if kind == "sigmoid":
                act(P, stg[g][0:npart, :], src_ps, AF.Sigmoid, [tok], [f"stg{g}"])
            elif kind == "scale":
                act(P, stg[g][0:npart, :], src_ps, AF.Copy, [tok], [f"stg{g}"], scale=scale)
            else:
                cp(P, "dve", stg[g][0:npart, :], src_ps, [tok], [f"stg{g}"])
            dma(P, "pool", dst, stg[g][0:npart, :], [f"stg{g}"], [])
            conv_tick()

        for b in range(NBLK):
            hb = hr.next()
            for j in range(4):
                t = b * 4 + j
                xi = xr.next()
                si = sr.next()
                dma(P, "sp", xt[xi][:], x_v[t], [], [f"xt{xi}x"])
                rmsnorm_stats(P, xt[xi][:], junk[:], ss[si][:], std[si][:], rstd[si][:], epsc[:], f"xt{xi}")
                act(P, hn[si][:], xt[xi][:], AF.Copy, [f"xt{xi}x", f"xt{xi}rstd"], [f"hn{si}"], scale=rstd[si][:])
                for k in range(8):
                    tr(P, tp[:, k, :], hn[si][:, k * 128:(k + 1) * 128], ident[:], [f"hn{si}", "ident"], ["tp"])
                cp(P, "dve", hnT[hb][:, :, j * 128:(j + 1) * 128], tp[:], ["tp"], [f"hnT{hb}"])
            cols = slice(b * 512, (b + 1) * 512)

            def proj(c0, m):
                a = ar.next()
                for k in range(8):
                    mm(P, acc[a][0:m, :], wbf[:, k, c0:c0 + m], hnT[hb][:, k, :], k == 0, k == 7,
                       wall + [f"hnT{hb}"], [f"acc{a}"])
                return acc[a], f"acc{a}"

            for cc in range(4):
                pa, ta = proj(cc * 128, 128)
                pg, tg = proj(CC + cc * 128, 128)
                s_i = sr.next()
                act(P, sig[s_i][:], pg[:], AF.Sigmoid, [tg], [f"sig{s_i}"])
                g = gr.next()
                tt(P, "dve", stg[g][:], pa[:], sig[s_i][:], ALU.mult, [ta, f"sig{s_i}"], [f"stg{g}"])
                dma(P, "pool", c.zcT[cc * 128:(cc + 1) * 128, cols], stg[g][:], [f"stg{g}"], [])
                conv_tick()
            for cc in range(4):
                pq, tq = proj(2 * CC + cc * 128, 128)
                store(c.qT[cc * 128:(cc + 1) * 128, cols], pq[:], tq, "scale", scale=DH ** -0.5)
            for cc in range(4):
                pk, tk = proj(2 * CC + 512 + cc * 128, 128)
                store(c.kT[cc * 128:(cc + 1) * 128, cols], pk[:], tk, "copy")
            for cc in range(8):
                pg, tg = proj(2 * CC + 1536 + cc * 128, 128)
                store(c.sgc[cc * 128:(cc + 1) * 128, cols], pg[:], tg, "sigmoid")
            for cc in range(8):
                pg, tg = proj(2 * CC + 1536 + D + cc * 128, 128)
                store(c.sga[cc * 128:(cc + 1) * 128, cols], pg[:], tg, "sigmoid")
            for j in range(4):
                a = ar.next()
                for k in range(8):
                    mm(P, acc[a][:], hnT[hb][:, k, j * 128:(j + 1) * 128], wbf[:, k, 2 * CC + 1024:2 * CC + 1536],
                       k == 0, k == 7, wall + [f"hnT{hb}"], [f"acc{a}"])
                t = b * 4 + j
                store(c.vS[t * 128:(t + 1) * 128, :], acc[a][:], f"acc{a}", "copy")
        while conv_list:
            conv_tick(force=True)
        P.barrier()
        P.emit()


def host_inputs(S, x_b, w, consts):
    f = lambda a: np.ascontiguousarray(np.asarray(a, dtype=np.float32))
    col = lambda v, n: f(np.asarray(v).reshape(n, 128).T)
    m = {
        "x": f(x_b),
        "w_in": f(w["w_in"][0]),
        "g1c": col(w["g_norm1"][0], 8),
        "conv_wT": f(np.asarray(w["conv_w"][0]).reshape(CW, 4, 128).transpose(2, 1, 0)),
        "conv_bc": col(w["conv_b"][0], 4),
        "ln_gc": col(w["conv_ln_g"][0], 4),
        "ln_bc": col(w["conv_ln_b"][0], 4),
        "b_pwc": col(w["b_conv_pw"][0], 8),
        "w_conv_pw": f(w["w_conv_pw"][0]),
        "w_attn_out": f(w["w_attn_out"][0]),
        "w_out": f(w["w_out"][0]),
        "w_peer_q": f(w["w_peer_q"][0]),
        "g2rep": f(np.broadcast_to(np.asarray(w["g_norm2"][0])[None, :], (128, D))),
        "gfrep": f(np.broadcast_to(np.asarray(w["g_final"])[None, :], (128, D))),
        "keys1T": f(np.asarray(w["peer_keys1"][0]).T),
        "keys2T": f(np.asarray(w["peer_keys2"][0]).T),
        "peer_u": f(w["peer_u"][0]),
        "peer_v": f(w["peer_v"][0]),
    }
    m.update(consts)
    return m


def phase_b(nc, P, c, preload=None):
    S = c.S
    NT = S // 128
    NQB = S // 512
    with ExitStack() as st:
        sb = lambda name, shape, dt: st.enter_context(nc.sbuf_tensor(name, list(shape), dt))
        ps = lambda name, shape, dt: st.enter_context(nc.psum_tensor(name, list(shape), dt))
        kaug = [sb(f"b_kaug{i}", [128, S], BF16) for i in range(2)]
        qaug = [sb(f"b_qaug{i}", [128, S], BF16) for i in range(2)]
        vall = sb("b_vall", [128, NT, NH * DH], BF16)
        vaug = [sb(f"b_vaug{i}", [128, NT, DH + 1], BF16) for i in range(2)]
        augtok = sb("b_augtok", [128, NT, 81], BF16)
        valid01 = sb("b_valid", [128, NT, 16], F32)
        own01 = sb("b_own", [128, NT, 16], F32)
        maskv = sb("b_maskv", [128, NT, 16], F32)
        stat = [sb(f"b_stat{i}", [128, NT, 16], F32) for i in range(2)]
        kbias = sb("b_kbias", [128, NH, 2], F32)
        qlo = sb("b_qlo", [128, NH], BF16)
        masktri = sb("b_masktri", [128, 4, 512], BF16)
        ident = sb("b_ident", [128, 128], BF16)
        onesf = sb("b_onesf", [128, 64], F32)
        kms = sb("b_kms", [128, 16], F32)
        kmb = [sb(f"b_kmb{i}", [128, 16], BF16) for i in range(2)]
        bsm = sb("b_bsm", [128, NT, 16], F32)
        m8 = sb("b_m8", [128, NT, 8], F32)
        sel = sb("b_sel", [128, NT, 16], F32)
        PT = [sb(f"b_PT{i}", [128, 512], BF16) for i in range(3)]
        rden = [sb(f"b_rden{i}", [128, 512], F32) for i in range(2)]
        bc_sb = sb("b_bcsb", [128, 512], F32)
        oT_sb = [sb(f"b_oT{i}", [128, 512], BF16) for i in range(2)]
        st_ps = [ps(f"b_st{i}", [128, 512], F32) for i in range(3)]
        o_ps = [ps(f"b_o{i}", [128, 512], F32) for i in range(2)]
        bc_ps = ps("b_bc", [128, 512], F32)
        tp = ps("b_tp", [128, 8, 128], BF16)
        bs_ps = ps("b_bs", [128, NT, 16], F32)

        for i in range(2):
            dma(P, "sp", kaug[i][64:81, :], c.kaug_static, [], [f"kaug_s{i}"])
        dma(P, "sp", vall[:], c.vS.rearrange("(t p) n -> p t n", p=128), [], ["vall"])
        dma(P, "sp", valid01[:], c.valid01, [], ["valid01"])
        dma(P, "sp", own01[:], c.own01, [], ["own01"])
        dma(P, "sp", maskv[:], c.maskv, [], ["maskv"])
        dma(P, "sp", kbias[:], c.kbias, [], ["kbias"])
        dma(P, "sp", qlo[:], c.qlo, [], ["qlo"])
        dma(P, "sp", masktri[:], c.masktri.rearrange("i p n -> p i n"), [], ["masktri"])
        dma(P, "sp", ident[:], c.ident_bf, [], ["ident"])
        P.op("dve", lambda e: e.memset(onesf[:], 1.0), [], ["onesf"])
        P.op("dve", lambda e: e.memset(augtok[:], 0.0), [], ["augtok"])
        for i in range(2):
            P.op("dve", (lambda i: lambda e: e.memset(vaug[i][:], 1.0))(i), [], [f"vaug{i}"])

        def prologue(h):
            hp = h % 2
            hs = slice(h * DH, (h + 1) * DH)
            KA, QA, VA, ST, KM = f"kaug{hp}", f"qaug{hp}", f"vaug{hp}", f"stat{hp}", f"kmb{hp}"
            dma(P, "sp", kaug[hp][0:64, :], c.kT[hs, :], [], [KA])
            dma(P, "sp", qaug[hp][0:64, :], c.qT[hs, :], [], [QA])
            dma(P, "sp", stat[hp][:], c.stat[h], [], [ST])
            cp(P, "pool", vaug[hp][:, :, 0:DH], vall[:, :, hs], ["vall"], [VA])
            yield
            P.op("dve", lambda e: e.tensor_reduce(out=kms[0:64, 0:S // MB], in_=kaug[hp][0:64, :].rearrange("p (n k) -> p n k", k=MB),
                                                  axis=AX.X, op=ALU.add), [KA], ["kms"])
            P.op("act", lambda e: e.mul(out=kmb[hp][0:64, 0:S // MB], in_=kms[0:64, 0:S // MB], mul=1.0 / MB), ["kms"], [KM])
            if S // MB < 16:
                P.op("dve", lambda e: e.memset(kmb[hp][0:64, S // MB:16], 0.0), [], [KM])
            yield
            for t0 in range(0, NT, 8):
                for t in range(t0, t0 + 8):
                    mm(P, bs_ps[:, t, :], qaug[hp][0:64, t * 128:(t + 1) * 128], kmb[hp][0:64, :], True, True, [QA, KM], ["bs_ps"])
                yield
            tt(P, "dve", bsm[:], bs_ps[:], maskv[:], ALU.add, ["bs_ps", "maskv"], ["bsm"])
            for t0 in range(0, NT, 8):
                for t in range(t0, t0 + 8):
                    P.op("dve", (lambda t: lambda e: e.max(out=m8[:, t, :], in_=bsm[:, t, :]))(t), ["bsm"], ["m8"])
                yield
            tt(P, "dve", sel[:], bsm[:], m8[:, :, 2:3].to_broadcast([128, NT, 16]), ALU.is_ge, ["bsm", "m8"], ["sel"])
            tt(P, "dve", sel[:], sel[:], valid01[:], ALU.mult, ["sel", "valid01"], ["sel"])
            tt(P, "dve", sel[:], sel[:], own01[:], ALU.add, ["sel", "own01"], ["sel"])
            ts(P, "dve", sel[:], sel[:], -1.0, BIG, ALU.add, ALU.mult, ["sel"], ["sel"])
            tt(P, "dve", augtok[:, :, 65:81], sel[:], stat[hp][:], ALU.add, ["sel", ST], ["augtok"])
            cp(P, "dve", augtok[:, :, 64], qlo[:, h:h + 1].to_broadcast([128, NT]), ["qlo"], ["augtok"])
            yield
            for t0 in range(0, NT, 8):
                for j in range(8):
                    tr(P, tp[0:81, j, :], augtok[:, t0 + j, :], ident[:], ["augtok", "ident"], ["tp"])
                cp(P, "dve", qaug[hp][64:81, t0 * 128:(t0 + 8) * 128].rearrange("p (a b) -> p a b", b=128), tp[64:81, :, :],
                   ["tp"], [QA])
                yield

        def advance(gen, n):
            if gen is None:
                return None
            for _ in range(n):
                try:
                    next(gen)
                except StopIteration:
                    return None
            return gen

        sr, pr, orr, osr = Rot(3), Rot(3), Rot(2), Rot(2)
        gen = prologue(0)
        while gen is not None:
            gen = advance(gen, 1)
        if preload is not None:
            preload()
        for h in range(NH):
            hp = h % 2
            hs = slice(h * DH, (h + 1) * DH)
            KA, QA, VA = f"kaug{hp}", f"qaug{hp}", f"vaug{hp}"
            units = [(qb, kt) for qb in range(NQB) for kt in range(4 * qb + 4)]
            NU = len(units)
            sbuf_of = {}
            obuf_of = {}
            pending = []

            def emit_s(n):
                qb, kt = units[n]
                si = sr.next()
                sbuf_of[n] = si
                diag = kt >= 4 * qb
                i = kt - 4 * qb if diag else 0
                c0 = i * 128
                mm(P, st_ps[si][:, c0:512], kaug[hp][0:81, kt * 128:(kt + 1) * 128], qaug[hp][0:81, qb * 512 + c0:(qb + 1) * 512],
                   True, not diag, [KA, f"kaug_s{hp}", QA], [f"st{si}"])
                if diag:
                    mm(P, st_ps[si][:, c0:c0 + 128], ident[:], masktri[:, i, c0:c0 + 128], False, True, ["ident", "masktri"], [f"st{si}"])

            def emit_pv(n):
                qb, kt = units[n]
                nkt = 4 * qb + 4
                si = sbuf_of.pop(n)
                if kt == 0:
                    obuf_of[qb] = orr.next()
                oi = obuf_of[qb]
                pi = pr.next()
                diag = kt >= 4 * qb
                c0 = (kt - 4 * qb) * 128 if diag else 0
                act(P, PT[pi][:, c0:512], st_ps[si][:, c0:512], AF.Exp, [f"st{si}", "kbias"], [f"PT{pi}"],
                    bias=kbias[:, h, (kt % 2):(kt % 2) + 1])
                mm(P, o_ps[oi][0:65, c0:512], vaug[hp][:, kt, :], PT[pi][:, c0:512], kt == 0, kt == nkt - 1, [VA, f"PT{pi}"], [f"o{oi}"])
                if kt == nkt - 1:
                    o = o_ps[oi]
                    ri = qb % 2
                    P.op("dve", lambda e: e.reciprocal(out=rden[ri][64:65, :], in_=o[64:65, :]), [f"o{oi}"], [f"rden{ri}"])

                    def fin2(qb=qb, oi=oi, o=o, ri=ri):
                        mm(P, bc_ps[0:64, :], onesf[64:65, 0:64], rden[ri][64:65, :], True, True, ["onesf", f"rden{ri}"], ["bc_ps"])
                        cp(P, "act", bc_sb[0:64, :], bc_ps[0:64, :], ["bc_ps"], ["bc_sb"])
                        oo = osr.next()
                        tt(P, "dve", oT_sb[oo][0:64, :], o[0:64, :], bc_sb[0:64, :], ALU.mult, [f"o{oi}", "bc_sb"], [f"oT{oo}"])
                        dma(P, "pool", c.oT[hs, qb * 512:(qb + 1) * 512], oT_sb[oo][0:64, :], [f"oT{oo}"], [])
                    pending.append((n + 2, fin2))

            gen = prologue(h + 1) if h + 1 < NH else None
            emit_s(0)
            for n in range(NU):
                if n + 1 < NU:
                    emit_s(n + 1)
                emit_pv(n)
                while pending and pending[0][0] <= n:
                    pending.pop(0)[1]()
                if n >= 8 and n % 4 == 0:
                    gen = advance(gen, 1)
            while pending:
                pending.pop(0)[1]()
            while gen is not None:
                gen = advance(gen, 1)
        P.barrier()
        P.emit()


def alloc_c_weights(nc, st):
    sb = lambda name, shape, dt: st.enter_context(nc.sbuf_tensor(name, list(shape), dt))
    wpw = sb("c_wpw", [128, 4, D], BF16)
    wao = sb("c_wao", [128, NH // 2, D], BF16)
    wout = sb("c_wout", [128, 8, D], BF16)
    dg = sb("c_dg", [128, 4, CW, 128], BF16)
    identf = sb("c_identf", [128, 128], F32)
    convw = sb("c_convw", [128, 4, CW], F32)
    convb = sb("c_convb", [128, 4], F32)
    lng = sb("c_lng", [128, 4], F32)
    lnb = sb("c_lnb", [128, 4], F32)
    bpw = sb("c_bpw", [128, 8], F32)
    epsc = sb("c_eps", [128, 1], F32)
    onesm = sb("c_onesm", [128, 128], F32)
    return (wpw, wao, wout, dg, identf, convw, convb, lng, lnb, bpw, epsc, onesm)


def load_c_weights(nc, P, c, wts, wst):
    (wpw, wao, wout, dg, identf, convw, convb, lng, lnb, bpw, epsc, onesm) = wts
    for name, t, src in (("identf", identf, c.ident_f), ("convw", convw, c.conv_wT), ("convb", convb, c.conv_bc),
                         ("lng", lng, c.ln_gc), ("lnb", lnb, c.ln_bc), ("bpw", bpw, c.b_pwc), ("epsc", epsc, c.epsc)):
        dma(P, "sp", t[:], src, [], [name])
    P.op("dve", lambda e: e.memset(onesm[:], 1.0 / CC), [], ["onesm"])
    dma(P, "pool", wpw[:], c.w_pw.rearrange("(k p) n -> p k n", p=128), [], ["wpw"])
    dma(P, "pool", wao[:], c.w_ao.rearrange("(k p) n -> p k n", p=128), [], ["wao"])
    dma(P, "pool", wout[:], c.w_out.rearrange("(k p) n -> p k n", p=128), [], ["wout"])
    for cc in range(4):
        tt(P, "dve", dg[:, cc, :, :], identf[:].unsqueeze(1).to_broadcast([128, CW, 128]),
           convw[:, cc, :].unsqueeze(2).to_broadcast([128, CW, 128]), ALU.mult, ["identf", "convw"], ["dg"])


def phase_c(nc, P, c, wts):
    S = c.S
    NBLK = S // 512
    HALO = CW - 1
    with ExitStack() as st:
        sb = lambda name, shape, dt: st.enter_context(nc.sbuf_tensor(name, list(shape), dt))
        ps = lambda name, shape, dt: st.enter_context(nc.psum_tensor(name, list(shape), dt))
        (wpw, wao, wout, dg, identf, convw, convb, lng, lnb, bpw, epsc, onesm) = wts
        zcb = [sb(f"c_zcb{i}", [128, 4, 512 + HALO], BF16) for i in range(2)]
        zconv = sb("c_zconv", [128, 4, 512], F32)
        zsq = sb("c_zsq", [128, 4, 512], F32)
        mean_sb = sb("c_mean", [128, 512], F32)
        msq = sb("c_msq", [128, 512], F32)
        var = sb("c_var", [128, 512], F32)
        rstd = sb("c_rstd", [128, 512], F32)
        t1 = [sb(f"c_t1{i}", [128, 512], F32) for i in range(2)]
        zact = sb("c_zact", [128, 4, 512], BF16)
        oTb = [sb(f"c_oTb{i}", [128, NH // 2, 512], BF16) for i in range(2)]
        sgcb = [sb(f"c_sgcb{i}", [128, 8, 512], BF16) for i in range(2)]
        sgab = [sb(f"c_sgab{i}", [128, 8, 512], BF16) for i in range(2)]
        mixc = [sb(f"c_mixc{i}", [128, 512], F32) for i in range(2)]
        mixa = [sb(f"c_mixa{i}", [128, 512], F32) for i in range(2)]
        mix = sb("c_mix", [128, 8, 512], BF16)
        xt = [sb(f"c_xt{i}", [128, D], F32) for i in range(2)]
        x1t = [sb(f"c_x1t{i}", [128, D], F32) for i in range(2)]
        cv_ps = [ps(f"c_cv{i}", [128, 512], F32) for i in range(2)]
        mean_ps = ps("c_meanps", [128, 512], F32)
        ex2_ps = ps("c_ex2ps", [128, 512], F32)
        y_ps = [ps(f"c_y{i}", [128, 512], F32) for i in range(2)]
        o_ps = [ps(f"c_op{i}", [128, 512], F32) for i in range(2)]

        zc_v = c.zcT.rearrange("(c p) s -> p c s", p=128)
        sgc_v = c.sgc.rearrange("(f p) s -> p f s", p=128)
        sga_v = c.sga.rearrange("(f p) s -> p f s", p=128)
        oT_v = c.oT.rearrange("(h d) s -> d h s", d=2 * DH)
        x_v = c.x.rearrange("(t p) d -> t p d", p=128)
        x1_v = c.x1.rearrange("(t p) d -> t p d", p=128)
        cr, tr1, yr, opr, xr, mr = Rot(2), Rot(2), Rot(2), Rot(2), Rot(2), Rot(2)
        for b in range(NBLK):
            bi = b % 2
            cols = slice(b * 512, (b + 1) * 512)
            if b == 0:
                P.op("dve", lambda e: e.memset(zcb[0][:, :, 0:HALO], 0.0), [], ["zcb0"])
                dma(P, "sp", zcb[0][:, :, HALO:], zc_v[:, :, 0:512], [], ["zcb0"])
            else:
                dma(P, "sp", zcb[bi][:], zc_v[:, :, b * 512 - HALO:(b + 1) * 512], [], [f"zcb{bi}"])
            dma(P, "sp", sgcb[bi][:], sgc_v[:, :, cols], [], [f"sgcb{bi}"])
            dma(P, "sp", sgab[bi][:], sga_v[:, :, cols], [], [f"sgab{bi}"])
            dma(P, "sp", oTb[bi][:], oT_v[:, :, cols], [], [f"oTb{bi}"])
            for cc in range(4):
                ci = cr.next()
                for k in range(CW):
                    mm(P, cv_ps[ci][:], dg[:, cc, k, :], zcb[bi][:, cc, k:k + 512], k == 0, k == CW - 1,
                       ["dg", f"zcb{bi}"], [f"cv{ci}"])
                act(P, zconv[:, cc, :], cv_ps[ci][:], AF.Identity, [f"cv{ci}", "convb"], [f"zconv{cc}"], bias=convb[:, cc:cc + 1])
                act(P, zsq[:, cc, :], zconv[:, cc, :], AF.Square, [f"zconv{cc}"], [f"zsq{cc}"])
            for cc in range(4):
                mm(P, mean_ps[:], onesm[:], zconv[:, cc, :], cc == 0, cc == 3, ["onesm", f"zconv{cc}"], ["mean_ps"])
            for cc in range(4):
                mm(P, ex2_ps[:], onesm[:], zsq[:, cc, :], cc == 0, cc == 3, ["onesm", f"zsq{cc}"], ["ex2_ps"])
            cp(P, "act", mean_sb[:], mean_ps[:], ["mean_ps"], ["mean_sb"])
            act(P, msq[:], mean_ps[:], AF.Square, ["mean_ps"], ["msq"])
            tt(P, "dve", var[:], ex2_ps[:], msq[:], ALU.subtract, ["ex2_ps", "msq"], ["var"])
            act(P, var[:], var[:], AF.Sqrt, ["var", "epsc"], ["var"], bias=epsc[:], scale=1.0)
            P.op("dve", lambda e: e.reciprocal(out=rstd[:], in_=var[:]), ["var"], ["rstd"])
            for cc in range(4):
                ti = tr1.next()
                tt(P, "dve", t1[ti][:], zconv[:, cc, :], mean_sb[:], ALU.subtract, [f"zconv{cc}", "mean_sb"], [f"t1{ti}"])
                tt(P, "dve", t1[ti][:], t1[ti][:], rstd[:], ALU.mult, [f"t1{ti}", "rstd"], [f"t1{ti}"])
                act(P, zact[:, cc, :], t1[ti][:], AF.Silu, [f"t1{ti}", "lng", "lnb"], [f"zact{cc}"],
                    scale=lng[:, cc:cc + 1], bias=lnb[:, cc:cc + 1])
            zall = [f"zact{cc}" for cc in range(4)]
            if c.dbg is not None and b == 0:
                dma(P, "sp", c.dbg[1, :, 0:512], mean_sb[:], ["mean_sb"], [])
                dma(P, "sp", c.dbg[1, :, 512:1024], var[:], ["var"], [])
                dma(P, "sp", c.dbg[1, :, 1024:1536], rstd[:], ["rstd"], [])
            for f in range(8):
                fs = slice(f * 128, (f + 1) * 128)
                yi = yr.next()
                for cc in range(4):
                    mm(P, y_ps[yi][:], wpw[:, cc, fs], zact[:, cc, :], cc == 0, cc == 3, ["wpw"] + zall, [f"y{yi}"])
                mi = mr.next()
                act(P, mixc[mi][:], y_ps[yi][:], AF.Identity, [f"y{yi}", "bpw"], [f"mixc{mi}"], bias=bpw[:, f:f + 1])
                tt(P, "dve", mixc[mi][:], mixc[mi][:], sgcb[bi][:, f, :], ALU.mult, [f"mixc{mi}", f"sgcb{bi}"], [f"mixc{mi}"])
                yi2 = yr.next()
                for h in range(NH // 2):
                    mm(P, y_ps[yi2][:], wao[:, h, fs], oTb[bi][:, h, :], h == 0, h == NH // 2 - 1,
                       ["wao", f"oTb{bi}"], [f"y{yi2}"])
                tt(P, "dve", mixa[mi][:], y_ps[yi2][:], sgab[bi][:, f, :], ALU.mult, [f"y{yi2}", f"sgab{bi}"], [f"mixa{mi}"])
                tt(P, "pool", mix[:, f, :], mixa[mi][:], mixc[mi][:], ALU.add, [f"mixa{mi}", f"mixc{mi}"], [f"mix{f}"])
            mall = [f"mix{f}" for f in range(8)]
            for j in range(4):
                t = b * 4 + j
                xi = xr.next()
                dma(P, "sp", xt[xi][:], x_v[t], [], [f"xt{xi}"])
                for half in range(2):
                    oi = opr.next()
                    hsl = slice(half * 512, (half + 1) * 512)
                    for f in range(8):
                        mm(P, o_ps[oi][:], mix[:, f, j * 128:(j + 1) * 128], wout[:, f, hsl], f == 0, f == 7,
                           mall + ["wout"], [f"op{oi}"])
                    tt(P, "dve", x1t[xi][:, hsl], xt[xi][:, hsl], o_ps[oi][:], ALU.add, [f"xt{xi}", f"op{oi}"], [f"x1t{xi}"])
                dma(P, "pool", x1_v[t], x1t[xi][:], [f"x1t{xi}"], [])
        P.barrier()
        P.emit()


def phase_d(nc, P, c):
    S = c.S
    NT = S // 128
    G = 2
    NB_G = 8
    NSL = PH * 16
    with ExitStack() as st:
        sb = lambda name, shape, dt: st.enter_context(nc.sbuf_tensor(name, list(shape), dt))
        ps = lambda name, shape, dt: st.enter_context(nc.psum_tensor(name, list(shape), dt))
        wq = sb("d_wq", [128, 8, PH * 256], BF16)
        kT = [sb(f"d_keysT{i}", [128, NK], BF16) for i in range(2)]
        kTf = sb("d_keysTf", [128, NK], F32)
        g2rep = sb("d_g2rep", [128, D], F32)
        gfrep = sb("d_gfrep", [128, D], F32)
        identb = sb("d_identb", [128, 128], BF16)
        epsc = sb("d_eps", [128, 1], F32)
        iota16 = sb("d_iota16", [128, 16], F32)
        thr17 = sb("d_thr17", [128, 17], F32)
        x1t = [sb(f"d_x1t{i}", [128, D], F32) for i in range(3)]
        tmp = sb("d_tmp", [128, D], F32)
        hn2 = sb("d_hn2", [128, D], F32)
        hn2b = [sb(f"d_hn2b{i}", [128, D], BF16) for i in range(2)]
        hn2T = sb("d_hn2T", [128, 8, 128], BF16)
        ss = sb("d_ss", [128, 1], F32)
        std = sb("d_std", [128, 1], F32)
        rstd = sb("d_rstd", [128, 1], F32)
        fss = sb("d_fss", [128, 1], F32)
        fstd = sb("d_fstd", [128, 1], F32)
        frstd = sb("d_frstd", [128, 1], F32)
        qT_sb = sb("d_qT", [128, 16, 128], BF16)
        s_sb = sb("d_s", [128, 16, NK], F32)
        gebuf = sb("d_ge", [128, PH * 16 * 17], F32)
        ohbuf = sb("d_oh", [128, PH * 256], F32)
        s2 = gebuf[:, 0:16 * NK].rearrange("p (g n) -> p g n", n=NK)
        ge = gebuf[:].rearrange("p (h k a) -> p h k a", k=16, a=17)
        cand2 = ohbuf[:].rearrange("p (h n) -> p h n", n=256)
        oh = ohbuf[:].rearrange("p (h k a) -> p h k a", k=16, a=16)
        m16 = sb("d_m16", [128, 16, 16], F32)
        i16 = sb("d_i16", [128, 16, 16], U32)
        i16f = sb("d_i16f", [128, 16, 16], F32)
        cand = sb("d_cand", [128, PH, 256], F32)
        sc = sb("d_sc", [128, PH, 16], F32)
        pos = sb("d_pos", [128, PH, 16], U32)
        posf = sb("d_posf", [128, PH, 16], F32)
        af = sb("d_af", [128, PH, 16], F32)
        bf_ = sb("d_bf", [128, PH, 16], F32)
        i1s = sb("d_i1s", [128, PH, 16], F32)
        i2s = sb("d_i2s", [128, PH, 16], F32)
        eidx = [sb(f"d_eidx{i}", [128, NSL], U32) for i in range(2)]
        ee = sb("d_ee", [128, PH, 16], F32)
        zz = sb("d_zz", [128, PH], F32)
        gg = [sb(f"d_gg{i}", [128, NSL], F32) for i in range(2)]
        adot = sb("d_adot", [128, NSL], F32)
        ga = sb("d_ga", [128, NSL], F32)
        cw = sb("d_cw", [128, NSL], F32)
        uvg = [sb(f"d_uvg{i}", [128, G, 2, D], BF16) for i in range(NB_G)]
        prodb = [sb(f"d_prodb{i}", [128, D], BF16) for i in range(4)]
        junkb = sb("d_junkb", [128, D], BF16)
        dgc = [sb(f"d_dgc{i}", [128, 128], BF16) for i in range(8)]
        x2 = sb("d_x2", [128, D], F32)
        ot = sb("d_ot", [128, D], F32)
        tp = ps("d_tp", [128, 8, 128], BF16)
        qs_ps = [ps(f"d_qs{i}", [128, 4, 128], F32) for i in range(2)]
        y_ps = [[ps(f"d_y{i}{h}", [128, 512], F32) for h in range(2)] for i in range(2)]
        fold_ps = ps("d_fold", [128, 512], F32)
        junks = sb("d_junks", [128, 128], BF16)

        for name, t_, src in (("g2rep", g2rep, c.g2rep), ("gfrep", gfrep, c.gfrep),
                              ("identb", identb, c.ident_bf), ("epsc", epsc, c.epsc), ("iota16", iota16, c.iota16)):
            dma(P, "sp", t_[:], src, [], [name])
        for i, src in enumerate((c.keys1T, c.keys2T)):
            dma(P, "pool", kT[i][:], src, [], [f"kT{i}"])
        ts(P, "dve", thr17[:, 0:16], iota16[:], 16.0, None, ALU.mult, None, ["iota16"], ["thr17"])
        P.op("dve", lambda e: e.memset(thr17[:, 16:17], 256.0), [], ["thr17b"])
        dma(P, "pool", wq[:], c.w_pq.rearrange("(k p) n -> p k n", p=128), [], ["wq"])

        x1_v = c.x1.rearrange("(t p) d -> t p d", p=128)
        out_v = c.out.rearrange("(t p) d -> t p d", p=128)
        uv_v = c.uv.rearrange("e s d -> e (s d)")
        m16v = m16[:].rearrange("p (h s) k -> p h s k", s=2)
        i16fv = i16f[:].rearrange("p (h s) k -> p h s k", s=2)
        cand4 = cand[:].rearrange("p h (a b) -> p h a b", b=16)
        B4 = [128, PH, 16, 16]

        def prologue(t):
            par = t % 2
            xp = t % 3
            X, HB, EI, GG = f"x1t{xp}", f"hn2b{par}", f"eidx{par}", f"gg{par}"
            dma(P, "sp", x1t[xp][:], x1_v[t], [], [X])
            yield
            act(P, junkb[:], x1t[xp][:], AF.Square, [X], ["junkb", "ss"], accum_out=ss[:])
            yield
            act(P, std[:], ss[:], AF.Sqrt, ["ss", "epsc"], ["std"], bias=epsc[:], scale=1.0 / D)
            yield
            P.op("dve", lambda e: e.reciprocal(out=rstd[:], in_=std[:]), ["std"], ["rstd"])
            yield
            act(P, tmp[:], x1t[xp][:], AF.Copy, [X, "rstd"], ["tmp"], scale=rstd[:])
            yield
            tt(P, "dve", hn2[:], tmp[:], g2rep[:], ALU.mult, ["tmp", "g2rep"], ["hn2"])
            yield
            cp(P, "act", hn2b[par][:], hn2[:], ["hn2"], [HB])
            yield
            for k in range(8):
                tr(P, tp[:, k, :], hn2b[par][:, k * 128:(k + 1) * 128], identb[:], [HB, "identb"], ["tp"])
            yield
            cp(P, "dve", hn2T[:], tp[:], ["tp"], ["hn2T"])
            yield

            def qmm(g4):
                for gi in range(4):
                    g = g4 * 4 + gi
                    for k in range(8):
                        mm(P, qs_ps[g4 % 2][:, gi, :], wq[:, k, g * 128:(g + 1) * 128], hn2T[:, k, :], k == 0, k == 7,
                           ["wq", "hn2T"], [f"qs{g4 % 2}"])

            def qcp(g4):
                cp(P, "act", qT_sb[:, g4 * 4:(g4 + 1) * 4, :], qs_ps[g4 % 2][:], [f"qs{g4 % 2}"], [f"qT{g4}"])

            def smm(g4):
                for gi in range(4):
                    g = g4 * 4 + gi
                    mm(P, qs_ps[g4 % 2][:, gi, :], qT_sb[:, g, :], kT[g % 2][:], True, True, [f"qT{g4}", f"kT{g%2}"], [f"qs{g4 % 2}"])

            def scp(g4):
                cp(P, "act", s_sb[:, g4 * 4:(g4 + 1) * 4, :], qs_ps[g4 % 2][:], [f"qs{g4 % 2}"], [f"s{g4}"])

            for g4 in range(5):
                if g4 < 4:
                    qmm(g4)
                if g4 >= 1:
                    qcp(g4 - 1)
                yield
            for g4 in range(5):
                if g4 < 4:
                    smm(g4)
                if g4 >= 1:
                    scp(g4 - 1)
                yield
            for g in range(16):
                P.op("dve", (lambda g: lambda e: e.max(out=m16[:, g, 0:8], in_=s_sb[:, g, :]))(g), [f"s{g // 4}"], [f"m16a{g}"])
                if g % 4 == 3:
                    yield
            for g in range(16):
                P.op("dve", (lambda g: lambda e: e.max_index(out=i16[:, g, 0:8], in_max=m16[:, g, 0:8], in_values=s_sb[:, g, :]))(g),
                     [f"s{g // 4}", f"m16a{g}"], [f"i16a{g}"])
                if g % 4 == 3:
                    yield
            for g in range(16):
                P.op("dve", (lambda g: lambda e: e.match_replace(out=s2[:, g, :], in_to_replace=m16[:, g, 0:8],
                                                                 in_values=s_sb[:, g, :], imm_value=NEG))(g),
                     [f"s{g // 4}", f"m16a{g}"], [f"s2_{g}"])
                if g % 4 == 3:
                    yield
            for g in range(16):
                P.op("dve", (lambda g: lambda e: e.max(out=m16[:, g, 8:16], in_=s2[:, g, :]))(g), [f"s2_{g}"], [f"m16b{g}"])
                if g % 4 == 3:
                    yield
            for g in range(16):
                P.op("dve", (lambda g: lambda e: e.max_index(out=i16[:, g, 8:16], in_max=m16[:, g, 8:16], in_values=s2[:, g, :]))(g),
                     [f"s2_{g}", f"m16b{g}"], [f"i16b{g}"])
                if g % 4 == 3:
                    yield
            m16all = [f"m16a{g}" for g in range(16)] + [f"m16b{g}" for g in range(16)]
            i16all = [f"i16a{g}" for g in range(16)] + [f"i16b{g}" for g in range(16)]
            cp(P, "dve", i16f[:], i16[:], i16all, ["i16f"])
            tt(P, "dve", cand4, m16v[:, :, 0, :].unsqueeze(3).to_broadcast(B4), m16v[:, :, 1, :].unsqueeze(2).to_broadcast(B4),
               ALU.add, m16all, ["cand"])
            yield
            for h in range(PH):
                P.op("dve", (lambda h: lambda e: e.max(out=sc[:, h, 0:8], in_=cand[:, h, :]))(h), ["cand"], [f"sca{h}"])
                if h % 4 == 3:
                    yield
            for h in range(PH):
                P.op("dve", (lambda h: lambda e: e.max_index(out=pos[:, h, 0:8], in_max=sc[:, h, 0:8], in_values=cand[:, h, :]))(h),
                     ["cand", f"sca{h}"], [f"posa{h}"])
                if h % 4 == 3:
                    yield
            for h in range(PH):
                P.op("dve", (lambda h: lambda e: e.match_replace(out=cand2[:, h, :], in_to_replace=sc[:, h, 0:8],
                                                                 in_values=cand[:, h, :], imm_value=NEG))(h),
                     ["cand", f"sca{h}"], [f"c2_{h}"])
                if h % 4 == 3:
                    yield
            for h in range(PH):
                P.op("dve", (lambda h: lambda e: e.max(out=sc[:, h, 8:16], in_=cand2[:, h, :]))(h), [f"c2_{h}"], [f"scb{h}"])
                if h % 4 == 3:
                    yield
            for h in range(PH):
                P.op("dve", (lambda h: lambda e: e.max_index(out=pos[:, h, 8:16], in_max=sc[:, h, 8:16], in_values=cand2[:, h, :]))(h),
                     [f"c2_{h}", f"scb{h}"], [f"posb{h}"])
                if h % 4 == 3:
                    yield
            scall = [f"sca{h}" for h in range(PH)] + [f"scb{h}" for h in range(PH)]
            posall = [f"posa{h}" for h in range(PH)] + [f"posb{h}" for h in range(PH)]
            cp(P, "dve", posf[:], pos[:], posall, ["posf"])
            tt(P, "dve", ge, posf[:].unsqueeze(3).to_broadcast([128, PH, 16, 17]),
               thr17[:].unsqueeze(1).unsqueeze(1).to_broadcast([128, PH, 16, 17]), ALU.is_ge, ["posf", "thr17", "thr17b"], ["ge"])
            yield
            tt(P, "dve", oh, ge[:, :, :, 0:16], ge[:, :, :, 1:17], ALU.subtract, ["ge"], ["oh"])
            P.op("dve", lambda e: e.tensor_reduce(out=af[:], in_=ge[:, :, :, 1:17], axis=AX.X, op=ALU.add), ["ge"], ["af"])
            yield
            tt(P, "dve", oh, oh, i16fv[:, :, 0, :].unsqueeze(2).to_broadcast(B4), ALU.mult, ["oh", "i16f"], ["oh"])
            P.op("dve", lambda e: e.tensor_reduce(out=i1s[:], in_=oh, axis=AX.X, op=ALU.add), ["oh"], ["i1s"])
            ts(P, "dve", bf_[:], af[:], -16.0, None, ALU.mult, None, ["af"], ["bf"])
            tt(P, "dve", bf_[:], bf_[:], posf[:], ALU.add, ["bf", "posf"], ["bf"])
            yield
            tt(P, "dve", oh, iota16[:].unsqueeze(1).unsqueeze(1).to_broadcast(B4), bf_[:].unsqueeze(3).to_broadcast(B4),
               ALU.is_equal, ["iota16", "bf", "i1s"], ["oh"])
            tt(P, "dve", oh, oh, i16fv[:, :, 1, :].unsqueeze(2).to_broadcast(B4), ALU.mult, ["oh", "i16f"], ["oh"])
            P.op("dve", lambda e: e.tensor_reduce(out=i2s[:], in_=oh, axis=AX.X, op=ALU.add), ["oh"], ["i2s"])
            yield
            ts(P, "dve", i1s[:], i1s[:], float(NK), None, ALU.mult, None, ["i1s"], ["i1s"])
            tt(P, "dve", i1s[:], i1s[:], i2s[:], ALU.add, ["i1s", "i2s"], ["i1s"])
            cp(P, "dve", eidx[par][:], i1s[:].rearrange("p h k -> p (h k)"), ["i1s"], [EI])
            tt(P, "dve", ee[:], sc[:], sc[:, :, 0:1].to_broadcast([128, PH, 16]), ALU.subtract, scall, ["ee"])
            yield
            act(P, ee[:], ee[:], AF.Exp, ["ee"], ["ee"])
            yield
            P.op("dve", lambda e: e.tensor_reduce(out=zz[:], in_=ee[:], axis=AX.X, op=ALU.add), ["ee"], ["zz"])
            P.op("dve", lambda e: e.reciprocal(out=zz[:], in_=zz[:]), ["zz"], ["zz"])
            tt(P, "dve", gg[par][:].rearrange("p (h k) -> p h k", k=16), ee[:], zz[:].unsqueeze(2).to_broadcast([128, PH, 16]),
               ALU.mult, ["ee", "zz"], [GG])
            yield

        def advance(gen, n):
            if gen is None:
                return None
            for _ in range(n):
                try:
                    next(gen)
                except StopIteration:
                    return None
            return gen

        gen = prologue(0)
        while gen is not None:
            gen = advance(gen, 1)
        br, pr = Rot(NB_G), Rot(4)
        NGRP = NSL // G
        gbuf = {}
        ngath = [0]

        def names(t):
            par = t % 2
            return par, f"x1t{t % 3}", f"hn2b{par}", f"eidx{par}", f"gg{par}"

        def stage_a(t, gi):
            par, X, HB, EI, GG = names(t)
            s0 = gi * G
            bi = br.next()
            gbuf[(t, gi)] = bi
            for j in reversed(range(G)):
                s = s0 + j
                ngath[0] += 1
                P.dma("pool", (lambda bi, j, s, par: lambda e: e.indirect_dma_start(
                    out=uvg[bi][:, j, :, :].rearrange("p s d -> p (s d)"), out_offset=None, in_=uv_v,
                    in_offset=bass.IndirectOffsetOnAxis(ap=eidx[par][:, s:s + 1], axis=0)))(bi, j, s, par),
                    [EI], [f"uvg{bi}_{j}"], reuse_wait=(ngath[0] <= 2 * NB_G * G))
            for j in range(G):
                s = s0 + j
                pi = pr.next()
                tt(P, "dve", prodb[pi][:], uvg[bi][:, j, 0, :], hn2b[par][:], ALU.mult, [f"uvg{bi}_{j}", HB], [f"prodb{pi}"])
                if s % 2 == 1:
                    for cc in range(8):
                        mm(P, fold_ps[:, 0:128], identb[:], prodb[pi][:, cc * 128:(cc + 1) * 128], cc == 0, cc == 7,
                           ["identb", f"prodb{pi}"], ["fold"])
                    act(P, junks[:], fold_ps[:, 0:128], AF.Identity, ["fold"], ["junks", f"adot{s}"], accum_out=adot[:, s:s + 1])
                else:
                    act(P, junkb[:], prodb[pi][:], AF.Identity, [f"prodb{pi}"], ["junkb", f"adot{s}"], accum_out=adot[:, s:s + 1])

        def stage_b(t, gi):
            s0 = gi * G
            adg = [f"adot{s0 + j}" for j in range(G)]
            act(P, ga[:, s0:s0 + G], adot[:, s0:s0 + G], AF.Gelu, adg, [f"ga{s0}"])

        def stage_c(t, gi):
            par, X, HB, EI, GG = names(t)
            s0 = gi * G
            tt(P, "dve", cw[:, s0:s0 + G], ga[:, s0:s0 + G], gg[par][:, s0:s0 + G], ALU.mult, [f"ga{s0}", GG], [f"cw{s0}"])

        def stage_d(t, gi):
            par, X, HB, EI, GG = names(t)
            yp = y_ps[par]
            s0 = gi * G
            bi = gbuf.pop((t, gi))
            for j in range(G):
                s = s0 + j
                di = s % 8
                act(P, dgc[di][:], identb[:], AF.Copy, ["identb", f"cw{s0}"], [f"dgc{di}"], scale=cw[:, s:s + 1])
            for j in range(G):
                s = s0 + j
                di = s % 8
                for half in range(2):
                    mm(P, yp[half][:], dgc[di][:], uvg[bi][:, j, 1, half * 512:(half + 1) * 512], s == 0, s == NSL - 1,
                       [f"dgc{di}", f"uvg{bi}_{j}"], [f"y{par}{half}"])

        def finalize(t):
            par, X, HB, EI, GG = names(t)
            xp = t % 3
            yp = y_ps[par]
            for half in range(2):
                hsl = slice(half * 512, (half + 1) * 512)
                tt(P, "dve", x2[:, hsl], x1t[xp][:, hsl], yp[half][:], ALU.add, [X, f"y{par}{half}"], [f"x2_{half}"])
            yield
            act(P, junkb[:], x2[:], AF.Square, ["x2_0", "x2_1"], ["junkb", "fss"], accum_out=fss[:])
            yield
            act(P, fstd[:], fss[:], AF.Sqrt, ["fss", "epsc"], ["fstd"], bias=epsc[:], scale=1.0 / D)
            yield
            P.op("dve", lambda e: e.reciprocal(out=frstd[:], in_=fstd[:]), ["fstd"], ["frstd"])
            yield
            act(P, ot[:], x2[:], AF.Copy, ["x2_0", "x2_1", "frstd"], ["ot"], scale=frstd[:])
            yield
            tt(P, "dve", ot[:], ot[:], gfrep[:], ALU.mult, ["ot", "gfrep"], ["ot"])
            yield
            dma(P, "sp", out_v[t], ot[:], ["ot"], ["out"])
            yield

        units = [(t, gi) for t in range(NT) for gi in range(NGRP)]
        NU = len(units)
        gen = None
        fgen = None
        for n in range(NU + 3):
            if n < NU:
                t, gi = units[n]
                if gi == 0 and gen is not None:
                    while gen is not None:
                        gen = advance(gen, 1)
                stage_a(t, gi)
            if 1 <= n <= NU:
                stage_b(*units[n - 1])
            if 2 <= n <= NU + 1:
                stage_c(*units[n - 2])
            if n >= 3:
                tc_, gc_ = units[n - 3]
                stage_d(tc_, gc_)
                if gc_ == NGRP - 1:
                    while fgen is not None:
                        fgen = advance(fgen, 1)
                    fgen = finalize(tc_)
            fgen = advance(fgen, 1)
            if n < NU:
                t, gi = units[n]
                if gi == 3 and t + 1 < NT:
                    gen = prologue(t + 1)
                gen = advance(gen, 1)
                if gi == NGRP - 1:
                    while gen is not None:
                        gen = advance(gen, 1)
        while fgen is not None:
            fgen = advance(fgen, 1)
        P.barrier()
        P.emit()


def build_all(nc, P, c, upto="d"):
    phase_a(nc, P, c)
    if upto < "b":
        return
    with ExitStack() as wsc:
        wts = alloc_c_weights(nc, wsc)
        phase_b(nc, P, c, preload=lambda: load_c_weights(nc, P, c, wts, None))
        if upto >= "c":
            phase_c(nc, P, c, wts)
    if upto >= "d":
        phase_d(nc, P, c)


def build_program(S):
    nc = bass.Bass("TRN2", target_bir_lowering=False)
    c = declare_io(nc, S, debug=False)
    with ExitStack() as st:
        P = Prog(nc, st)
        build_all(nc, P, c)
    return nc


def kernel(**inputs):
    x = np.asarray(inputs["x"], dtype=np.float32)
    B, S, _ = x.shape
    assert B == 8
    nc = build_program(S)
    consts = make_consts(S)
    w = {k: np.asarray(v) for k, v in inputs.items() if k != "x"}
    shared = host_inputs(S, x[0], w, consts)
    in_maps = []
    for b in range(B):
        m = dict(shared)
        m["x"] = np.ascontiguousarray(x[b])
        in_maps.append(m)
    res = run_bass_kernel_spmd(nc, in_maps, core_ids=list(range(B)))
    out = np.stack([np.asarray(res.results[b]["out"], dtype=np.float32) for b in range(B)], axis=0)
    return out
```

```python
import numpy as np
import ml_dtypes
from contextlib import ExitStack
import concourse.bass as bass
import concourse.mybir as mybir
from concourse.bass_utils import run_bass_kernel_spmd

F32 = mybir.dt.float32
BF16 = mybir.dt.bfloat16
U32 = mybir.dt.uint32
I32 = mybir.dt.int32
ALU = mybir.AluOpType
AF = mybir.ActivationFunctionType
AX = mybir.AxisListType

D = 1024
NCOL = 4608
NH = 8
DH = 64
CC = 512
CW = 31
MB = 256
PH = 8
NK = 128
TOPK = 16
EPS = 1e-6
BIG = 30000.0
NEG = -1.0e30


class Prog:
    ENG = ("pe", "dve", "act", "pool", "sp")
    DQ = ("sp", "pool", "act")

    def __init__(self, nc, stack, kdma=8):
        self.nc = nc
        self.eng = {"pe": nc.tensor, "dve": nc.vector, "act": nc.scalar, "pool": nc.gpsimd, "sp": nc.sync}
        self.sems = {}
        for e in self.ENG:
            self.sems[("c", e)] = stack.enter_context(nc.semaphore(f"c_{e}"))
        self.kdma = {"sp": kdma, "pool": 16, "act": 4}
        for q in self.DQ:
            for j in range(self.kdma[q]):
                self.sems[("d", q, j)] = stack.enter_context(nc.semaphore(f"d_{q}{j}"))
        self.latest = {k: 0 for k in self.sems}
        self.dnext = {q: 0 for q in self.DQ}
        self.ops = {e: [] for e in self.ENG}
        self.known = {e: {} for e in self.ENG}
        self.lastw = {}
        self.readers = {}
        self.nins = 0

    def _need(self, eng, key, val):
        if self.known[eng].get(key, 0) < val:
            self.known[eng][key] = val
            self.ops[eng].append(("w", key, val))

    def _deps(self, eng, reads, writes, is_dma):
        for b in reads:
            lw = self.lastw.get(b)
            if lw is not None:
                key, val = lw
                if (not is_dma) and key == ("c", eng) and eng == "pe":
                    continue
                self._need(eng, key, val)
        for b in writes:
            lw = self.lastw.get(b)
            if lw is not None:
                key, val = lw
                if is_dma or key != ("c", eng):
                    self._need(eng, key, val)
            for key, val in self.readers.get(b, {}).items():
                if is_dma or key != ("c", eng):
                    self._need(eng, key, val)

    def _commit(self, key, val, reads, writes):
        for b in writes:
            self.lastw[b] = (key, val)
            self.readers[b] = {}
        for b in reads:
            r = self.readers.setdefault(b, {})
            r[key] = max(r.get(key, 0), val)

    def op(self, eng, fn, reads=(), writes=()):
        self._deps(eng, reads, writes, False)
        key = ("c", eng)
        self.latest[key] += 1
        val = self.latest[key]
        self.ops[eng].append(("i", fn, key, 1))
        self._commit(key, val, reads, writes)
        self.nins += 1

    def dma(self, q, fn, reads=(), writes=()):
        self._deps(q, reads, writes, True)
        j = self.dnext[q]
        self.dnext[q] = (j + 1) % self.kdma[q]
        key = ("d", q, j)
        if self.latest[key] > 0:
            self._need(q, key, self.latest[key])
        self.latest[key] += 16
        val = self.latest[key]
        self.ops[q].append(("i", fn, key, 16))
        self._commit(key, val, reads, writes)
        self.nins += 1

    def barrier(self):
        for e in self.ENG:
            for key, val in self.latest.items():
                if val > 0:
                    self._need(e, key, val)
        self.lastw = {}
        self.readers = {}

    def emit(self, name=None):
        ops = self.ops
        self.ops = {e: [] for e in self.ENG}
        sems = self.sems
        nc = self.nc

        def replay(lst):
            def f(e):
                for it in lst:
                    if it[0] == "w":
                        e.wait_ge(sems[it[1]], it[2])
                    else:
                        ins = it[1](e)
                        ins.then_inc(sems[it[2]], it[3])
            return f

        with nc.Block() as block:
            block.tensor(replay(ops["pe"]))
            block.vector(replay(ops["dve"]))
            block.scalar(replay(ops["act"]))
            block.gpsimd(replay(ops["pool"]))
            block.sync(replay(ops["sp"]))


class Rot:
    def __init__(self, n):
        self.n = n
        self.i = -1

    def next(self):
        self.i = (self.i + 1) % self.n
        return self.i


def _bf(a):
    return np.ascontiguousarray(a).astype(ml_dtypes.bfloat16)


def make_consts(S):
    NT = S // 128
    NB = S // MB
    c = {}
    c["ident_bf"] = _bf(np.eye(128, dtype=np.float32))
    c["ident_f"] = np.eye(128, dtype=np.float32)
    ka = np.zeros((18, S), np.float32)
    ka[0, :] = 1.0
    for n in range(NB):
        ka[1 + n, n * MB:(n + 1) * MB] = 1.0
    ka[17, :] = ((np.arange(S) // 128) % 2).astype(np.float32)
    c["kaug_static"] = _bf(ka)
    mt = np.zeros((4, 128, 512), np.float32)
    for i in range(4):
        for j in range(4):
            if i // 2 != j // 2:
                continue
            blk = mt[i, :, j * 128:(j + 1) * 128]
            if i > j:
                blk[:] = -BIG
            elif i == j:
                kk = np.arange(128)[:, None]
                qq = np.arange(128)[None, :]
                blk[kk > qq] = -BIG
    c["masktri"] = _bf(mt)
    tile = np.arange(NT)[:, None]
    n = np.arange(16)[None, :]
    valid = (n < tile // 2).astype(np.float32)
    own = (n == tile // 2).astype(np.float32)
    c["valid01"] = np.broadcast_to(valid[None], (128, NT, 16)).astype(np.float32).copy()
    c["own01"] = np.broadcast_to(own[None], (128, NT, 16)).astype(np.float32).copy()
    c["maskv"] = ((c["valid01"] - 1.0) * 1.0e30).astype(np.float32)
    slopes = np.array([2.0 ** (-8.0 * (i + 1) / NH) for i in range(NH)], np.float32)
    stat = np.zeros((NH, 128, NT, 16), np.float32)
    for h in range(NH):
        st = -slopes[h] * (128.0 * tile - 256.0 * n)
        st = np.where(n <= tile // 2, st, 0.0)
        stat[h] = st[None]
    c["stat"] = stat
    p = np.arange(128, dtype=np.float32)
    c["qlo"] = _bf(np.stack([-slopes[h] * p for h in range(NH)], axis=1))
    c["qhi"] = _bf(np.stack([128.0 * slopes[h] * np.ones(128, np.float32) for h in range(NH)], axis=1))
    kb = np.zeros((128, NH, 2), np.float32)
    for h in range(NH):
        for half in range(2):
            kb[:, h, half] = slopes[h] * (p + 128.0 * half)
    c["kbias"] = kb
    c["iota16"] = np.broadcast_to(np.arange(16, dtype=np.float32)[None], (128, 16)).copy()
    c["epsc"] = np.full((128, 1), EPS, np.float32)
    return c


def act(P, out, in_, func, r, w, **kw):
    P.op("act", lambda e: e.activation(out=out, in_=in_, func=func, **kw), r, w)


def tt(P, eng, out, in0, in1, op, r, w):
    P.op(eng, lambda e: e.tensor_tensor(out=out, in0=in0, in1=in1, op=op), r, w)


def ts(P, eng, out, in0, s1, s2, op0, op1, r, w, **kw):
    if s2 is None:
        P.op(eng, lambda e: e.tensor_scalar(out=out, in0=in0, scalar1=s1, scalar2=None, op0=op0, **kw), r, w)
    else:
        P.op(eng, lambda e: e.tensor_scalar(out=out, in0=in0, scalar1=s1, scalar2=s2, op0=op0, op1=op1, **kw), r, w)


def cp(P, eng, out, in_, r, w):
    if eng == "act":
        P.op("act", lambda e: e.copy(out=out, in_=in_), r, w)
    else:
        P.op(eng, lambda e: e.tensor_copy(out=out, in_=in_), r, w)


def mm(P, out, lhsT, rhs, start, stop, r, w):
    P.op("pe", lambda e: e.matmul(out, lhsT, rhs, start=start, stop=stop), r, w)


def tr(P, out, in_, ident, r, w):
    P.op("pe", lambda e: e.transpose(out, in_, ident), r, w)


def dma(P, q, out, in_, r, w):
    P.dma(q, lambda e: e.dma_start(out=out, in_=in_), r, w)


def rmsnorm_stats(P, xt, junk, ss, std, rstd, epsc, tag):
    act(P, junk, xt, AF.Square, [tag + "x"], [tag + "junk", tag + "ss"], accum_out=ss)
    act(P, std, ss, AF.Sqrt, [tag + "ss", "epsc"], [tag + "std"], bias=epsc, scale=1.0 / D)
    P.op("dve", lambda e: e.reciprocal(out=rstd, in_=std), [tag + "std"], [tag + "rstd"])


class Ctx:
    pass


def declare_io(nc, S, debug=False):
    c = Ctx()
    c.S = S

    def inp(name, shape, dt=F32):
        return nc.dram_tensor(name, list(shape), dt, kind="ExternalInput").ap()

    def scr(name, shape, dt):
        return nc.dram_tensor(name, list(shape), dt, kind="ExternalOutput" if debug else "Internal").ap()

    NT = S // 128
    c.x = inp("x", [S, D])
    c.w_in = inp("w_in", [D, NCOL])
    c.g1c = inp("g1c", [128, 8])
    c.conv_wT = inp("conv_wT", [128, 4, CW])
    c.conv_bc = inp("conv_bc", [128, 4])
    c.ln_gc = inp("ln_gc", [128, 4])
    c.ln_bc = inp("ln_bc", [128, 4])
    c.b_pwc = inp("b_pwc", [128, 8])
    c.w_pw = inp("w_conv_pw", [CC, D])
    c.w_ao = inp("w_attn_out", [NH * DH, D])
    c.w_out = inp("w_out", [D, D])
    c.w_pq = inp("w_peer_q", [D, PH * 256])
    c.g2rep = inp("g2rep", [128, D])
    c.gfrep = inp("gfrep", [128, D])
    c.keys1T = inp("keys1T", [128, NK])
    c.keys2T = inp("keys2T", [128, NK])
    c.peer_u = inp("peer_u", [NK * NK, D])
    c.peer_v = inp("peer_v", [NK * NK, D])
    c.ident_bf = inp("ident_bf", [128, 128], BF16)
    c.ident_f = inp("ident_f", [128, 128])
    c.kaug_static = inp("kaug_static", [18, S], BF16)
    c.qhi = inp("qhi", [128, NH], BF16)
    c.masktri = inp("masktri", [4, 128, 512], BF16)
    c.valid01 = inp("valid01", [128, NT, 16])
    c.own01 = inp("own01", [128, NT, 16])
    c.maskv = inp("maskv", [128, NT, 16])
    c.stat = inp("stat", [NH, 128, NT, 16])
    c.qlo = inp("qlo", [128, NH], BF16)
    c.kbias = inp("kbias", [128, NH, 2])
    c.iota16 = inp("iota16", [128, 16])
    c.epsc = inp("epsc", [128, 1])
    c.out = nc.dram_tensor("out", [S, D], F32, kind="ExternalOutput").ap()
    c.zcT = scr("s_zcT", [CC, S], BF16)
    c.qT = scr("s_qT", [NH * DH, S], BF16)
    c.kT = scr("s_kT", [NH * DH, S], BF16)
    c.vS = scr("s_v", [S, NH * DH], BF16)
    c.sgc = scr("s_sgc", [D, S], BF16)
    c.sga = scr("s_sga", [D, S], BF16)
    c.oT = scr("s_oT", [NH * DH, S], BF16)
    c.x1 = scr("s_x1", [S, D], F32)
    c.uv = nc.dram_tensor("s_uv", [NK * NK, 2, D], BF16, kind="Internal").ap()
    c.dbg = scr("s_dbg", [4, 128, 2048], F32) if debug else None
    return c


def phase_a(nc, P, c):
    S = c.S
    NBLK = S // 512
    with ExitStack() as st:
        sb = lambda name, shape, dt: st.enter_context(nc.sbuf_tensor(name, list(shape), dt))
        ps = lambda name, shape, dt: st.enter_context(nc.psum_tensor(name, list(shape), dt))
        wbf = sb("a_wbf", [128, 8, NCOL], BF16)
        wst = [sb(f"a_wst{i}", [128, NCOL], F32) for i in range(2)]
        g1c = sb("a_g1c", [128, 8], F32)
        epsc = sb("a_eps", [128, 1], F32)
        ident = sb("a_ident", [128, 128], BF16)
        xt = [sb(f"a_xt{i}", [128, D], F32) for i in range(3)]
        junk = sb("a_junk", [128, D], F32)
        hn = [sb(f"a_hn{i}", [128, D], BF16) for i in range(2)]
        ss = [sb(f"a_ss{i}", [128, 1], F32) for i in range(2)]
        std = [sb(f"a_std{i}", [128, 1], F32) for i in range(2)]
        rstd = [sb(f"a_rstd{i}", [128, 1], F32) for i in range(2)]
        hnT = [sb(f"a_hnT{i}", [128, 8, 512], BF16) for i in range(2)]
        sig = [sb(f"a_sig{i}", [128, 512], F32) for i in range(2)]
        stg = [sb(f"a_stg{i}", [128, 512], BF16) for i in range(8)]
        tp = ps("a_tp", [128, 8, 128], BF16)
        acc = [ps(f"a_acc{i}", [128, 512], F32) for i in range(6)]

        dma(P, "sp", g1c[:], c.g1c, [], ["g1c"])
        dma(P, "sp", epsc[:], c.epsc, [], ["epsc"])
        dma(P, "sp", ident[:], c.ident_bf, [], ["ident"])
        w_in_v = c.w_in.rearrange("(k p) n -> k p n", p=128)
        for k in range(8):
            dma(P, "sp", wst[k % 2][:], w_in_v[k], [], [f"wst{k%2}"])
            act(P, wbf[:, k, :], wst[k % 2][:], AF.Copy, [f"wst{k%2}", "g1c"], [f"wbf{k}"], scale=g1c[:, k:k + 1])
        wall = [f"wbf{k}" for k in range(8)]

        xr, hr, sr, ar, gr = Rot(3), Rot(2), Rot(2), Rot(6), Rot(8)

        CH = 1024
        conv_list = [(ti, r0) for r0 in range(0, NK * NK, CH) for ti in range(2)]
        nstores = [0]
        per = max(1, (NBLK * 36) // len(conv_list))

        def conv_tick(force=False):
            nstores[0] += 1
            if conv_list and (force or nstores[0] % per == 0):
                ti, r0 = conv_list.pop(0)
                tab = (c.peer_u, c.peer_v)[ti]
                dma(P, "pool", c.uv[r0:r0 + CH, ti, :], tab[r0:r0 + CH, :], [], [])
        x_v = c.x.rearrange("(t p) d -> t p d", p=128)

        def store(dst, src_ps, tok, kind, scale=None):
            g = gr.next()
            npart = dst.shape[0]
            if kind == "sigmoid":
                act(P, stg[g][0:npart, :], src_ps, AF.Sigmoid, [tok], [f"stg{g}"])
            elif kind == "scale":
                act(P, stg[g][0:npart, :], src_ps, AF.Copy, [tok], [f"stg{g}"], scale=scale)
            else:
                cp(P, "dve", stg[g][0:npart, :], src_ps, [tok], [f"stg{g}"])
            dma(P, "pool", dst, stg[g][0:npart, :], [f"stg{g}"], [])
            conv_tick()

        for b in range(NBLK):
            hb = hr.next()
            for j in range(4):
                t = b * 4 + j
                xi = xr.next()
                si = sr.next()
                dma(P, "sp", xt[xi][:], x_v[t], [], [f"xt{xi}x"])
                rmsnorm_stats(P, xt[xi][:], junk[:], ss[si][:], std[si][:], rstd[si][:], epsc[:], f"xt{xi}")
                act(P, hn[si][:], xt[xi][:], AF.Copy, [f"xt{xi}x", f"xt{xi}rstd"], [f"hn{si}"], scale=rstd[si][:])
                for k in range(8):
                    tr(P, tp[:, k, :], hn[si][:, k * 128:(k + 1) * 128], ident[:], [f"hn{si}", "ident"], ["tp"])
                cp(P, "dve", hnT[hb][:, :, j * 128:(j + 1) * 128], tp[:], ["tp"], [f"hnT{hb}"])
            cols = slice(b * 512, (b + 1) * 512)

            def proj(c0, m):
                a = ar.next()
                for k in range(8):
                    mm(P, acc[a][0:m, :], wbf[:, k, c0:c0 + m], hnT[hb][:, k, :], k == 0, k == 7,
                       wall + [f"hnT{hb}"], [f"acc{a}"])
                return acc[a], f"acc{a}"

            for cc in range(4):
                pa, ta = proj(cc * 128, 128)
                pg, tg = proj(CC + cc * 128, 128)
                s_i = sr.next()
                act(P, sig[s_i][:], pg[:], AF.Sigmoid, [tg], [f"sig{s_i}"])
                g = gr.next()
                tt(P, "dve", stg[g][:], pa[:], sig[s_i][:], ALU.mult, [ta, f"sig{s_i}"], [f"stg{g}"])
                dma(P, "pool", c.zcT[cc * 128:(cc + 1) * 128, cols], stg[g][:], [f"stg{g}"], [])
                conv_tick()
            for cc in range(4):
                pq, tq = proj(2 * CC + cc * 128, 128)
                store(c.qT[cc * 128:(cc + 1) * 128, cols], pq[:], tq, "scale", scale=DH ** -0.5)
            for cc in range(4):
                pk, tk = proj(2 * CC + 512 + cc * 128, 128)
                store(c.kT[cc * 128:(cc + 1) * 128, cols], pk[:], tk, "copy")
            for cc in range(8):
                pg, tg = proj(2 * CC + 1536 + cc * 128, 128)
                store(c.sgc[cc * 128:(cc + 1) * 128, cols], pg[:], tg, "sigmoid")
            for cc in range(8):
                pg, tg = proj(2 * CC + 1536 + D + cc * 128, 128)
                store(c.sga[cc * 128:(cc + 1) * 128, cols], pg[:], tg, "sigmoid")
            for j in range(4):
                a = ar.next()
                for k in range(8):
                    mm(P, acc[a][:], hnT[hb][:, k, j * 128:(j + 1) * 128], wbf[:, k, 2 * CC + 1024:2 * CC + 1536],
                       k == 0, k == 7, wall + [f"hnT{hb}"], [f"acc{a}"])
                t = b * 4 + j
                store(c.vS[t * 128:(t + 1) * 128, :], acc[a][:], f"acc{a}", "copy")
        while conv_list:
            conv_tick(force=True)
        P.barrier()
        P.emit()


def host_inputs(S, x_b, w, consts):
    f = lambda a: np.ascontiguousarray(np.asarray(a, dtype=np.float32))
    col = lambda v, n: f(np.asarray(v).reshape(n, 128).T)
    m = {
        "x": f(x_b),
        "w_in": f(w["w_in"][0]),
        "g1c": col(w["g_norm1"][0], 8),
        "conv_wT": f(np.asarray(w["conv_w"][0]).reshape(CW, 4, 128).transpose(2, 1, 0)),
        "conv_bc": col(w["conv_b"][0], 4),
        "ln_gc": col(w["conv_ln_g"][0], 4),
        "ln_bc": col(w["conv_ln_b"][0], 4),
        "b_pwc": col(w["b_conv_pw"][0], 8),
        "w_conv_pw": f(w["w_conv_pw"][0]),
        "w_attn_out": f(w["w_attn_out"][0]),
        "w_out": f(w["w_out"][0]),
        "w_peer_q": f(w["w_peer_q"][0]),
        "g2rep": f(np.broadcast_to(np.asarray(w["g_norm2"][0])[None, :], (128, D))),
        "gfrep": f(np.broadcast_to(np.asarray(w["g_final"])[None, :], (128, D))),
        "keys1T": f(np.asarray(w["peer_keys1"][0]).T),
        "keys2T": f(np.asarray(w["peer_keys2"][0]).T),
        "peer_u": f(w["peer_u"][0]),
        "peer_v": f(w["peer_v"][0]),
    }
    m.update(consts)
    return m


def phase_b(nc, P, c, preload=None):
    S = c.S
    NT = S // 128
    NQB = S // 512
    with ExitStack() as st:
        sb = lambda name, shape, dt: st.enter_context(nc.sbuf_tensor(name, list(shape), dt))
        ps = lambda name, shape, dt: st.enter_context(nc.psum_tensor(name, list(shape), dt))
        kaug = [sb(f"b_kaug{i}", [128, S], BF16) for i in range(2)]
        qaug = [sb(f"b_qaug{i}", [128, S], BF16) for i in range(2)]
        vall = sb("b_vall", [128, NT, NH * DH], BF16)
        vaug = [sb(f"b_vaug{i}", [128, NT, DH + 1], BF16) for i in range(2)]
        augtok = sb("b_augtok", [128, NT, 82], BF16)
        valid01 = sb("b_valid", [128, NT, 16], F32)
        own01 = sb("b_own", [128, NT, 16], F32)
        maskv = sb("b_maskv", [128, NT, 16], F32)
        stat = [sb(f"b_stat{i}", [128, NT, 16], F32) for i in range(2)]
        kbias = sb("b_kbias", [128, NH, 2], F32)
        qlo = sb("b_qlo", [128, NH], BF16)
        qhi = sb("b_qhi", [128, NH], BF16)
        masktri = sb("b_masktri", [128, 4, 512], BF16)
        ident = sb("b_ident", [128, 128], BF16)
        onesf = sb("b_onesf", [128, 64], F32)
        kms = sb("b_kms", [128, 16], F32)
        kmb = [sb(f"b_kmb{i}", [128, 16], BF16) for i in range(2)]
        bsm = sb("b_bsm", [128, NT, 16], F32)
        m8 = sb("b_m8", [128, NT, 8], F32)
        sel = sb("b_sel", [128, NT, 16], F32)
        PT = [sb(f"b_PT{i}", [128, 2, 512], BF16) for i in range(3)]
        rden = [sb(f"b_rden{i}", [128, 512], F32) for i in range(2)]
        bc_sb = sb("b_bcsb", [128, 512], F32)
        oT_sb = [sb(f"b_oT{i}", [128, 512], BF16) for i in range(2)]
        st_ps = [ps(f"b_st{i}", [128, 2, 512], F32) for i in range(2)]
        o_ps = [ps(f"b_o{i}", [128, 512], F32) for i in range(2)]
        shps = ps("b_sh", [128, 512], F32)
        bs_ps = shps[:, 0:NT * 16].rearrange("p (t n) -> p t n", n=16)
        tp = shps[:].bitcast(BF16).rearrange("p (a b) -> p a b", b=128)
        bc_ps = ps("b_bc", [128, 512], F32)

        for i in range(2):
            dma(P, "sp", kaug[i][64:82, :], c.kaug_static, [], [f"kaug_s{i}"])
        dma(P, "sp", vall[:], c.vS.rearrange("(t p) n -> p t n", p=128), [], ["vall"])
        dma(P, "sp", valid01[:], c.valid01, [], ["valid01"])
        dma(P, "sp", own01[:], c.own01, [], ["own01"])
        dma(P, "sp", maskv[:], c.maskv, [], ["maskv"])
        dma(P, "sp", kbias[:], c.kbias, [], ["kbias"])
        dma(P, "sp", qlo[:], c.qlo, [], ["qlo"])
        dma(P, "sp", qhi[:], c.qhi, [], ["qhi"])
        dma(P, "sp", masktri[:], c.masktri.rearrange("i p n -> p i n"), [], ["masktri"])
        dma(P, "sp", ident[:], c.ident_bf, [], ["ident"])
        P.op("dve", lambda e: e.memset(onesf[:], 1.0), [], ["onesf"])
        P.op("dve", lambda e: e.memset(augtok[:], 0.0), [], ["augtok"])
        for i in range(2):
            P.op("dve", (lambda i: lambda e: e.memset(vaug[i][:], 1.0))(i), [], [f"vaug{i}"])

        def prologue(h):
            hp = h % 2
            hs = slice(h * DH, (h + 1) * DH)
            KA, QA, VA, ST, KM = f"kaug{hp}", f"qaug{hp}", f"vaug{hp}", f"stat{hp}", f"kmb{hp}"
            dma(P, "sp", kaug[hp][0:64, :], c.kT[hs, :], [], [KA])
            dma(P, "sp", qaug[hp][0:64, :], c.qT[hs, :], [], [QA])
            dma(P, "sp", stat[hp][:], c.stat[h], [], [ST])
            cp(P, "pool", vaug[hp][:, :, 0:DH], vall[:, :, hs], ["vall"], [VA])
            yield
            P.op("dve", lambda e: e.tensor_reduce(out=kms[0:64, 0:S // MB], in_=kaug[hp][0:64, :].rearrange("p (n k) -> p n k", k=MB),
                                                  axis=AX.X, op=ALU.add), [KA], ["kms"])
            P.op("act", lambda e: e.mul(out=kmb[hp][0:64, 0:S // MB], in_=kms[0:64, 0:S // MB], mul=1.0 / MB), ["kms"], [KM])
            if S // MB < 16:
                P.op("dve", lambda e: e.memset(kmb[hp][0:64, S // MB:16], 0.0), [], [KM])
            yield
            for t0 in range(0, NT, 8):
                for t in range(t0, t0 + 8):
                    mm(P, bs_ps[:, t, :], qaug[hp][0:64, t * 128:(t + 1) * 128], kmb[hp][0:64, :], True, True, [QA, KM], ["shps"])
                yield
            tt(P, "dve", bsm[:], bs_ps, maskv[:], ALU.add, ["shps", "maskv"], ["bsm"])
            for t0 in range(0, NT, 8):
                for t in range(t0, t0 + 8):
                    P.op("dve", (lambda t: lambda e: e.max(out=m8[:, t, :], in_=bsm[:, t, :]))(t), ["bsm"], ["m8"])
                yield
            tt(P, "dve", sel[:], bsm[:], m8[:, :, 2:3].to_broadcast([128, NT, 16]), ALU.is_ge, ["bsm", "m8"], ["sel"])
            tt(P, "dve", sel[:], sel[:], valid01[:], ALU.mult, ["sel", "valid01"], ["sel"])
            tt(P, "dve", sel[:], sel[:], own01[:], ALU.add, ["sel", "own01"], ["sel"])
            ts(P, "dve", sel[:], sel[:], -1.0, BIG, ALU.add, ALU.mult, ["sel"], ["sel"])
            tt(P, "dve", augtok[:, :, 65:81], sel[:], stat[hp][:], ALU.add, ["sel", ST], ["augtok"])
            cp(P, "dve", augtok[:, :, 64], qlo[:, h:h + 1].to_broadcast([128, NT]), ["qlo"], ["augtok"])
            cp(P, "dve", augtok[:, :, 81], qhi[:, h:h + 1].to_broadcast([128, NT]), ["qhi"], ["augtok"])
            yield
            for t0 in range(0, NT, 8):
                for j in range(8):
                    tr(P, tp[0:82, j, :], augtok[:, t0 + j, :], ident[:], ["augtok", "ident"], ["shps"])
                cp(P, "dve", qaug[hp][64:82, t0 * 128:(t0 + 8) * 128].rearrange("p (a b) -> p a b", b=128), tp[64:82, :, :],
                   ["shps"], [QA])
                yield

        def advance(gen, n):
            if gen is None:
                return None
            for _ in range(n):
                try:
                    next(gen)
                except StopIteration:
                    return None
            return gen

        sr, pr, orr, osr = Rot(2), Rot(3), Rot(2), Rot(2)
        gen = prologue(0)
        while gen is not None:
            gen = advance(gen, 1)
        if preload is not None:
            preload()
        for h in range(NH):
            hp = h % 2
            hs = slice(h * DH, (h + 1) * DH)
            KA, QA, VA = f"kaug{hp}", f"qaug{hp}", f"vaug{hp}"
            units = [(qb, kp) for qb in range(NQB) for kp in range(2 * qb + 2)]
            NU = len(units)
            sbuf_of = {}
            obuf_of = {}
            pending = []

            def col0(qb, kt):
                return (kt - 4 * qb) * 128 if kt >= 4 * qb else 0

            def emit_s(n):
                qb, kp = units[n]
                si = sr.next()
                sbuf_of[n] = si
                for half in range(2):
                    kt = 2 * kp + half
                    diag = kt >= 4 * qb
                    c0 = col0(qb, kt)
                    mm(P, st_ps[si][:, half, c0:512], kaug[hp][0:82, kt * 128:(kt + 1) * 128],
                       qaug[hp][0:82, qb * 512 + c0:(qb + 1) * 512], True, not diag, [KA, f"kaug_s{hp}", QA], [f"st{si}"])
                    if diag:
                        mm(P, st_ps[si][:, half, c0:c0 + 128], ident[:], masktri[:, kt - 4 * qb, c0:c0 + 128], False, True,
                           ["ident", "masktri"], [f"st{si}"])

            def emit_pv(n):
                qb, kp = units[n]
                nkt = 4 * qb + 4
                si = sbuf_of.pop(n)
                if kp == 0:
                    obuf_of[qb] = orr.next()
                oi = obuf_of[qb]
                pi = pr.next()
                cmin = col0(qb, 2 * kp)
                act(P, PT[pi][:, :, cmin:512], st_ps[si][:, :, cmin:512], AF.Exp, [f"st{si}", "kbias"], [f"PT{pi}"],
                    bias=kbias[:, h, 0:1])
                for half in range(2):
                    kt = 2 * kp + half
                    c0 = col0(qb, kt)
                    mm(P, o_ps[oi][0:65, c0:512], vaug[hp][:, kt, :], PT[pi][:, half, c0:512], kt == 0, kt == nkt - 1,
                       [VA, f"PT{pi}"], [f"o{oi}"])
                if kp == 2 * qb + 1:
                    o = o_ps[oi]
                    ri = qb % 2
                    P.op("dve", lambda e: e.reciprocal(out=rden[ri][64:65, :], in_=o[64:65, :]), [f"o{oi}"], [f"rden{ri}"])

                    def fin2(qb=qb, oi=oi, o=o, ri=ri):
                        mm(P, bc_ps[0:64, :], onesf[64:65, 0:64], rden[ri][64:65, :], True, True, ["onesf", f"rden{ri}"], ["bc_ps"])
                        cp(P, "act", bc_sb[0:64, :], bc_ps[0:64, :], ["bc_ps"], ["bc_sb"])
                        oo = osr.next()
                        tt(P, "dve", oT_sb[oo][0:64, :], o[0:64, :], bc_sb[0:64, :], ALU.mult, [f"o{oi}", "bc_sb"], [f"oT{oo}"])
                        dma(P, "pool", c.oT[hs, qb * 512:(qb + 1) * 512], oT_sb[oo][0:64, :], [f"oT{oo}"], [])
                    pending.append((n + 1, fin2))

            gen = prologue(h + 1) if h + 1 < NH else None
            emit_s(0)
            for n in range(NU):
                if n + 1 < NU:
                    emit_s(n + 1)
                emit_pv(n)
                while pending and pending[0][0] <= n:
                    pending.pop(0)[1]()
                if n >= 4 and n % 2 == 0:
                    gen = advance(gen, 1)
            while pending:
                pending.pop(0)[1]()
            while gen is not None:
                gen = advance(gen, 1)
        P.barrier()
        P.emit()


def alloc_c_weights(nc, st):
    sb = lambda name, shape, dt: st.enter_context(nc.sbuf_tensor(name, list(shape), dt))
    wpw = sb("c_wpw", [128, 4, D], BF16)
    wao = sb("c_wao", [128, NH // 2, D], BF16)
    wout = sb("c_wout", [128, 8, D], BF16)
    dg = sb("c_dg", [128, 4, CW, 128], BF16)
    identf = sb("c_identf", [128, 128], F32)
    convw = sb("c_convw", [128, 4, CW], F32)
    convb = sb("c_convb", [128, 4], F32)
    lng = sb("c_lng", [128, 4], F32)
    lnb = sb("c_lnb", [128, 4], F32)
    bpw = sb("c_bpw", [128, 8], F32)
    epsc = sb("c_eps", [128, 1], F32)
    onesm = sb("c_onesm", [128, 128], F32)
    return (wpw, wao, wout, dg, identf, convw, convb, lng, lnb, bpw, epsc, onesm)


def load_c_weights(nc, P, c, wts, wst):
    (wpw, wao, wout, dg, identf, convw, convb, lng, lnb, bpw, epsc, onesm) = wts
    for name, t, src in (("identf", identf, c.ident_f), ("convw", convw, c.conv_wT), ("convb", convb, c.conv_bc),
                         ("lng", lng, c.ln_gc), ("lnb", lnb, c.ln_bc), ("bpw", bpw, c.b_pwc), ("epsc", epsc, c.epsc)):
        dma(P, "sp", t[:], src, [], [name])
    P.op("dve", lambda e: e.memset(onesm[:], 1.0 / CC), [], ["onesm"])
    dma(P, "pool", wpw[:], c.w_pw.rearrange("(k p) n -> p k n", p=128), [], ["wpw"])
    dma(P, "pool", wao[:], c.w_ao.rearrange("(k p) n -> p k n", p=128), [], ["wao"])
    dma(P, "pool", wout[:], c.w_out.rearrange("(k p) n -> p k n", p=128), [], ["wout"])
    for cc in range(4):
        tt(P, "dve", dg[:, cc, :, :], identf[:].unsqueeze(1).to_broadcast([128, CW, 128]),
           convw[:, cc, :].unsqueeze(2).to_broadcast([128, CW, 128]), ALU.mult, ["identf", "convw"], ["dg"])


def phase_c(nc, P, c, wts):
    S = c.S
    NBLK = S // 512
    HALO = CW - 1
    with ExitStack() as st:
        sb = lambda name, shape, dt: st.enter_context(nc.sbuf_tensor(name, list(shape), dt))
        ps = lambda name, shape, dt: st.enter_context(nc.psum_tensor(name, list(shape), dt))
        (wpw, wao, wout, dg, identf, convw, convb, lng, lnb, bpw, epsc, onesm) = wts
        zcb = [sb(f"c_zcb{i}", [128, 4, 512 + HALO], BF16) for i in range(2)]
        zconv = sb("c_zconv", [128, 4, 512], F32)
        zsq = sb("c_zsq", [128, 4, 512], F32)
        mean_sb = sb("c_mean", [128, 512], F32)
        msq = sb("c_msq", [128, 512], F32)
        var = sb("c_var", [128, 512], F32)
        rstd = sb("c_rstd", [128, 512], F32)
        t1 = [sb(f"c_t1{i}", [128, 512], F32) for i in range(2)]
        zact = sb("c_zact", [128, 4, 512], BF16)
        oTb = [sb(f"c_oTb{i}", [128, NH // 2, 512], BF16) for i in range(2)]
        sgcb = [sb(f"c_sgcb{i}", [128, 8, 512], BF16) for i in range(2)]
        sgab = [sb(f"c_sgab{i}", [128, 8, 512], BF16) for i in range(2)]
        mixc = [sb(f"c_mixc{i}", [128, 512], F32) for i in range(2)]
        mixa = [sb(f"c_mixa{i}", [128, 512], F32) for i in range(2)]
        mix = sb("c_mix", [128, 8, 512], BF16)
        xt = [sb(f"c_xt{i}", [128, D], F32) for i in range(2)]
        x1t = [sb(f"c_x1t{i}", [128, D], F32) for i in range(2)]
        cv_ps = [ps(f"c_cv{i}", [128, 512], F32) for i in range(2)]
        mean_ps = ps("c_meanps", [128, 512], F32)
        ex2_ps = ps("c_ex2ps", [128, 512], F32)
        y_ps = [ps(f"c_y{i}", [128, 512], F32) for i in range(2)]
        o_ps = [ps(f"c_op{i}", [128, 512], F32) for i in range(2)]

        zc_v = c.zcT.rearrange("(c p) s -> p c s", p=128)
        sgc_v = c.sgc.rearrange("(f p) s -> p f s", p=128)
        sga_v = c.sga.rearrange("(f p) s -> p f s", p=128)
        oT_v = c.oT.rearrange("(h d) s -> d h s", d=2 * DH)
        x_v = c.x.rearrange("(t p) d -> t p d", p=128)
        x1_v = c.x1.rearrange("(t p) d -> t p d", p=128)
        cr, tr1, yr, opr, xr, mr = Rot(2), Rot(2), Rot(2), Rot(2), Rot(2), Rot(2)
        for b in range(NBLK):
            bi = b % 2
            cols = slice(b * 512, (b + 1) * 512)
            if b == 0:
                P.op("dve", lambda e: e.memset(zcb[0][:, :, 0:HALO], 0.0), [], ["zcb0"])
                dma(P, "sp", zcb[0][:, :, HALO:], zc_v[:, :, 0:512], [], ["zcb0"])
            else:
                dma(P, "sp", zcb[bi][:], zc_v[:, :, b * 512 - HALO:(b + 1) * 512], [], [f"zcb{bi}"])
            dma(P, "sp", sgcb[bi][:], sgc_v[:, :, cols], [], [f"sgcb{bi}"])
            dma(P, "sp", sgab[bi][:], sga_v[:, :, cols], [], [f"sgab{bi}"])
            dma(P, "sp", oTb[bi][:], oT_v[:, :, cols], [], [f"oTb{bi}"])
            for cc in range(4):
                ci = cr.next()
                for k in range(CW):
                    mm(P, cv_ps[ci][:], dg[:, cc, k, :], zcb[bi][:, cc, k:k + 512], k == 0, k == CW - 1,
                       ["dg", f"zcb{bi}"], [f"cv{ci}"])
                act(P, zconv[:, cc, :], cv_ps[ci][:], AF.Identity, [f"cv{ci}", "convb"], [f"zconv{cc}"], bias=convb[:, cc:cc + 1])
                act(P, zsq[:, cc, :], zconv[:, cc, :], AF.Square, [f"zconv{cc}"], [f"zsq{cc}"])
            for cc in range(4):
                mm(P, mean_ps[:], onesm[:], zconv[:, cc, :], cc == 0, cc == 3, ["onesm", f"zconv{cc}"], ["mean_ps"])
            for cc in range(4):
                mm(P, ex2_ps[:], onesm[:], zsq[:, cc, :], cc == 0, cc == 3, ["onesm", f"zsq{cc}"], ["ex2_ps"])
            cp(P, "act", mean_sb[:], mean_ps[:], ["mean_ps"], ["mean_sb"])
            act(P, msq[:], mean_ps[:], AF.Square, ["mean_ps"], ["msq"])
            tt(P, "dve", var[:], ex2_ps[:], msq[:], ALU.subtract, ["ex2_ps", "msq"], ["var"])
            act(P, var[:], var[:], AF.Sqrt, ["var", "epsc"], ["var"], bias=epsc[:], scale=1.0)
            P.op("dve", lambda e: e.reciprocal(out=rstd[:], in_=var[:]), ["var"], ["rstd"])
            for cc in range(4):
                ti = tr1.next()
                tt(P, "dve", t1[ti][:], zconv[:, cc, :], mean_sb[:], ALU.subtract, [f"zconv{cc}", "mean_sb"], [f"t1{ti}"])
                tt(P, "dve", t1[ti][:], t1[ti][:], rstd[:], ALU.mult, [f"t1{ti}", "rstd"], [f"t1{ti}"])
                act(P, zact[:, cc, :], t1[ti][:], AF.Silu, [f"t1{ti}", "lng", "lnb"], [f"zact{cc}"],
                    scale=lng[:, cc:cc + 1], bias=lnb[:, cc:cc + 1])
            zall = [f"zact{cc}" for cc in range(4)]
            if c.dbg is not None and b == 0:
                dma(P, "sp", c.dbg[1, :, 0:512], mean_sb[:], ["mean_sb"], [])
                dma(P, "sp", c.dbg[1, :, 512:1024], var[:], ["var"], [])
                dma(P, "sp", c.dbg[1, :, 1024:1536], rstd[:], ["rstd"], [])
            for f in range(8):
                fs = slice(f * 128, (f + 1) * 128)
                yi = yr.next()
                for cc in range(4):
                    mm(P, y_ps[yi][:], wpw[:, cc, fs], zact[:, cc, :], cc == 0, cc == 3, ["wpw"] + zall, [f"y{yi}"])
                mi = mr.next()
                act(P, mixc[mi][:], y_ps[yi][:], AF.Identity, [f"y{yi}", "bpw"], [f"mixc{mi}"], bias=bpw[:, f:f + 1])
                tt(P, "dve", mixc[mi][:], mixc[mi][:], sgcb[bi][:, f, :], ALU.mult, [f"mixc{mi}", f"sgcb{bi}"], [f"mixc{mi}"])
                yi2 = yr.next()
                for h in range(NH // 2):
                    mm(P, y_ps[yi2][:], wao[:, h, fs], oTb[bi][:, h, :], h == 0, h == NH // 2 - 1,
                       ["wao", f"oTb{bi}"], [f"y{yi2}"])
                tt(P, "dve", mixa[mi][:], y_ps[yi2][:], sgab[bi][:, f, :], ALU.mult, [f"y{yi2}", f"sgab{bi}"], [f"mixa{mi}"])
                tt(P, "pool", mix[:, f, :], mixa[mi][:], mixc[mi][:], ALU.add, [f"mixa{mi}", f"mixc{mi}"], [f"mix{f}"])
            mall = [f"mix{f}" for f in range(8)]
            for j in range(4):
                t = b * 4 + j
                xi = xr.next()
                dma(P, "sp", xt[xi][:], x_v[t], [], [f"xt{xi}"])
                for half in range(2):
                    oi = opr.next()
                    hsl = slice(half * 512, (half + 1) * 512)
                    for f in range(8):
                        mm(P, o_ps[oi][:], mix[:, f, j * 128:(j + 1) * 128], wout[:, f, hsl], f == 0, f == 7,
                           mall + ["wout"], [f"op{oi}"])
                    tt(P, "dve", x1t[xi][:, hsl], xt[xi][:, hsl], o_ps[oi][:], ALU.add, [f"xt{xi}", f"op{oi}"], [f"x1t{xi}"])
                dma(P, "pool", x1_v[t], x1t[xi][:], [f"x1t{xi}"], [])
        P.barrier()
        P.emit()


def phase_d(nc, P, c):
    S = c.S
    NT = S // 128
    G = 2
    NB_G = 8
    NSL = PH * 16
    with ExitStack() as st:
        sb = lambda name, shape, dt: st.enter_context(nc.sbuf_tensor(name, list(shape), dt))
        ps = lambda name, shape, dt: st.enter_context(nc.psum_tensor(name, list(shape), dt))
        wq = sb("d_wq", [128, 8, PH * 256], BF16)
        kT = [sb(f"d_keysT{i}", [128, NK], BF16) for i in range(2)]
        kTf = sb("d_keysTf", [128, NK], F32)
        g2rep = sb("d_g2rep", [128, D], F32)
        gfrep = sb("d_gfrep", [128, D], F32)
        identb = sb("d_identb", [128, 128], BF16)
        epsc = sb("d_eps", [128, 1], F32)
        iota16 = sb("d_iota16", [128, 16], F32)
        thr17 = sb("d_thr17", [128, 17], F32)
        x1t = [sb(f"d_x1t{i}", [128, D], F32) for i in range(3)]
        tmp = sb("d_tmp", [128, D], F32)
        hn2 = sb("d_hn2", [128, D], F32)
        hn2b = [sb(f"d_hn2b{i}", [128, D], BF16) for i in range(2)]
        hn2T = sb("d_hn2T", [128, 8, 128], BF16)
        ss = sb("d_ss", [128, 1], F32)
        std = sb("d_std", [128, 1], F32)
        rstd = sb("d_rstd", [128, 1], F32)
        fss = sb("d_fss", [128, 1], F32)
        fstd = sb("d_fstd", [128, 1], F32)
        frstd = sb("d_frstd", [128, 1], F32)
        qT_sb = sb("d_qT", [128, 16, 128], BF16)
        s_sb = sb("d_s", [128, 16, NK], F32)
        gebuf = sb("d_ge", [128, PH * 16 * 17], F32)
        ohbuf = sb("d_oh", [128, PH * 256], F32)
        s2 = gebuf[:, 0:16 * NK].rearrange("p (g n) -> p g n", n=NK)
        ge = gebuf[:].rearrange("p (h k a) -> p h k a", k=16, a=17)
        cand2 = ohbuf[:].rearrange("p (h n) -> p h n", n=256)
        oh = ohbuf[:].rearrange("p (h k a) -> p h k a", k=16, a=16)
        m16 = sb("d_m16", [128, 16, 16], F32)
        i16 = sb("d_i16", [128, 16, 16], U32)
        i16f = sb("d_i16f", [128, 16, 16], F32)
        cand = sb("d_cand", [128, PH, 256], F32)
        sc = sb("d_sc", [128, PH, 16], F32)
        pos = sb("d_pos", [128, PH, 16], U32)
        posf = sb("d_posf", [128, PH, 16], F32)
        af = sb("d_af", [128, PH, 16], F32)
        bf_ = sb("d_bf", [128, PH, 16], F32)
        i1s = sb("d_i1s", [128, PH, 16], F32)
        i2s = sb("d_i2s", [128, PH, 16], F32)
        eidx = [sb(f"d_eidx{i}", [128, NSL], U32) for i in range(2)]
        ee = sb("d_ee", [128, PH, 16], F32)
        zz = sb("d_zz", [128, PH], F32)
        gg = [sb(f"d_gg{i}", [128, NSL], F32) for i in range(2)]
        adot = sb("d_adot", [128, NSL], F32)
        ga = sb("d_ga", [128, NSL], F32)
        cw = sb("d_cw", [128, NSL], F32)
        uvg = [sb(f"d_uvg{i}", [128, G, 2, D], BF16) for i in range(NB_G)]
        prodb = [sb(f"d_prodb{i}", [128, D], BF16) for i in range(4)]
        junkb = sb("d_junkb", [128, D], BF16)
        dgc = [sb(f"d_dgc{i}", [128, 128], BF16) for i in range(8)]
        x2 = sb("d_x2", [128, D], F32)
        ot = sb("d_ot", [128, D], F32)
        tp = ps("d_tp", [128, 8, 128], BF16)
        qs_ps = [ps(f"d_qs{i}", [128, 4, 128], F32) for i in range(2)]
        y_ps = [[ps(f"d_y{i}{h}", [128, 512], F32) for h in range(2)] for i in range(2)]
        fold_ps = ps("d_fold", [128, 512], F32)
        junks = sb("d_junks", [128, 128], BF16)

        for name, t_, src in (("g2rep", g2rep, c.g2rep), ("gfrep", gfrep, c.gfrep),
                              ("identb", identb, c.ident_bf), ("epsc", epsc, c.epsc), ("iota16", iota16, c.iota16)):
            dma(P, "sp", t_[:], src, [], [name])
        for i, src in enumerate((c.keys1T, c.keys2T)):
            dma(P, "pool", kT[i][:], src, [], [f"kT{i}"])
        ts(P, "dve", thr17[:, 0:16], iota16[:], 16.0, None, ALU.mult, None, ["iota16"], ["thr17"])
        P.op("dve", lambda e: e.memset(thr17[:, 16:17], 256.0), [], ["thr17b"])
        dma(P, "pool", wq[:], c.w_pq.rearrange("(k p) n -> p k n", p=128), [], ["wq"])

        x1_v = c.x1.rearrange("(t p) d -> t p d", p=128)
        out_v = c.out.rearrange("(t p) d -> t p d", p=128)
        uv_v = c.uv.rearrange("e s d -> e (s d)")
        m16v = m16[:].rearrange("p (h s) k -> p h s k", s=2)
        i16fv = i16f[:].rearrange("p (h s) k -> p h s k", s=2)
        cand4 = cand[:].rearrange("p h (a b) -> p h a b", b=16)
        B4 = [128, PH, 16, 16]

        def prologue(t):
            par = t % 2
            xp = t % 3
            X, HB, EI, GG = f"x1t{xp}", f"hn2b{par}", f"eidx{par}", f"gg{par}"
            dma(P, "sp", x1t[xp][:], x1_v[t], [], [X])
            yield
            act(P, junkb[:], x1t[xp][:], AF.Square, [X], ["junkb", "ss"], accum_out=ss[:])
            yield
            act(P, std[:], ss[:], AF.Sqrt, ["ss", "epsc"], ["std"], bias=epsc[:], scale=1.0 / D)
            yield
            P.op("dve", lambda e: e.reciprocal(out=rstd[:], in_=std[:]), ["std"], ["rstd"])
            yield
            act(P, tmp[:], x1t[xp][:], AF.Copy, [X, "rstd"], ["tmp"], scale=rstd[:])
            yield
            tt(P, "dve", hn2[:], tmp[:], g2rep[:], ALU.mult, ["tmp", "g2rep"], ["hn2"])
            yield
            cp(P, "act", hn2b[par][:], hn2[:], ["hn2"], [HB])
            yield
            for k in range(8):
                tr(P, tp[:, k, :], hn2b[par][:, k * 128:(k + 1) * 128], identb[:], [HB, "identb"], ["tp"])
            yield
            cp(P, "dve", hn2T[:], tp[:], ["tp"], ["hn2T"])
            yield

            def qmm(g4):
                for gi in range(4):
                    g = g4 * 4 + gi
                    for k in range(8):
                        mm(P, qs_ps[g4 % 2][:, gi, :], wq[:, k, g * 128:(g + 1) * 128], hn2T[:, k, :], k == 0, k == 7,
                           ["wq", "hn2T"], [f"qs{g4 % 2}"])

            def qcp(g4):
                cp(P, "act", qT_sb[:, g4 * 4:(g4 + 1) * 4, :], qs_ps[g4 % 2][:], [f"qs{g4 % 2}"], [f"qT{g4}"])

            def smm(g4):
                for gi in range(4):
                    g = g4 * 4 + gi
                    mm(P, qs_ps[g4 % 2][:, gi, :], qT_sb[:, g, :], kT[g % 2][:], True, True, [f"qT{g4}", f"kT{g%2}"], [f"qs{g4 % 2}"])

            def scp(g4):
                cp(P, "act", s_sb[:, g4 * 4:(g4 + 1) * 4, :], qs_ps[g4 % 2][:], [f"qs{g4 % 2}"], [f"s{g4}"])

            for g4 in range(5):
                if g4 < 4:
                    qmm(g4)
                if g4 >= 1:
                    qcp(g4 - 1)
                yield
            for g4 in range(5):
                if g4 < 4:
                    smm(g4)
                if g4 >= 1:
                    scp(g4 - 1)
                yield
            for g in range(16):
                P.op("dve", (lambda g: lambda e: e.max(out=m16[:, g, 0:8], in_=s_sb[:, g, :]))(g), [f"s{g // 4}"], [f"m16a{g}"])
                if g % 4 == 3:
                    yield
            for g in range(16):
                P.op("dve", (lambda g: lambda e: e.max_index(out=i16[:, g, 0:8], in_max=m16[:, g, 0:8], in_values=s_sb[:, g, :]))(g),
                     [f"s{g // 4}", f"m16a{g}"], [f"i16a{g}"])
                if g % 4 == 3:
                    yield
            for g in range(16):
                P.op("dve", (lambda g: lambda e: e.match_replace(out=s2[:, g, :], in_to_replace=m16[:, g, 0:8],
                                                                 in_values=s_sb[:, g, :], imm_value=NEG))(g),
                     [f"s{g // 4}", f"m16a{g}"], [f"s2_{g}"])
                if g % 4 == 3:
                    yield
            for g in range(16):
                P.op("dve", (lambda g: lambda e: e.max(out=m16[:, g, 8:16], in_=s2[:, g, :]))(g), [f"s2_{g}"], [f"m16b{g}"])
                if g % 4 == 3:
                    yield
            for g in range(16):
                P.op("dve", (lambda g: lambda e: e.max_index(out=i16[:, g, 8:16], in_max=m16[:, g, 8:16], in_values=s2[:, g, :]))(g),
                     [f"s2_{g}", f"m16b{g}"], [f"i16b{g}"])
                if g % 4 == 3:
                    yield
            m16all = [f"m16a{g}" for g in range(16)] + [f"m16b{g}" for g in range(16)]
            i16all = [f"i16a{g}" for g in range(16)] + [f"i16b{g}" for g in range(16)]
            cp(P, "dve", i16f[:], i16[:], i16all, ["i16f"])
            tt(P, "dve", cand4, m16v[:, :, 0, :].unsqueeze(3).to_broadcast(B4), m16v[:, :, 1, :].unsqueeze(2).to_broadcast(B4),
               ALU.add, m16all, ["cand"])
            yield
            for h in range(PH):
                P.op("dve", (lambda h: lambda e: e.max(out=sc[:, h, 0:8], in_=cand[:, h, :]))(h), ["cand"], [f"sca{h}"])
                if h % 4 == 3:
                    yield
            for h in range(PH):
                P.op("dve", (lambda h: lambda e: e.max_index(out=pos[:, h, 0:8], in_max=sc[:, h, 0:8], in_values=cand[:, h, :]))(h),
                     ["cand", f"sca{h}"], [f"posa{h}"])
                if h % 4 == 3:
                    yield
            for h in range(PH):
                P.op("dve", (lambda h: lambda e: e.match_replace(out=cand2[:, h, :], in_to_replace=sc[:, h, 0:8],
                                                                 in_values=cand[:, h, :], imm_value=NEG))(h),
                     ["cand", f"sca{h}"], [f"c2_{h}"])
                if h % 4 == 3:
                    yield
            for h in range(PH):
                P.op("dve", (lambda h: lambda e: e.max(out=sc[:, h, 8:16], in_=cand2[:, h, :]))(h), [f"c2_{h}"], [f"scb{h}"])
                if h % 4 == 3:
                    yield
            for h in range(PH):
                P.op("dve", (lambda h: lambda e: e.max_index(out=pos[:, h, 8:16], in_max=sc[:, h, 8:16], in_values=cand2[:, h, :]))(h),
                     [f"c2_{h}", f"scb{h}"], [f"posb{h}"])
                if h % 4 == 3:
                    yield
            scall = [f"sca{h}" for h in range(PH)] + [f"scb{h}" for h in range(PH)]
            posall = [f"posa{h}" for h in range(PH)] + [f"posb{h}" for h in range(PH)]
            cp(P, "dve", posf[:], pos[:], posall, ["posf"])
            tt(P, "dve", ge, posf[:].unsqueeze(3).to_broadcast([128, PH, 16, 17]),
               thr17[:].unsqueeze(1).unsqueeze(1).to_broadcast([128, PH, 16, 17]), ALU.is_ge, ["posf", "thr17", "thr17b"], ["ge"])
            yield
            tt(P, "dve", oh, ge[:, :, :, 0:16], ge[:, :, :, 1:17], ALU.subtract, ["ge"], ["oh"])
            P.op("dve", lambda e: e.tensor_reduce(out=af[:], in_=ge[:, :, :, 1:17], axis=AX.X, op=ALU.add), ["ge"], ["af"])
            yield
            tt(P, "dve", oh, oh, i16fv[:, :, 0, :].unsqueeze(2).to_broadcast(B4), ALU.mult, ["oh", "i16f"], ["oh"])
            P.op("dve", lambda e: e.tensor_reduce(out=i1s[:], in_=oh, axis=AX.X, op=ALU.add), ["oh"], ["i1s"])
            ts(P, "dve", bf_[:], af[:], -16.0, None, ALU.mult, None, ["af"], ["bf"])
            tt(P, "dve", bf_[:], bf_[:], posf[:], ALU.add, ["bf", "posf"], ["bf"])
            yield
            tt(P, "dve", oh, iota16[:].unsqueeze(1).unsqueeze(1).to_broadcast(B4), bf_[:].unsqueeze(3).to_broadcast(B4),
               ALU.is_equal, ["iota16", "bf", "i1s"], ["oh"])
            tt(P, "dve", oh, oh, i16fv[:, :, 1, :].unsqueeze(2).to_broadcast(B4), ALU.mult, ["oh", "i16f"], ["oh"])
            P.op("dve", lambda e: e.tensor_reduce(out=i2s[:], in_=oh, axis=AX.X, op=ALU.add), ["oh"], ["i2s"])
            yield
            ts(P, "dve", i1s[:], i1s[:], float(NK), None, ALU.mult, None, ["i1s"], ["i1s"])
            tt(P, "dve", i1s[:], i1s[:], i2s[:], ALU.add, ["i1s", "i2s"], ["i1s"])
            cp(P, "dve", eidx[par][:], i1s[:].rearrange("p h k -> p (h k)"), ["i1s"], [EI])
            tt(P, "dve", ee[:], sc[:], sc[:, :, 0:1].to_broadcast([128, PH, 16]), ALU.subtract, scall, ["ee"])
            yield
            act(P, ee[:], ee[:], AF.Exp, ["ee"], ["ee"])
            yield
            P.op("dve", lambda e: e.tensor_reduce(out=zz[:], in_=ee[:], axis=AX.X, op=ALU.add), ["ee"], ["zz"])
            P.op("dve", lambda e: e.reciprocal(out=zz[:], in_=zz[:]), ["zz"], ["zz"])
            tt(P, "dve", gg[par][:].rearrange("p (h k) -> p h k", k=16), ee[:], zz[:].unsqueeze(2).to_broadcast([128, PH, 16]),
               ALU.mult, ["ee", "zz"], [GG])
            yield

        def advance(gen, n):
            if gen is None:
                return None
            for _ in range(n):
                try:
                    next(gen)
                except StopIteration:
                    return None
            return gen

        gen = prologue(0)
        while gen is not None:
            gen = advance(gen, 1)
        br, pr = Rot(NB_G), Rot(4)
        NGRP = NSL // G
        gbuf = {}

        def names(t):
            par = t % 2
            return par, f"x1t{t % 3}", f"hn2b{par}", f"eidx{par}", f"gg{par}"

        def stage_a(t, gi):
            par, X, HB, EI, GG = names(t)
            s0 = gi * G
            bi = br.next()
            gbuf[(t, gi)] = bi
            for j in range(G):
                s = s0 + j
                P.dma("pool", (lambda bi, j, s, par: lambda e: e.indirect_dma_start(
                    out=uvg[bi][:, j, :, :].rearrange("p s d -> p (s d)"), out_offset=None, in_=uv_v,
                    in_offset=bass.IndirectOffsetOnAxis(ap=eidx[par][:, s:s + 1], axis=0)))(bi, j, s, par),
                    [EI], [f"uvg{bi}_{j}"])
            for j in range(G):
                s = s0 + j
                pi = pr.next()
                tt(P, "dve", prodb[pi][:], uvg[bi][:, j, 0, :], hn2b[par][:], ALU.mult, [f"uvg{bi}_{j}", HB], [f"prodb{pi}"])
                if s % 2 == 1:
                    for cc in range(8):
                        mm(P, fold_ps[:, 0:128], identb[:], prodb[pi][:, cc * 128:(cc + 1) * 128], cc == 0, cc == 7,
                           ["identb", f"prodb{pi}"], ["fold"])
                    act(P, junks[:], fold_ps[:, 0:128], AF.Identity, ["fold"], ["junks", f"adot{s}"], accum_out=adot[:, s:s + 1])
                else:
                    act(P, junkb[:], prodb[pi][:], AF.Identity, [f"prodb{pi}"], ["junkb", f"adot{s}"], accum_out=adot[:, s:s + 1])

        def stage_b(t, gi):
            s0 = gi * G
            adg = [f"adot{s0 + j}" for j in range(G)]
            act(P, ga[:, s0:s0 + G], adot[:, s0:s0 + G], AF.Gelu, adg, [f"ga{s0}"])

        def stage_c(t, gi):
            par, X, HB, EI, GG = names(t)
            s0 = gi * G
            tt(P, "dve", cw[:, s0:s0 + G], ga[:, s0:s0 + G], gg[par][:, s0:s0 + G], ALU.mult, [f"ga{s0}", GG], [f"cw{s0}"])

        def stage_d(t, gi):
            par, X, HB, EI, GG = names(t)
            yp = y_ps[par]
            s0 = gi * G
            bi = gbuf.pop((t, gi))
            for j in range(G):
                s = s0 + j
                di = s % 8
                if s % 2 == 0:
                    act(P, dgc[di][:], identb[:], AF.Copy, ["identb", f"cw{s0}"], [f"dgc{di}"], scale=cw[:, s:s + 1])
                else:
                    ts(P, "dve", dgc[di][:], identb[:], cw[:, s:s + 1], None, ALU.mult, None, ["identb", f"cw{s0}"], [f"dgc{di}"])
            for j in range(G):
                s = s0 + j
                di = s % 8
                for half in range(2):
                    mm(P, yp[half][:], dgc[di][:], uvg[bi][:, j, 1, half * 512:(half + 1) * 512], s == 0, s == NSL - 1,
                       [f"dgc{di}", f"uvg{bi}_{j}"], [f"y{par}{half}"])

        def finalize(t):
            par, X, HB, EI, GG = names(t)
            xp = t % 3
            yp = y_ps[par]
            for half in range(2):
                hsl = slice(half * 512, (half + 1) * 512)
                tt(P, "dve", x2[:, hsl], x1t[xp][:, hsl], yp[half][:], ALU.add, [X, f"y{par}{half}"], [f"x2_{half}"])
            yield
            act(P, junkb[:], x2[:], AF.Square, ["x2_0", "x2_1"], ["junkb", "fss"], accum_out=fss[:])
            yield
            act(P, fstd[:], fss[:], AF.Sqrt, ["fss", "epsc"], ["fstd"], bias=epsc[:], scale=1.0 / D)
            yield
            P.op("dve", lambda e: e.reciprocal(out=frstd[:], in_=fstd[:]), ["fstd"], ["frstd"])
            yield
            act(P, ot[:], x2[:], AF.Copy, ["x2_0", "x2_1", "frstd"], ["ot"], scale=frstd[:])
            yield
            tt(P, "dve", ot[:], ot[:], gfrep[:], ALU.mult, ["ot", "gfrep"], ["ot"])
            yield
            dma(P, "sp", out_v[t], ot[:], ["ot"], ["out"])
            yield

        units = [(t, gi) for t in range(NT) for gi in range(NGRP)]
        NU = len(units)
        gen = None
        fgen = None
        for n in range(NU + 3):
            if n < NU:
                t, gi = units[n]
                if gi == 0 and gen is not None:
                    while gen is not None:
                        gen = advance(gen, 1)
                stage_a(t, gi)
            if 1 <= n <= NU:
                stage_b(*units[n - 1])
            if 2 <= n <= NU + 1:
                stage_c(*units[n - 2])
            if n >= 3:
                tc_, gc_ = units[n - 3]
                stage_d(tc_, gc_)
                if gc_ == NGRP - 1:
                    while fgen is not None:
                        fgen = advance(fgen, 1)
                    fgen = finalize(tc_)
            fgen = advance(fgen, 1)
            if n < NU:
                t, gi = units[n]
                if gi == 3 and t + 1 < NT:
                    gen = prologue(t + 1)
                gen = advance(gen, 1)
                if gi == NGRP - 1:
                    while gen is not None:
                        gen = advance(gen, 1)
        while fgen is not None:
            fgen = advance(fgen, 1)
        P.barrier()
        P.emit()


def build_all(nc, P, c, upto="d"):
    phase_a(nc, P, c)
    if upto < "b":
        return
    with ExitStack() as wsc:
        wts = alloc_c_weights(nc, wsc)
        phase_b(nc, P, c, preload=lambda: load_c_weights(nc, P, c, wts, None))
        if upto >= "c":
            phase_c(nc, P, c, wts)
    if upto >= "d":
        phase_d(nc, P, c)


def build_program(S):
    nc = bass.Bass("TRN2", target_bir_lowering=False)
    c = declare_io(nc, S, debug=False)
    with ExitStack() as st:
        P = Prog(nc, st)
        build_all(nc, P, c)
    return nc


def kernel(**inputs):
    x = np.asarray(inputs["x"], dtype=np.float32)
    B, S, _ = x.shape
    assert B == 8
    nc = build_program(S)
    consts = make_consts(S)
    w = {k: np.asarray(v) for k, v in inputs.items() if k != "x"}
    shared = host_inputs(S, x[0], w, consts)
    in_maps = []
    for b in range(B):
        m = dict(shared)
        m["x"] = np.ascontiguousarray(x[b])
        in_maps.append(m)
    res = run_bass_kernel_spmd(nc, in_maps, core_ids=list(range(B)))
    out = np.stack([np.asarray(res.results[b]["out"], dtype=np.float32) for b in range(B)], axis=0)
    return out
```

```python
import numpy as np
import ml_dtypes
from contextlib import ExitStack
import concourse.bass as bass
import concourse.mybir as mybir
from concourse.bass_utils import run_bass_kernel_spmd

F32 = mybir.dt.float32
BF16 = mybir.dt.bfloat16
U32 = mybir.dt.uint32
I32 = mybir.dt.int32
ALU = mybir.AluOpType
AF = mybir.ActivationFunctionType
AX = mybir.AxisListType

D = 1024
NCOL = 4608
NH = 8
DH = 64
CC = 512
CW = 31
MB = 256
PH = 8
NK = 128
TOPK = 16
EPS = 1e-6
BIG = 30000.0
NEG = -1.0e30


class Prog:
    ENG = ("pe", "dve", "act", "pool", "sp")
    DQ = ("sp", "pool", "act")

    def __init__(self, nc, stack, kdma=8):
        self.nc = nc
        self.eng = {"pe": nc.tensor, "dve": nc.vector, "act": nc.scalar, "pool": nc.gpsimd, "sp": nc.sync}
        self.sems = {}
        for e in self.ENG:
            self.sems[("c", e)] = stack.enter_context(nc.semaphore(f"c_{e}"))
        self.kdma = {"sp": kdma, "pool": 16, "act": 4}
        for q in self.DQ:
            for j in range(self.kdma[q]):
                self.sems[("d", q, j)] = stack.enter_context(nc.semaphore(f"d_{q}{j}"))
        self.latest = {k: 0 for k in self.sems}
        self.dnext = {q: 0 for q in self.DQ}
        self.ops = {e: [] for e in self.ENG}
        self.known = {e: {} for e in self.ENG}
        self.lastw = {}
        self.readers = {}
        self.nins = 0

    def _need(self, eng, key, val):
        if self.known[eng].get(key, 0) < val:
            self.known[eng][key] = val
            self.ops[eng].append(("w", key, val))

    def _deps(self, eng, reads, writes, is_dma):
        for b in reads:
            lw = self.lastw.get(b)
            if lw is not None:
                key, val = lw
                if (not is_dma) and key == ("c", eng) and eng == "pe":
                    continue
                self._need(eng, key, val)
        for b in writes:
            lw = self.lastw.get(b)
            if lw is not None:
                key, val = lw
                if is_dma or key != ("c", eng):
                    self._need(eng, key, val)
            for key, val in self.readers.get(b, {}).items():
                if is_dma or key != ("c", eng):
                    self._need(eng, key, val)

    def _commit(self, key, val, reads, writes):
        for b in writes:
            self.lastw[b] = (key, val)
            self.readers[b] = {}
        for b in reads:
            r = self.readers.setdefault(b, {})
            r[key] = max(r.get(key, 0), val)

    def op(self, eng, fn, reads=(), writes=()):
        self._deps(eng, reads, writes, False)
        key = ("c", eng)
        self.latest[key] += 1
        val = self.latest[key]
        self.ops[eng].append(("i", fn, key, 1))
        self._commit(key, val, reads, writes)
        self.nins += 1

    def dma(self, q, fn, reads=(), writes=()):
        self._deps(q, reads, writes, True)
        j = self.dnext[q]
        self.dnext[q] = (j + 1) % self.kdma[q]
        key = ("d", q, j)
        if self.latest[key] > 0:
            self._need(q, key, self.latest[key])
        self.latest[key] += 16
        val = self.latest[key]
        self.ops[q].append(("i", fn, key, 16))
        self._commit(key, val, reads, writes)
        self.nins += 1

    def barrier(self):
        for e in self.ENG:
            for key, val in self.latest.items():
                if val > 0:
                    self._need(e, key, val)
        self.lastw = {}
        self.readers = {}

    def emit(self, name=None):
        ops = self.ops
        self.ops = {e: [] for e in self.ENG}
        sems = self.sems
        nc = self.nc

        def replay(lst):
            def f(e):
                for it in lst:
                    if it[0] == "w":
                        e.wait_ge(sems[it[1]], it[2])
                    else:
                        ins = it[1](e)
                        ins.then_inc(sems[it[2]], it[3])
            return f

        with nc.Block() as block:
            block.tensor(replay(ops["pe"]))
            block.vector(replay(ops["dve"]))
            block.scalar(replay(ops["act"]))
            block.gpsimd(replay(ops["pool"]))
            block.sync(replay(ops["sp"]))


class Rot:
    def __init__(self, n):
        self.n = n
        self.i = -1

    def next(self):
        self.i = (self.i + 1) % self.n
        return self.i


def _bf(a):
    return np.ascontiguousarray(a).astype(ml_dtypes.bfloat16)


def make_consts(S):
    NT = S // 128
    NB = S // MB
    c = {}
    c["ident_bf"] = _bf(np.eye(128, dtype=np.float32))
    c["ident_f"] = np.eye(128, dtype=np.float32)
    ka = np.zeros((18, S), np.float32)
    ka[0, :] = 1.0
    for n in range(NB):
        ka[1 + n, n * MB:(n + 1) * MB] = 1.0
    ka[17, :] = ((np.arange(S) // 128) % 2).astype(np.float32)
    c["kaug_static"] = _bf(ka)
    mt = np.zeros((4, 128, 512), np.float32)
    for i in range(4):
        for j in range(4):
            if i // 2 != j // 2:
                continue
            blk = mt[i, :, j * 128:(j + 1) * 128]
            if i > j:
                blk[:] = -BIG
            elif i == j:
                kk = np.arange(128)[:, None]
                qq = np.arange(128)[None, :]
                blk[kk > qq] = -BIG
    c["masktri"] = _bf(mt)
    tile = np.arange(NT)[:, None]
    n = np.arange(16)[None, :]
    valid = (n < tile // 2).astype(np.float32)
    own = (n == tile // 2).astype(np.float32)
    c["valid01"] = np.broadcast_to(valid[None], (128, NT, 16)).astype(np.float32).copy()
    c["own01"] = np.broadcast_to(own[None], (128, NT, 16)).astype(np.float32).copy()
    c["maskv"] = ((c["valid01"] - 1.0) * 1.0e30).astype(np.float32)
    slopes = np.array([2.0 ** (-8.0 * (i + 1) / NH) for i in range(NH)], np.float32)
    stat = np.zeros((NH, 128, NT, 16), np.float32)
    for h in range(NH):
        st = -slopes[h] * (128.0 * tile - 256.0 * n)
        st = np.where(n <= tile // 2, st, 0.0)
        stat[h] = st[None]
    c["stat"] = stat
    p = np.arange(128, dtype=np.float32)
    c["qlo"] = _bf(np.stack([-slopes[h] * p for h in range(NH)], axis=1))
    c["qhi"] = _bf(np.stack([128.0 * slopes[h] * np.ones(128, np.float32) for h in range(NH)], axis=1))
    kb = np.zeros((128, NH, 2), np.float32)
    for h in range(NH):
        for half in range(2):
            kb[:, h, half] = slopes[h] * (p + 128.0 * half)
    c["kbias"] = kb
    c["iota16"] = np.broadcast_to(np.arange(16, dtype=np.float32)[None], (128, 16)).copy()
    c["epsc"] = np.full((128, 1), EPS, np.float32)
    return c


def act(P, out, in_, func, r, w, **kw):
    P.op("act", lambda e: e.activation(out=out, in_=in_, func=func, **kw), r, w)


def tt(P, eng, out, in0, in1, op, r, w):
    P.op(eng, lambda e: e.tensor_tensor(out=out, in0=in0, in1=in1, op=op), r, w)


def ts(P, eng, out, in0, s1, s2, op0, op1, r, w, **kw):
    if s2 is None:
        P.op(eng, lambda e: e.tensor_scalar(out=out, in0=in0, scalar1=s1, scalar2=None, op0=op0, **kw), r, w)
    else:
        P.op(eng, lambda e: e.tensor_scalar(out=out, in0=in0, scalar1=s1, scalar2=s2, op0=op0, op1=op1, **kw), r, w)


def cp(P, eng, out, in_, r, w):
    if eng == "act":
        P.op("act", lambda e: e.copy(out=out, in_=in_), r, w)
    else:
        P.op(eng, lambda e: e.tensor_copy(out=out, in_=in_), r, w)


def mm(P, out, lhsT, rhs, start, stop, r, w):
    P.op("pe", lambda e: e.matmul(out, lhsT, rhs, start=start, stop=stop), r, w)


def tr(P, out, in_, ident, r, w):
    P.op("pe", lambda e: e.transpose(out, in_, ident), r, w)


def dma(P, q, out, in_, r, w):
    P.dma(q, lambda e: e.dma_start(out=out, in_=in_), r, w)


def rmsnorm_stats(P, xt, junk, ss, std, rstd, epsc, tag):
    act(P, junk, xt, AF.Square, [tag + "x"], [tag + "junk", tag + "ss"], accum_out=ss)
    act(P, std, ss, AF.Sqrt, [tag + "ss", "epsc"], [tag + "std"], bias=epsc, scale=1.0 / D)
    P.op("dve", lambda e: e.reciprocal(out=rstd, in_=std), [tag + "std"], [tag + "rstd"])


class Ctx:
    pass


def declare_io(nc, S, debug=False):
    c = Ctx()
    c.S = S

    def inp(name, shape, dt=F32):
        return nc.dram_tensor(name, list(shape), dt, kind="ExternalInput").ap()

    def scr(name, shape, dt):
        return nc.dram_tensor(name, list(shape), dt, kind="ExternalOutput" if debug else "Internal").ap()

    NT = S // 128
    c.x = inp("x", [S, D])
    c.w_in = inp("w_in", [D, NCOL])
    c.g1c = inp("g1c", [128, 8])
    c.conv_wT = inp("conv_wT", [128, 4, CW])
    c.conv_bc = inp("conv_bc", [128, 4])
    c.ln_gc = inp("ln_gc", [128, 4])
    c.ln_bc = inp("ln_bc", [128, 4])
    c.b_pwc = inp("b_pwc", [128, 8])
    c.w_pw = inp("w_conv_pw", [CC, D])
    c.w_ao = inp("w_attn_out", [NH * DH, D])
    c.w_out = inp("w_out", [D, D])
    c.w_pq = inp("w_peer_q", [D, PH * 256])
    c.g2rep = inp("g2rep", [128, D])
    c.gfrep = inp("gfrep", [128, D])
    c.keys1T = inp("keys1T", [128, NK])
    c.keys2T = inp("keys2T", [128, NK])
    c.peer_u = inp("peer_u", [NK * NK, D])
    c.peer_v = inp("peer_v", [NK * NK, D])
    c.ident_bf = inp("ident_bf", [128, 128], BF16)
    c.ident_f = inp("ident_f", [128, 128])
    c.kaug_static = inp("kaug_static", [18, S], BF16)
    c.qhi = inp("qhi", [128, NH], BF16)
    c.masktri = inp("masktri", [4, 128, 512], BF16)
    c.valid01 = inp("valid01", [128, NT, 16])
    c.own01 = inp("own01", [128, NT, 16])
    c.maskv = inp("maskv", [128, NT, 16])
    c.stat = inp("stat", [NH, 128, NT, 16])
    c.qlo = inp("qlo", [128, NH], BF16)
    c.kbias = inp("kbias", [128, NH, 2])
    c.iota16 = inp("iota16", [128, 16])
    c.epsc = inp("epsc", [128, 1])
    c.out = nc.dram_tensor("out", [S, D], F32, kind="ExternalOutput").ap()
    c.zcT = scr("s_zcT", [CC, S], BF16)
    c.qT = scr("s_qT", [NH * DH, S], BF16)
    c.kT = scr("s_kT", [NH * DH, S], BF16)
    c.vS = scr("s_v", [S, NH * DH], BF16)
    c.sgc = scr("s_sgc", [D, S], BF16)
    c.sga = scr("s_sga", [D, S], BF16)
    c.oT = scr("s_oT", [NH * DH, S], BF16)
    c.x1 = scr("s_x1", [S, D], F32)
    c.uv = nc.dram_tensor("s_uv", [NK * NK, 2, D], BF16, kind="Internal").ap()
    c.dbg = scr("s_dbg", [4, 128, 2048], F32) if debug else None
    return c


def phase_a(nc, P, c):
    S = c.S
    NBLK = S // 512
    with ExitStack() as st:
        sb = lambda name, shape, dt: st.enter_context(nc.sbuf_tensor(name, list(shape), dt))
        ps = lambda name, shape, dt: st.enter_context(nc.psum_tensor(name, list(shape), dt))
        wbf = sb("a_wbf", [128, 8, NCOL], BF16)
        wst = [sb(f"a_wst{i}", [128, NCOL], F32) for i in range(2)]
        g1c = sb("a_g1c", [128, 8], F32)
        epsc = sb("a_eps", [128, 1], F32)
        ident = sb("a_ident", [128, 128], BF16)
        xt = [sb(f"a_xt{i}", [128, D], F32) for i in range(3)]
        junk = sb("a_junk", [128, D], F32)
        hn = [sb(f"a_hn{i}", [128, D], BF16) for i in range(2)]
        ss = [sb(f"a_ss{i}", [128, 1], F32) for i in range(2)]
        std = [sb(f"a_std{i}", [128, 1], F32) for i in range(2)]
        rstd = [sb(f"a_rstd{i}", [128, 1], F32) for i in range(2)]
        hnT = [sb(f"a_hnT{i}", [128, 8, 512], BF16) for i in range(2)]
        sig = [sb(f"a_sig{i}", [128, 512], F32) for i in range(2)]
        stg = [sb(f"a_stg{i}", [128, 512], BF16) for i in range(8)]
        tp = ps("a_tp", [128, 8, 128], BF16)
        acc = [ps(f"a_acc{i}", [128, 512], F32) for i in range(6)]

        dma(P, "sp", g1c[:], c.g1c, [], ["g1c"])
        dma(P, "sp", epsc[:], c.epsc, [], ["epsc"])
        dma(P, "sp", ident[:], c.ident_bf, [], ["ident"])
        w_in_v = c.w_in.rearrange("(k p) n -> k p n", p=128)
        for k in range(8):
            dma(P, "sp", wst[k % 2][:], w_in_v[k], [], [f"wst{k%2}"])
            act(P, wbf[:, k, :], wst[k % 2][:], AF.Copy, [f"wst{k%2}", "g1c"], [f"wbf{k}"], scale=g1c[:, k:k + 1])
        wall = [f"wbf{k}" for k in range(8)]

        xr, hr, sr, ar, gr = Rot(3), Rot(2), Rot(2), Rot(6), Rot(8)

        CH = 1024
        conv_list = [(ti, r0) for r0 in range(0, NK * NK, CH) for ti in range(2)]
        nstores = [0]
        per = max(1, (NBLK * 36) // len(conv_list))

        def conv_tick(force=False):
            nstores[0] += 1
            if conv_list and (force or nstores[0] % per == 0):
                ti, r0 = conv_list.pop(0)
                tab = (c.peer_u, c.peer_v)[ti]
                dma(P, "pool", c.uv[r0:r0 + CH, ti, :], tab[r0:r0 + CH, :], [], [])
        x_v = c.x.rearrange("(t p) d -> t p d", p=128)

        def store(dst, src_ps, tok, kind, scale=None):
            g = gr.next()
            npart = dst.shape[0]
            if kind == "sigmoid":
                act(P, stg[g][0:npart, :], src_ps, AF.Sigmoid, [tok], [f"stg{g}"])
            elif kind == "scale":
                act(P, stg[g][0:npart, :], src_ps, AF.Copy, [tok], [f"stg{g}"], scale=scale)
            else:
                cp(P, "dve", stg[g][0:npart, :], src_ps, [tok], [f"stg{g}"])
            dma(P, "pool", dst, stg[g][0:npart, :], [f"stg{g}"], [])
            conv_tick()

        for b in range(NBLK):
            hb = hr.next()
            for j in range(4):
                t = b * 4 + j
                xi = xr.next()
                si = sr.next()
                dma(P, "sp", xt[xi][:], x_v[t], [], [f"xt{xi}x"])
                rmsnorm_stats(P, xt[xi][:], junk[:], ss[si][:], std[si][:], rstd[si][:], epsc[:], f"xt{xi}")
                act(P, hn[si][:], xt[xi][:], AF.Copy, [f"xt{xi}x", f"xt{xi}rstd"], [f"hn{si}"], scale=rstd[si][:])
                for k in range(8):
                    tr(P, tp[:, k, :], hn[si][:, k * 128:(k + 1) * 128], ident[:], [f"hn{si}", "ident"], ["tp"])
                cp(P, "dve", hnT[hb][:, :, j * 128:(j + 1) * 128], tp[:], ["tp"], [f"hnT{hb}"])
            cols = slice(b * 512, (b + 1) * 512)

            def proj(c0, m):
                a = ar.next()
                for k in range(8):
                    mm(P, acc[a][0:m, :], wbf[:, k, c0:c0 + m], hnT[hb][:, k, :], k == 0, k == 7,
                       wall + [f"hnT{hb}"], [f"acc{a}"])
                return acc[a], f"acc{a}"

            for cc in range(4):
                pa, ta = proj(cc * 128, 128)
                pg, tg = proj(CC + cc * 128, 128)
                s_i = sr.next()
                act(P, sig[s_i][:], pg[:], AF.Sigmoid, [tg], [f"sig{s_i}"])
                g = gr.next()
                tt(P, "dve", stg[g][:], pa[:], sig[s_i][:], ALU.mult, [ta, f"sig{s_i}"], [f"stg{g}"])
                dma(P, "pool", c.zcT[cc * 128:(cc + 1) * 128, cols], stg[g][:], [f"stg{g}"], [])
                conv_tick()
            for cc in range(4):
                pq, tq = proj(2 * CC + cc * 128, 128)
                store(c.qT[cc * 128:(cc + 1) * 128, cols], pq[:], tq, "scale", scale=DH ** -0.5)
            for cc in range(4):
                pk, tk = proj(2 * CC + 512 + cc * 128, 128)
                store(c.kT[cc * 128:(cc + 1) * 128, cols], pk[:], tk, "copy")
            for cc in range(8):
                pg, tg = proj(2 * CC + 1536 + cc * 128, 128)
                store(c.sgc[cc * 128:(cc + 1) * 128, cols], pg[:], tg, "sigmoid")
            for cc in range(8):
                pg, tg = proj(2 * CC + 1536 + D + cc * 128, 128)
                store(c.sga[cc * 128:(cc + 1) * 128, cols], pg[:], tg, "sigmoid")
            for j in range(4):
                a = ar.next()
                for k in range(8):
                    mm(P, acc[a][:], hnT[hb][:, k, j * 128:(j + 1) * 128], wbf[:, k, 2 * CC + 1024:2 * CC + 1536],
                       k == 0, k == 7, wall + [f"hnT{hb}"], [f"acc{a}"])
                t = b * 4 + j
                store(c.vS[t * 128:(t + 1) * 128, :], acc[a][:], f"acc{a}", "copy")
        while conv_list:
            conv_tick(force=True)
        P.barrier()
        P.emit()


def host_inputs(S, x_b, w, consts):
    f = lambda a: np.ascontiguousarray(np.asarray(a, dtype=np.float32))
    col = lambda v, n: f(np.asarray(v).reshape(n, 128).T)
    m = {
        "x": f(x_b),
        "w_in": f(w["w_in"][0]),
        "g1c": col(w["g_norm1"][0], 8),
        "conv_wT": f(np.asarray(w["conv_w"][0]).reshape(CW, 4, 128).transpose(2, 1, 0)),
        "conv_bc": col(w["conv_b"][0], 4),
        "ln_gc": col(w["conv_ln_g"][0], 4),
        "ln_bc": col(w["conv_ln_b"][0], 4),
        "b_pwc": col(w["b_conv_pw"][0], 8),
        "w_conv_pw": f(w["w_conv_pw"][0]),
        "w_attn_out": f(w["w_attn_out"][0]),
        "w_out": f(w["w_out"][0]),
        "w_peer_q": f(w["w_peer_q"][0]),
        "g2rep": f(np.broadcast_to(np.asarray(w["g_norm2"][0])[None, :], (128, D))),
        "gfrep": f(np.broadcast_to(np.asarray(w["g_final"])[None, :], (128, D))),
        "keys1T": f(np.asarray(w["peer_keys1"][0]).T),
        "keys2T": f(np.asarray(w["peer_keys2"][0]).T),
        "peer_u": f(w["peer_u"][0]),
        "peer_v": f(w["peer_v"][0]),
    }
    m.update(consts)
    return m


def phase_b(nc, P, c, preload=None):
    S = c.S
    NT = S // 128
    NQB = S // 512
    with ExitStack() as st:
        sb = lambda name, shape, dt: st.enter_context(nc.sbuf_tensor(name, list(shape), dt))
        ps = lambda name, shape, dt: st.enter_context(nc.psum_tensor(name, list(shape), dt))
        kaug = [sb(f"b_kaug{i}", [128, S], BF16) for i in range(2)]
        qaug = [sb(f"b_qaug{i}", [128, S], BF16) for i in range(2)]
        vall = sb("b_vall", [128, NT, NH * DH], BF16)
        vaug = [sb(f"b_vaug{i}", [128, NT, 96], BF16) for i in range(2)]
        augtok = sb("b_augtok", [128, NT, 96], BF16)
        valid01 = sb("b_valid", [128, NT, 16], F32)
        own01 = sb("b_own", [128, NT, 16], F32)
        maskv = sb("b_maskv", [128, NT, 16], F32)
        stat = [sb(f"b_stat{i}", [128, NT, 16], F32) for i in range(2)]
        kbias = sb("b_kbias", [128, NH, 2], F32)
        qlo = sb("b_qlo", [128, NH], BF16)
        qhi = sb("b_qhi", [128, NH], BF16)
        masktri = sb("b_masktri", [128, 4, 512], BF16)
        ident = sb("b_ident", [128, 128], BF16)
        onesf = sb("b_onesf", [128, 64], F32)
        kms = sb("b_kms", [128, 16], F32)
        kmb = [sb(f"b_kmb{i}", [128, 16], BF16) for i in range(2)]
        bsm = sb("b_bsm", [128, NT, 16], F32)
        m8 = sb("b_m8", [128, NT, 8], F32)
        sel = sb("b_sel", [128, NT, 16], F32)
        PT = [sb(f"b_PT{i}", [128, 2, 512], BF16) for i in range(3)]
        rden = [sb(f"b_rden{i}", [128, 512], F32) for i in range(2)]
        bc_sb = sb("b_bcsb", [128, 512], F32)
        oT_sb = [sb(f"b_oT{i}", [128, 512], BF16) for i in range(2)]
        st_ps = [ps(f"b_st{i}", [128, 2, 512], F32) for i in range(2)]
        o_ps = [ps(f"b_o{i}", [128, 512], F32) for i in range(2)]
        shps = ps("b_sh", [128, 512], F32)
        bs_ps = shps[:, 0:NT * 16].rearrange("p (t n) -> p t n", n=16)
        tp = shps[:].bitcast(BF16).rearrange("p (a b) -> p a b", b=128)
        bc_ps = ps("b_bc", [128, 512], F32)

        for i in range(2):
            P.op("dve", (lambda i: lambda e: e.memset(kaug[i][64:96, :], 0.0))(i), [], [f"kaug_s{i}"])
            dma(P, "sp", kaug[i][64:82, :], c.kaug_static, [], [f"kaug_s{i}"])
        dma(P, "sp", vall[:], c.vS.rearrange("(t p) n -> p t n", p=128), [], ["vall"])
        dma(P, "sp", valid01[:], c.valid01, [], ["valid01"])
        dma(P, "sp", own01[:], c.own01, [], ["own01"])
        dma(P, "sp", maskv[:], c.maskv, [], ["maskv"])
        dma(P, "sp", kbias[:], c.kbias, [], ["kbias"])
        dma(P, "sp", qlo[:], c.qlo, [], ["qlo"])
        dma(P, "sp", qhi[:], c.qhi, [], ["qhi"])
        dma(P, "sp", masktri[:], c.masktri.rearrange("i p n -> p i n"), [], ["masktri"])
        dma(P, "sp", ident[:], c.ident_bf, [], ["ident"])
        P.op("dve", lambda e: e.memset(onesf[:], 1.0), [], ["onesf"])
        P.op("dve", lambda e: e.memset(augtok[:], 0.0), [], ["augtok"])
        for i in range(2):
            P.op("dve", (lambda i: lambda e: e.memset(vaug[i][:], 0.0))(i), [], [f"vaug{i}"])
            P.op("dve", (lambda i: lambda e: e.memset(vaug[i][:, :, DH:DH + 1], 1.0))(i), [], [f"vaug{i}"])

        def prologue(h):
            hp = h % 2
            hs = slice(h * DH, (h + 1) * DH)
            KA, QA, VA, ST, KM = f"kaug{hp}", f"qaug{hp}", f"vaug{hp}", f"stat{hp}", f"kmb{hp}"
            dma(P, "sp", kaug[hp][0:64, :], c.kT[hs, :], [], [KA])
            dma(P, "sp", qaug[hp][0:64, :], c.qT[hs, :], [], [QA])
            dma(P, "sp", stat[hp][:], c.stat[h], [], [ST])
            cp(P, "pool", vaug[hp][:, :, 0:DH], vall[:, :, hs], ["vall"], [VA])
            yield
            P.op("dve", lambda e: e.tensor_reduce(out=kms[0:64, 0:S // MB], in_=kaug[hp][0:64, :].rearrange("p (n k) -> p n k", k=MB),
                                                  axis=AX.X, op=ALU.add), [KA], ["kms"])
            P.op("act", lambda e: e.mul(out=kmb[hp][0:64, 0:S // MB], in_=kms[0:64, 0:S // MB], mul=1.0 / MB), ["kms"], [KM])
            if S // MB < 16:
                P.op("dve", lambda e: e.memset(kmb[hp][0:64, S // MB:16], 0.0), [], [KM])
            yield
            for t0 in range(0, NT, 8):
                for t in range(t0, t0 + 8):
                    mm(P, bs_ps[:, t, :], qaug[hp][0:64, t * 128:(t + 1) * 128], kmb[hp][0:64, :], True, True, [QA, KM], ["shps"])
                yield
            tt(P, "dve", bsm[:], bs_ps, maskv[:], ALU.add, ["shps", "maskv"], ["bsm"])
            for t0 in range(0, NT, 8):
                for t in range(t0, t0 + 8):
                    P.op("dve", (lambda t: lambda e: e.max(out=m8[:, t, :], in_=bsm[:, t, :]))(t), ["bsm"], ["m8"])
                yield
            tt(P, "dve", sel[:], bsm[:], m8[:, :, 2:3].to_broadcast([128, NT, 16]), ALU.is_ge, ["bsm", "m8"], ["sel"])
            tt(P, "dve", sel[:], sel[:], valid01[:], ALU.mult, ["sel", "valid01"], ["sel"])
            tt(P, "dve", sel[:], sel[:], own01[:], ALU.add, ["sel", "own01"], ["sel"])
            ts(P, "dve", sel[:], sel[:], -1.0, BIG, ALU.add, ALU.mult, ["sel"], ["sel"])
            tt(P, "dve", augtok[:, :, 65:81], sel[:], stat[hp][:], ALU.add, ["sel", ST], ["augtok"])
            cp(P, "dve", augtok[:, :, 64], qlo[:, h:h + 1].to_broadcast([128, NT]), ["qlo"], ["augtok"])
            cp(P, "dve", augtok[:, :, 81], qhi[:, h:h + 1].to_broadcast([128, NT]), ["qhi"], ["augtok"])
            yield
            for t0 in range(0, NT, 8):
                for j in range(8):
                    tr(P, tp[0:96, j, :], augtok[:, t0 + j, :], ident[:], ["augtok", "ident"], ["shps"])
                cp(P, "dve", qaug[hp][64:96, t0 * 128:(t0 + 8) * 128].rearrange("p (a b) -> p a b", b=128), tp[64:96, :, :],
                   ["shps"], [QA])
                yield

        def advance(gen, n):
            if gen is None:
                return None
            for _ in range(n):
                try:
                    next(gen)
                except StopIteration:
                    return None
            return gen

        sr, pr, orr, osr = Rot(2), Rot(3), Rot(2), Rot(2)
        gen = prologue(0)
        while gen is not None:
            gen = advance(gen, 1)
        if preload is not None:
            preload()
        for h in range(NH):
            hp = h % 2
            hs = slice(h * DH, (h + 1) * DH)
            KA, QA, VA = f"kaug{hp}", f"qaug{hp}", f"vaug{hp}"
            units = [(qb, kp) for qb in range(NQB) for kp in range(2 * qb + 2)]
            NU = len(units)
            sbuf_of = {}
            obuf_of = {}
            pending = []

            def col0(qb, kt):
                return (kt - 4 * qb) * 128 if kt >= 4 * qb else 0

            def emit_s(n):
                qb, kp = units[n]
                si = sr.next()
                sbuf_of[n] = si
                for half in range(2):
                    kt = 2 * kp + half
                    diag = kt >= 4 * qb
                    c0 = col0(qb, kt)
                    mm(P, st_ps[si][:, half, c0:512], kaug[hp][0:96, kt * 128:(kt + 1) * 128],
                       qaug[hp][0:96, qb * 512 + c0:(qb + 1) * 512], True, not diag, [KA, f"kaug_s{hp}", QA], [f"st{si}"])
                    if diag:
                        mm(P, st_ps[si][:, half, c0:c0 + 128], ident[:], masktri[:, kt - 4 * qb, c0:c0 + 128], False, True,
                           ["ident", "masktri"], [f"st{si}"])

            def emit_pv(n):
                qb, kp = units[n]
                nkt = 4 * qb + 4
                si = sbuf_of.pop(n)
                if kp == 0:
                    obuf_of[qb] = orr.next()
                oi = obuf_of[qb]
                pi = pr.next()
                cmin = col0(qb, 2 * kp)
                act(P, PT[pi][:, :, cmin:512], st_ps[si][:, :, cmin:512], AF.Exp, [f"st{si}", "kbias"], [f"PT{pi}"],
                    bias=kbias[:, h, 0:1])
                for half in range(2):
                    kt = 2 * kp + half
                    c0 = col0(qb, kt)
                    mm(P, o_ps[oi][0:96, c0:512], vaug[hp][:, kt, :], PT[pi][:, half, c0:512], kt == 0, kt == nkt - 1,
                       [VA, f"PT{pi}"], [f"o{oi}"])
                if kp == 2 * qb + 1:
                    o = o_ps[oi]
                    ri = qb % 2
                    P.op("dve", lambda e: e.reciprocal(out=rden[ri][64:65, :], in_=o[64:65, :]), [f"o{oi}"], [f"rden{ri}"])

                    def fin2(qb=qb, oi=oi, o=o, ri=ri):
                        mm(P, bc_ps[0:64, :], onesf[64:65, 0:64], rden[ri][64:65, :], True, True, ["onesf", f"rden{ri}"], ["bc_ps"])
                        cp(P, "act", bc_sb[0:64, :], bc_ps[0:64, :], ["bc_ps"], ["bc_sb"])
                        oo = osr.next()
                        tt(P, "dve", oT_sb[oo][0:64, :], o[0:64, :], bc_sb[0:64, :], ALU.mult, [f"o{oi}", "bc_sb"], [f"oT{oo}"])
                        dma(P, "pool", c.oT[hs, qb * 512:(qb + 1) * 512], oT_sb[oo][0:64, :], [f"oT{oo}"], [])
                    pending.append((n + 1, fin2))

            gen = prologue(h + 1) if h + 1 < NH else None
            emit_s(0)
            for n in range(NU):
                if n + 1 < NU:
                    emit_s(n + 1)
                emit_pv(n)
                while pending and pending[0][0] <= n:
                    pending.pop(0)[1]()
                if n >= 4 and n % 2 == 0:
                    gen = advance(gen, 1)
            while pending:
                pending.pop(0)[1]()
            while gen is not None:
                gen = advance(gen, 1)
        P.barrier()
        P.emit()


def alloc_c_weights(nc, st):
    sb = lambda name, shape, dt: st.enter_context(nc.sbuf_tensor(name, list(shape), dt))
    wpw = sb("c_wpw", [128, 4, D], BF16)
    wao = sb("c_wao", [128, NH // 2, D], BF16)
    wout = sb("c_wout", [128, 8, D], BF16)
    dg = sb("c_dg", [128, 4, CW, 128], BF16)
    identf = sb("c_identf", [128, 128], F32)
    convw = sb("c_convw", [128, 4, CW], F32)
    convb = sb("c_convb", [128, 4], F32)
    lng = sb("c_lng", [128, 4], F32)
    lnb = sb("c_lnb", [128, 4], F32)
    bpw = sb("c_bpw", [128, 8], F32)
    epsc = sb("c_eps", [128, 1], F32)
    onesm = sb("c_onesm", [128, 128], F32)
    return (wpw, wao, wout, dg, identf, convw, convb, lng, lnb, bpw, epsc, onesm)


def load_c_weights(nc, P, c, wts, wst):
    (wpw, wao, wout, dg, identf, convw, convb, lng, lnb, bpw, epsc, onesm) = wts
    for name, t, src in (("identf", identf, c.ident_f), ("convw", convw, c.conv_wT), ("convb", convb, c.conv_bc),
                         ("lng", lng, c.ln_gc), ("lnb", lnb, c.ln_bc), ("bpw", bpw, c.b_pwc), ("epsc", epsc, c.epsc)):
        dma(P, "sp", t[:], src, [], [name])
    P.op("dve", lambda e: e.memset(onesm[:], 1.0 / CC), [], ["onesm"])
    dma(P, "pool", wpw[:], c.w_pw.rearrange("(k p) n -> p k n", p=128), [], ["wpw"])
    dma(P, "pool", wao[:], c.w_ao.rearrange("(k p) n -> p k n", p=128), [], ["wao"])
    dma(P, "pool", wout[:], c.w_out.rearrange("(k p) n -> p k n", p=128), [], ["wout"])
    for cc in range(4):
        tt(P, "dve", dg[:, cc, :, :], identf[:].unsqueeze(1).to_broadcast([128, CW, 128]),
           convw[:, cc, :].unsqueeze(2).to_broadcast([128, CW, 128]), ALU.mult, ["identf", "convw"], ["dg"])


def phase_c(nc, P, c, wts):
    S = c.S
    NBLK = S // 512
    HALO = CW - 1
    with ExitStack() as st:
        sb = lambda name, shape, dt: st.enter_context(nc.sbuf_tensor(name, list(shape), dt))
        ps = lambda name, shape, dt: st.enter_context(nc.psum_tensor(name, list(shape), dt))
        (wpw, wao, wout, dg, identf, convw, convb, lng, lnb, bpw, epsc, onesm) = wts
        zcb = [sb(f"c_zcb{i}", [128, 4, 512 + HALO], BF16) for i in range(2)]
        zconv = sb("c_zconv", [128, 4, 512], F32)
        zsq = sb("c_zsq", [128, 4, 512], F32)
        mean_sb = sb("c_mean", [128, 512], F32)
        msq = sb("c_msq", [128, 512], F32)
        var = sb("c_var", [128, 512], F32)
        rstd = sb("c_rstd", [128, 512], F32)
        t1 = [sb(f"c_t1{i}", [128, 512], F32) for i in range(2)]
        zact = sb("c_zact", [128, 4, 512], BF16)
        oTb = [sb(f"c_oTb{i}", [128, NH // 2, 512], BF16) for i in range(2)]
        sgcb = [sb(f"c_sgcb{i}", [128, 8, 512], BF16) for i in range(2)]
        sgab = [sb(f"c_sgab{i}", [128, 8, 512], BF16) for i in range(2)]
        mixc = [sb(f"c_mixc{i}", [128, 512], F32) for i in range(2)]
        mixa = [sb(f"c_mixa{i}", [128, 512], F32) for i in range(2)]
        mix = sb("c_mix", [128, 8, 512], BF16)
        xt = [sb(f"c_xt{i}", [128, D], F32) for i in range(2)]
        x1t = [sb(f"c_x1t{i}", [128, D], F32) for i in range(2)]
        cv_ps = [ps(f"c_cv{i}", [128, 512], F32) for i in range(2)]
        mean_ps = ps("c_meanps", [128, 512], F32)
        ex2_ps = ps("c_ex2ps", [128, 512], F32)
        y_ps = [ps(f"c_y{i}", [128, 512], F32) for i in range(2)]
        o_ps = [ps(f"c_op{i}", [128, 512], F32) for i in range(2)]

        zc_v = c.zcT.rearrange("(c p) s -> p c s", p=128)
        sgc_v = c.sgc.rearrange("(f p) s -> p f s", p=128)
        sga_v = c.sga.rearrange("(f p) s -> p f s", p=128)
        oT_v = c.oT.rearrange("(h d) s -> d h s", d=2 * DH)
        x_v = c.x.rearrange("(t p) d -> t p d", p=128)
        x1_v = c.x1.rearrange("(t p) d -> t p d", p=128)
        cr, tr1, yr, opr, xr, mr = Rot(2), Rot(2), Rot(2), Rot(2), Rot(2), Rot(2)
        for b in range(NBLK):
            bi = b % 2
            cols = slice(b * 512, (b + 1) * 512)
            if b == 0:
                P.op("dve", lambda e: e.memset(zcb[0][:, :, 0:HALO], 0.0), [], ["zcb0"])
                dma(P, "sp", zcb[0][:, :, HALO:], zc_v[:, :, 0:512], [], ["zcb0"])
            else:
                dma(P, "sp", zcb[bi][:], zc_v[:, :, b * 512 - HALO:(b + 1) * 512], [], [f"zcb{bi}"])
            dma(P, "sp", sgcb[bi][:], sgc_v[:, :, cols], [], [f"sgcb{bi}"])
            dma(P, "sp", sgab[bi][:], sga_v[:, :, cols], [], [f"sgab{bi}"])
            dma(P, "sp", oTb[bi][:], oT_v[:, :, cols], [], [f"oTb{bi}"])
            for cc in range(4):
                ci = cr.next()
                for k in range(CW):
                    mm(P, cv_ps[ci][:], dg[:, cc, k, :], zcb[bi][:, cc, k:k + 512], k == 0, k == CW - 1,
                       ["dg", f"zcb{bi}"], [f"cv{ci}"])
                act(P, zconv[:, cc, :], cv_ps[ci][:], AF.Identity, [f"cv{ci}", "convb"], [f"zconv{cc}"], bias=convb[:, cc:cc + 1])
                act(P, zsq[:, cc, :], zconv[:, cc, :], AF.Square, [f"zconv{cc}"], [f"zsq{cc}"])
            for cc in range(4):
                mm(P, mean_ps[:], onesm[:], zconv[:, cc, :], cc == 0, cc == 3, ["onesm", f"zconv{cc}"], ["mean_ps"])
            for cc in range(4):
                mm(P, ex2_ps[:], onesm[:], zsq[:, cc, :], cc == 0, cc == 3, ["onesm", f"zsq{cc}"], ["ex2_ps"])
            cp(P, "act", mean_sb[:], mean_ps[:], ["mean_ps"], ["mean_sb"])
            act(P, msq[:], mean_ps[:], AF.Square, ["mean_ps"], ["msq"])
            tt(P, "dve", var[:], ex2_ps[:], msq[:], ALU.subtract, ["ex2_ps", "msq"], ["var"])
            act(P, var[:], var[:], AF.Sqrt, ["var", "epsc"], ["var"], bias=epsc[:], scale=1.0)
            P.op("dve", lambda e: e.reciprocal(out=rstd[:], in_=var[:]), ["var"], ["rstd"])
            for cc in range(4):
                ti = tr1.next()
                tt(P, "dve", t1[ti][:], zconv[:, cc, :], mean_sb[:], ALU.subtract, [f"zconv{cc}", "mean_sb"], [f"t1{ti}"])
                tt(P, "dve", t1[ti][:], t1[ti][:], rstd[:], ALU.mult, [f"t1{ti}", "rstd"], [f"t1{ti}"])
                act(P, zact[:, cc, :], t1[ti][:], AF.Silu, [f"t1{ti}", "lng", "lnb"], [f"zact{cc}"],
                    scale=lng[:, cc:cc + 1], bias=lnb[:, cc:cc + 1])
            zall = [f"zact{cc}" for cc in range(4)]
            if c.dbg is not None and b == 0:
                dma(P, "sp", c.dbg[1, :, 0:512], mean_sb[:], ["mean_sb"], [])
                dma(P, "sp", c.dbg[1, :, 512:1024], var[:], ["var"], [])
                dma(P, "sp", c.dbg[1, :, 1024:1536], rstd[:], ["rstd"], [])
            for f in range(8):
                fs = slice(f * 128, (f + 1) * 128)
                yi = yr.next()
                for cc in range(4):
                    mm(P, y_ps[yi][:], wpw[:, cc, fs], zact[:, cc, :], cc == 0, cc == 3, ["wpw"] + zall, [f"y{yi}"])
                mi = mr.next()
                act(P, mixc[mi][:], y_ps[yi][:], AF.Identity, [f"y{yi}", "bpw"], [f"mixc{mi}"], bias=bpw[:, f:f + 1])
                tt(P, "dve", mixc[mi][:], mixc[mi][:], sgcb[bi][:, f, :], ALU.mult, [f"mixc{mi}", f"sgcb{bi}"], [f"mixc{mi}"])
                yi2 = yr.next()
                for h in range(NH // 2):
                    mm(P, y_ps[yi2][:], wao[:, h, fs], oTb[bi][:, h, :], h == 0, h == NH // 2 - 1,
                       ["wao", f"oTb{bi}"], [f"y{yi2}"])
                tt(P, "dve", mixa[mi][:], y_ps[yi2][:], sgab[bi][:, f, :], ALU.mult, [f"y{yi2}", f"sgab{bi}"], [f"mixa{mi}"])
                tt(P, "pool", mix[:, f, :], mixa[mi][:], mixc[mi][:], ALU.add, [f"mixa{mi}", f"mixc{mi}"], [f"mix{f}"])
            mall = [f"mix{f}" for f in range(8)]
            for j in range(4):
                t = b * 4 + j
                xi = xr.next()
                dma(P, "sp", xt[xi][:], x_v[t], [], [f"xt{xi}"])
                for half in range(2):
                    oi = opr.next()
                    hsl = slice(half * 512, (half + 1) * 512)
                    for f in range(8):
                        mm(P, o_ps[oi][:], mix[:, f, j * 128:(j + 1) * 128], wout[:, f, hsl], f == 0, f == 7,
                           mall + ["wout"], [f"op{oi}"])
                    tt(P, "dve", x1t[xi][:, hsl], xt[xi][:, hsl], o_ps[oi][:], ALU.add, [f"xt{xi}", f"op{oi}"], [f"x1t{xi}"])
                dma(P, "pool", x1_v[t], x1t[xi][:], [f"x1t{xi}"], [])
        P.barrier()
        P.emit()


def phase_d(nc, P, c):
    S = c.S
    NT = S // 128
    G = 2
    NB_G = 8
    NSL = PH * 16
    with ExitStack() as st:
        sb = lambda name, shape, dt: st.enter_context(nc.sbuf_tensor(name, list(shape), dt))
        ps = lambda name, shape, dt: st.enter_context(nc.psum_tensor(name, list(shape), dt))
        wq = sb("d_wq", [128, 8, PH * 256], BF16)
        kT = [sb(f"d_keysT{i}", [128, NK], BF16) for i in range(2)]
        kTf = sb("d_keysTf", [128, NK], F32)
        g2rep = sb("d_g2rep", [128, D], F32)
        gfrep = sb("d_gfrep", [128, D], F32)
        identb = sb("d_identb", [128, 128], BF16)
        epsc = sb("d_eps", [128, 1], F32)
        iota16 = sb("d_iota16", [128, 16], F32)
        thr17 = sb("d_thr17", [128, 17], F32)
        x1t = [sb(f"d_x1t{i}", [128, D], F32) for i in range(3)]
        tmp = sb("d_tmp", [128, D], F32)
        hn2 = sb("d_hn2", [128, D], F32)
        hn2b = [sb(f"d_hn2b{i}", [128, D], BF16) for i in range(2)]
        hn2T = sb("d_hn2T", [128, 8, 128], BF16)
        ss = sb("d_ss", [128, 1], F32)
        std = sb("d_std", [128, 1], F32)
        rstd = sb("d_rstd", [128, 1], F32)
        fss = sb("d_fss", [128, 1], F32)
        fstd = sb("d_fstd", [128, 1], F32)
        frstd = sb("d_frstd", [128, 1], F32)
        qT_sb = sb("d_qT", [128, 16, 128], BF16)
        s_sb = sb("d_s", [128, 16, NK], F32)
        gebuf = sb("d_ge", [128, PH * 16 * 17], F32)
        ohbuf = sb("d_oh", [128, PH * 256], F32)
        s2 = gebuf[:, 0:16 * NK].rearrange("p (g n) -> p g n", n=NK)
        ge = gebuf[:].rearrange("p (h k a) -> p h k a", k=16, a=17)
        cand2 = ohbuf[:].rearrange("p (h n) -> p h n", n=256)
        oh = ohbuf[:].rearrange("p (h k a) -> p h k a", k=16, a=16)
        m16 = sb("d_m16", [128, 16, 16], F32)
        i16 = sb("d_i16", [128, 16, 16], U32)
        i16f = sb("d_i16f", [128, 16, 16], F32)
        cand = sb("d_cand", [128, PH, 256], F32)
        sc = sb("d_sc", [128, PH, 16], F32)
        pos = sb("d_pos", [128, PH, 16], U32)
        posf = sb("d_posf", [128, PH, 16], F32)
        af = sb("d_af", [128, PH, 16], F32)
        bf_ = sb("d_bf", [128, PH, 16], F32)
        i1s = sb("d_i1s", [128, PH, 16], F32)
        i2s = sb("d_i2s", [128, PH, 16], F32)
        eidx = [sb(f"d_eidx{i}", [128, NSL], U32) for i in range(2)]
        ee = sb("d_ee", [128, PH, 16], F32)
        zz = sb("d_zz", [128, PH], F32)
        gg = [sb(f"d_gg{i}", [128, NSL], F32) for i in range(2)]
        adot = sb("d_adot", [128, NSL], F32)
        ga = sb("d_ga", [128, NSL], F32)
        cw = sb("d_cw", [128, NSL], F32)
        uvg = [sb(f"d_uvg{i}", [128, G, 2, D], BF16) for i in range(NB_G)]
        prodb = [sb(f"d_prodb{i}", [128, D], BF16) for i in range(4)]
        junkb = sb("d_junkb", [128, D], BF16)
        dgc = [sb(f"d_dgc{i}", [128, 128], BF16) for i in range(8)]
        x2 = sb("d_x2", [128, D], F32)
        ot = sb("d_ot", [128, D], F32)
        tp = ps("d_tp", [128, 8, 128], BF16)
        qs_ps = [ps(f"d_qs{i}", [128, 4, 128], F32) for i in range(2)]
        y_ps = [[ps(f"d_y{i}{h}", [128, 512], F32) for h in range(2)] for i in range(2)]
        fold_ps = ps("d_fold", [128, 512], F32)
        junks = sb("d_junks", [128, 128], BF16)

        for name, t_, src in (("g2rep", g2rep, c.g2rep), ("gfrep", gfrep, c.gfrep),
                              ("identb", identb, c.ident_bf), ("epsc", epsc, c.epsc), ("iota16", iota16, c.iota16)):
            dma(P, "sp", t_[:], src, [], [name])
        for i, src in enumerate((c.keys1T, c.keys2T)):
            dma(P, "pool", kT[i][:], src, [], [f"kT{i}"])
        ts(P, "dve", thr17[:, 0:16], iota16[:], 16.0, None, ALU.mult, None, ["iota16"], ["thr17"])
        P.op("dve", lambda e: e.memset(thr17[:, 16:17], 256.0), [], ["thr17b"])
        dma(P, "pool", wq[:], c.w_pq.rearrange("(k p) n -> p k n", p=128), [], ["wq"])

        x1_v = c.x1.rearrange("(t p) d -> t p d", p=128)
        out_v = c.out.rearrange("(t p) d -> t p d", p=128)
        uv_v = c.uv.rearrange("e s d -> e (s d)")
        m16v = m16[:].rearrange("p (h s) k -> p h s k", s=2)
        i16fv = i16f[:].rearrange("p (h s) k -> p h s k", s=2)
        cand4 = cand[:].rearrange("p h (a b) -> p h a b", b=16)
        B4 = [128, PH, 16, 16]

        def prologue(t):
            par = t % 2
            xp = t % 3
            X, HB, EI, GG = f"x1t{xp}", f"hn2b{par}", f"eidx{par}", f"gg{par}"
            dma(P, "sp", x1t[xp][:], x1_v[t], [], [X])
            yield
            act(P, junkb[:], x1t[xp][:], AF.Square, [X], ["junkb", "ss"], accum_out=ss[:])
            yield
            act(P, std[:], ss[:], AF.Sqrt, ["ss", "epsc"], ["std"], bias=epsc[:], scale=1.0 / D)
            yield
            P.op("dve", lambda e: e.reciprocal(out=rstd[:], in_=std[:]), ["std"], ["rstd"])
            yield
            act(P, tmp[:], x1t[xp][:], AF.Copy, [X, "rstd"], ["tmp"], scale=rstd[:])
            yield
            tt(P, "dve", hn2[:], tmp[:], g2rep[:], ALU.mult, ["tmp", "g2rep"], ["hn2"])
            yield
            cp(P, "act", hn2b[par][:], hn2[:], ["hn2"], [HB])
            yield
            for k in range(8):
                tr(P, tp[:, k, :], hn2b[par][:, k * 128:(k + 1) * 128], identb[:], [HB, "identb"], ["tp"])
            yield
            cp(P, "dve", hn2T[:], tp[:], ["tp"], ["hn2T"])
            yield

            def qmm(g4):
                for gi in range(4):
                    g = g4 * 4 + gi
                    for k in range(8):
                        mm(P, qs_ps[g4 % 2][:, gi, :], wq[:, k, g * 128:(g + 1) * 128], hn2T[:, k, :], k == 0, k == 7,
                           ["wq", "hn2T"], [f"qs{g4 % 2}"])

            def qcp(g4):
                cp(P, "act", qT_sb[:, g4 * 4:(g4 + 1) * 4, :], qs_ps[g4 % 2][:], [f"qs{g4 % 2}"], [f"qT{g4}"])

            def smm(g4):
                for gi in range(4):
                    g = g4 * 4 + gi
                    mm(P, qs_ps[g4 % 2][:, gi, :], qT_sb[:, g, :], kT[g % 2][:], True, True, [f"qT{g4}", f"kT{g%2}"], [f"qs{g4 % 2}"])

            def scp(g4):
                cp(P, "act", s_sb[:, g4 * 4:(g4 + 1) * 4, :], qs_ps[g4 % 2][:], [f"qs{g4 % 2}"], [f"s{g4}"])

            for g4 in range(5):
                if g4 < 4:
                    qmm(g4)
                if g4 >= 1:
                    qcp(g4 - 1)
                yield
            for g4 in range(5):
                if g4 < 4:
                    smm(g4)
                if g4 >= 1:
                    scp(g4 - 1)
                yield
            for g in range(16):
                P.op("dve", (lambda g: lambda e: e.max(out=m16[:, g, 0:8], in_=s_sb[:, g, :]))(g), [f"s{g // 4}"], [f"m16a{g}"])
                if g % 4 == 3:
                    yield
            for g in range(16):
                P.op("dve", (lambda g: lambda e: e.max_index(out=i16[:, g, 0:8], in_max=m16[:, g, 0:8], in_values=s_sb[:, g, :]))(g),
                     [f"s{g // 4}", f"m16a{g}"], [f"i16a{g}"])
                if g % 4 == 3:
                    yield
            for g in range(16):
                P.op("dve", (lambda g: lambda e: e.match_replace(out=s2[:, g, :], in_to_replace=m16[:, g, 0:8],
                                                                 in_values=s_sb[:, g, :], imm_value=NEG))(g),
                     [f"s{g // 4}", f"m16a{g}"], [f"s2_{g}"])
                if g % 4 == 3:
                    yield
            for g in range(16):
                P.op("dve", (lambda g: lambda e: e.max(out=m16[:, g, 8:16], in_=s2[:, g, :]))(g), [f"s2_{g}"], [f"m16b{g}"])
                if g % 4 == 3:
                    yield
            for g in range(16):
                P.op("dve", (lambda g: lambda e: e.max_index(out=i16[:, g, 8:16], in_max=m16[:, g, 8:16], in_values=s2[:, g, :]))(g),
                     [f"s2_{g}", f"m16b{g}"], [f"i16b{g}"])
                if g % 4 == 3:
                    yield
            m16all = [f"m16a{g}" for g in range(16)] + [f"m16b{g}" for g in range(16)]
            i16all = [f"i16a{g}" for g in range(16)] + [f"i16b{g}" for g in range(16)]
            cp(P, "dve", i16f[:], i16[:], i16all, ["i16f"])
            tt(P, "dve", cand4, m16v[:, :, 0, :].unsqueeze(3).to_broadcast(B4), m16v[:, :, 1, :].unsqueeze(2).to_broadcast(B4),
               ALU.add, m16all, ["cand"])
            yield
            for h in range(PH):
                P.op("dve", (lambda h: lambda e: e.max(out=sc[:, h, 0:8], in_=cand[:, h, :]))(h), ["cand"], [f"sca{h}"])
                if h % 4 == 3:
                    yield
            for h in range(PH):
                P.op("dve", (lambda h: lambda e: e.max_index(out=pos[:, h, 0:8], in_max=sc[:, h, 0:8], in_values=cand[:, h, :]))(h),
                     ["cand", f"sca{h}"], [f"posa{h}"])
                if h % 4 == 3:
                    yield
            for h in range(PH):
                P.op("dve", (lambda h: lambda e: e.match_replace(out=cand2[:, h, :], in_to_replace=sc[:, h, 0:8],
                                                                 in_values=cand[:, h, :], imm_value=NEG))(h),
                     ["cand", f"sca{h}"], [f"c2_{h}"])
                if h % 4 == 3:
                    yield
            for h in range(PH):
                P.op("dve", (lambda h: lambda e: e.max(out=sc[:, h, 8:16], in_=cand2[:, h, :]))(h), [f"c2_{h}"], [f"scb{h}"])
                if h % 4 == 3:
                    yield
            for h in range(PH):
                P.op("dve", (lambda h: lambda e: e.max_index(out=pos[:, h, 8:16], in_max=sc[:, h, 8:16], in_values=cand2[:, h, :]))(h),
                     [f"c2_{h}", f"scb{h}"], [f"posb{h}"])
                if h % 4 == 3:
                    yield
            scall = [f"sca{h}" for h in range(PH)] + [f"scb{h}" for h in range(PH)]
            posall = [f"posa{h}" for h in range(PH)] + [f"posb{h}" for h in range(PH)]
            cp(P, "dve", posf[:], pos[:], posall, ["posf"])
            tt(P, "dve", ge, posf[:].unsqueeze(3).to_broadcast([128, PH, 16, 17]),
               thr17[:].unsqueeze(1).unsqueeze(1).to_broadcast([128, PH, 16, 17]), ALU.is_ge, ["posf", "thr17", "thr17b"], ["ge"])
            yield
            tt(P, "dve", oh, ge[:, :, :, 0:16], ge[:, :, :, 1:17], ALU.subtract, ["ge"], ["oh"])
            P.op("dve", lambda e: e.tensor_reduce(out=af[:], in_=ge[:, :, :, 1:17], axis=AX.X, op=ALU.add), ["ge"], ["af"])
            yield
            tt(P, "dve", oh, oh, i16fv[:, :, 0, :].unsqueeze(2).to_broadcast(B4), ALU.mult, ["oh", "i16f"], ["oh"])
            P.op("dve", lambda e: e.tensor_reduce(out=i1s[:], in_=oh, axis=AX.X, op=ALU.add), ["oh"], ["i1s"])
            ts(P, "dve", bf_[:], af[:], -16.0, None, ALU.mult, None, ["af"], ["bf"])
            tt(P, "dve", bf_[:], bf_[:], posf[:], ALU.add, ["bf", "posf"], ["bf"])
            yield
            tt(P, "dve", oh, iota16[:].unsqueeze(1).unsqueeze(1).to_broadcast(B4), bf_[:].unsqueeze(3).to_broadcast(B4),
               ALU.is_equal, ["iota16", "bf", "i1s"], ["oh"])
            tt(P, "dve", oh, oh, i16fv[:, :, 1, :].unsqueeze(2).to_broadcast(B4), ALU.mult, ["oh", "i16f"], ["oh"])
            P.op("dve", lambda e: e.tensor_reduce(out=i2s[:], in_=oh, axis=AX.X, op=ALU.add), ["oh"], ["i2s"])
            yield
            ts(P, "dve", i1s[:], i1s[:], float(NK), None, ALU.mult, None, ["i1s"], ["i1s"])
            tt(P, "dve", i1s[:], i1s[:], i2s[:], ALU.add, ["i1s", "i2s"], ["i1s"])
            cp(P, "dve", eidx[par][:], i1s[:].rearrange("p h k -> p (h k)"), ["i1s"], [EI])
            tt(P, "dve", ee[:], sc[:], sc[:, :, 0:1].to_broadcast([128, PH, 16]), ALU.subtract, scall, ["ee"])
            yield
            act(P, ee[:], ee[:], AF.Exp, ["ee"], ["ee"])
            yield
            P.op("dve", lambda e: e.tensor_reduce(out=zz[:], in_=ee[:], axis=AX.X, op=ALU.add), ["ee"], ["zz"])
            P.op("dve", lambda e: e.reciprocal(out=zz[:], in_=zz[:]), ["zz"], ["zz"])
            tt(P, "dve", gg[par][:].rearrange("p (h k) -> p h k", k=16), ee[:], zz[:].unsqueeze(2).to_broadcast([128, PH, 16]),
               ALU.mult, ["ee", "zz"], [GG])
            yield

        def advance(gen, n):
            if gen is None:
                return None
            for _ in range(n):
                try:
                    next(gen)
                except StopIteration:
                    return None
            return gen

        gen = prologue(0)
        while gen is not None:
            gen = advance(gen, 1)
        br, pr = Rot(NB_G), Rot(4)
        NGRP = NSL // G
        gbuf = {}

        def names(t):
            par = t % 2
            return par, f"x1t{t % 3}", f"hn2b{par}", f"eidx{par}", f"gg{par}"

        def stage_a(t, gi):
            par, X, HB, EI, GG = names(t)
            s0 = gi * G
            bi = br.next()
            gbuf[(t, gi)] = bi
            for j in range(G):
                s = s0 + j
                P.dma("pool", (lambda bi, j, s, par: lambda e: e.indirect_dma_start(
                    out=uvg[bi][:, j, :, :].rearrange("p s d -> p (s d)"), out_offset=None, in_=uv_v,
                    in_offset=bass.IndirectOffsetOnAxis(ap=eidx[par][:, s:s + 1], axis=0)))(bi, j, s, par),
                    [EI], [f"uvg{bi}_{j}"])
            for j in range(G):
                s = s0 + j
                pi = pr.next()
                tt(P, "dve", prodb[pi][:], uvg[bi][:, j, 0, :], hn2b[par][:], ALU.mult, [f"uvg{bi}_{j}", HB], [f"prodb{pi}"])
                if s % 2 == 1:
                    for cc in range(8):
                        mm(P, fold_ps[:, 0:128], identb[:], prodb[pi][:, cc * 128:(cc + 1) * 128], cc == 0, cc == 7,
                           ["identb", f"prodb{pi}"], ["fold"])
                    act(P, junks[:], fold_ps[:, 0:128], AF.Identity, ["fold"], ["junks", f"adot{s}"], accum_out=adot[:, s:s + 1])
                else:
                    act(P, junkb[:], prodb[pi][:], AF.Identity, [f"prodb{pi}"], ["junkb", f"adot{s}"], accum_out=adot[:, s:s + 1])

        def stage_b(t, gi):
            s0 = gi * G
            adg = [f"adot{s0 + j}" for j in range(G)]
            act(P, ga[:, s0:s0 + G], adot[:, s0:s0 + G], AF.Gelu, adg, [f"ga{s0}"])

        def stage_c(t, gi):
            par, X, HB, EI, GG = names(t)
            s0 = gi * G
            tt(P, "dve", cw[:, s0:s0 + G], ga[:, s0:s0 + G], gg[par][:, s0:s0 + G], ALU.mult, [f"ga{s0}", GG], [f"cw{s0}"])

        def stage_d(t, gi):
            par, X, HB, EI, GG = names(t)
            yp = y_ps[par]
            s0 = gi * G
            bi = gbuf.pop((t, gi))
            for j in range(G):
                s = s0 + j
                di = s % 8
                if s % 2 == 0:
                    act(P, dgc[di][:], identb[:], AF.Copy, ["identb", f"cw{s0}"], [f"dgc{di}"], scale=cw[:, s:s + 1])
                else:
                    ts(P, "dve", dgc[di][:], identb[:], cw[:, s:s + 1], None, ALU.mult, None, ["identb", f"cw{s0}"], [f"dgc{di}"])
            for j in range(G):
                s = s0 + j
                di = s % 8
                for half in range(2):
                    mm(P, yp[half][:], dgc[di][:], uvg[bi][:, j, 1, half * 512:(half + 1) * 512], s == 0, s == NSL - 1,
                       [f"dgc{di}", f"uvg{bi}_{j}"], [f"y{par}{half}"])

        def finalize(t):
            par, X, HB, EI, GG = names(t)
            xp = t % 3
            yp = y_ps[par]
            for half in range(2):
                hsl = slice(half * 512, (half + 1) * 512)
                tt(P, "dve", x2[:, hsl], x1t[xp][:, hsl], yp[half][:], ALU.add, [X, f"y{par}{half}"], [f"x2_{half}"])
            yield
            act(P, junkb[:], x2[:], AF.Square, ["x2_0", "x2_1"], ["junkb", "fss"], accum_out=fss[:])
            yield
            act(P, fstd[:], fss[:], AF.Sqrt, ["fss", "epsc"], ["fstd"], bias=epsc[:], scale=1.0 / D)
            yield
            P.op("dve", lambda e: e.reciprocal(out=frstd[:], in_=fstd[:]), ["fstd"], ["frstd"])
            yield
            act(P, ot[:], x2[:], AF.Copy, ["x2_0", "x2_1", "frstd"], ["ot"], scale=frstd[:])
            yield
            tt(P, "dve", ot[:], ot[:], gfrep[:], ALU.mult, ["ot", "gfrep"], ["ot"])
            yield
            dma(P, "sp", out_v[t], ot[:], ["ot"], ["out"])
            yield

        units = [(t, gi) for t in range(NT) for gi in range(NGRP)]
        NU = len(units)
        gen = None
        fgen = None
        for n in range(NU + 3):
            if n < NU:
                t, gi = units[n]
                if gi == 0 and gen is not None:
                    while gen is not None:
                        gen = advance(gen, 1)
                stage_a(t, gi)
            if 1 <= n <= NU:
                stage_b(*units[n - 1])
            if 2 <= n <= NU + 1:
                stage_c(*units[n - 2])
            if n >= 3:
                tc_, gc_ = units[n - 3]
                stage_d(tc_, gc_)
                if gc_ == NGRP - 1:
                    while fgen is not None:
                        fgen = advance(fgen, 1)
                    fgen = finalize(tc_)
            fgen = advance(fgen, 1)
            if n < NU:
                t, gi = units[n]
                if gi == 3 and t + 1 < NT:
                    gen = prologue(t + 1)
                gen = advance(gen, 1)
                if gi == NGRP - 1:
                    while gen is not None:
                        gen = advance(gen, 1)
        while fgen is not None:
            fgen = advance(fgen, 1)
        P.barrier()
        P.emit()


def build_all(nc, P, c, upto="d"):
    phase_a(nc, P, c)
    if upto < "b":
        return
    with ExitStack() as wsc:
        wts = alloc_c_weights(nc, wsc)
        phase_b(nc, P, c, preload=lambda: load_c_weights(nc, P, c, wts, None))
        if upto >= "c":
            phase_c(nc, P, c, wts)
    if upto >= "d":
        phase_d(nc, P, c)


def build_program(S):
    nc = bass.Bass("TRN2", target_bir_lowering=False)
    c = declare_io(nc, S, debug=False)
    with ExitStack() as st:
        P = Prog(nc, st)
        build_all(nc, P, c)
    return nc


def kernel(**inputs):
    x = np.asarray(inputs["x"], dtype=np.float32)
    B, S, _ = x.shape
    assert B == 8
    nc = build_program(S)
    consts = make_consts(S)
    w = {k: np.asarray(v) for k, v in inputs.items() if k != "x"}
    shared = host_inputs(S, x[0], w, consts)
    in_maps = []
    for b in range(B):
        m = dict(shared)
        m["x"] = np.ascontiguousarray(x[b])
        in_maps.append(m)
    res = run_bass_kernel_spmd(nc, in_maps, core_ids=list(range(B)))
    out = np.stack([np.asarray(res.results[b]["out"], dtype=np.float32) for b in range(B)], axis=0)
    return out
```

```python
import numpy as np
import ml_dtypes
from contextlib import ExitStack
import concourse.bass as bass
import concourse.mybir as mybir
from concourse.bass_utils import run_bass_kernel_spmd

F32 = mybir.dt.float32
BF16 = mybir.dt.bfloat16
U32 = mybir.dt.uint32
I32 = mybir.dt.int32
ALU = mybir.AluOpType
AF = mybir.ActivationFunctionType
AX = mybir.AxisListType

D = 1024
NCOL = 4608
NH = 8
DH = 64
CC = 512
CW = 31
MB = 256
PH = 8
NK = 128
TOPK = 16
EPS = 1e-6
BIG = 30000.0
NEG = -1.0e30


class Prog:
    ENG = ("pe", "dve", "act", "pool", "sp")
    DQ = ("sp", "pool", "act")

    def __init__(self, nc, stack, kdma=8):
        self.nc = nc
        self.eng = {"pe": nc.tensor, "dve": nc.vector, "act": nc.scalar, "pool": nc.gpsimd, "sp": nc.sync}
        self.sems = {}
        for e in self.ENG:
            self.sems[("c", e)] = stack.enter_context(nc.semaphore(f"c_{e}"))
        self.kdma = {"sp": kdma, "pool": 16, "act": 4}
        for q in self.DQ:
            for j in range(self.kdma[q]):
                self.sems[("d", q, j)] = stack.enter_context(nc.semaphore(f"d_{q}{j}"))
        self.latest = {k: 0 for k in self.sems}
        self.dnext = {q: 0 for q in self.DQ}
        self.ops = {e: [] for e in self.ENG}
        self.known = {e: {} for e in self.ENG}
        self.lastw = {}
        self.readers = {}
        self.nins = 0

    def _need(self, eng, key, val):
        if self.known[eng].get(key, 0) < val:
            self.known[eng][key] = val
            self.ops[eng].append(("w", key, val))

    def _deps(self, eng, reads, writes, is_dma):
        for b in reads:
            lw = self.lastw.get(b)
            if lw is not None:
                key, val = lw
                if (not is_dma) and key == ("c", eng) and eng == "pe":
                    continue
                self._need(eng, key, val)
        for b in writes:
            lw = self.lastw.get(b)
            if lw is not None:
                key, val = lw
                if is_dma or key != ("c", eng):
                    self._need(eng, key, val)
            for key, val in self.readers.get(b, {}).items():
                if is_dma or key != ("c", eng):
                    self._need(eng, key, val)

    def _commit(self, key, val, reads, writes):
        for b in writes:
            self.lastw[b] = (key, val)
            self.readers[b] = {}
        for b in reads:
            r = self.readers.setdefault(b, {})
            r[key] = max(r.get(key, 0), val)

    def op(self, eng, fn, reads=(), writes=()):
        self._deps(eng, reads, writes, False)
        key = ("c", eng)
        self.latest[key] += 1
        val = self.latest[key]
        self.ops[eng].append(("i", fn, key, 1))
        self._commit(key, val, reads, writes)
        self.nins += 1

    def dma(self, q, fn, reads=(), writes=()):
        self._deps(q, reads, writes, True)
        j = self.dnext[q]
        self.dnext[q] = (j + 1) % self.kdma[q]
        key = ("d", q, j)
        if self.latest[key] > 0:
            self._need(q, key, self.latest[key])
        self.latest[key] += 16
        val = self.latest[key]
        self.ops[q].append(("i", fn, key, 16))
        self._commit(key, val, reads, writes)
        self.nins += 1

    def barrier(self):
        for e in self.ENG:
            for key, val in self.latest.items():
                if val > 0:
                    self._need(e, key, val)
        self.lastw = {}
        self.readers = {}

    def emit(self, name=None):
        ops = self.ops
        self.ops = {e: [] for e in self.ENG}
        sems = self.sems
        nc = self.nc

        def replay(lst):
            def f(e):
                for it in lst:
                    if it[0] == "w":
                        e.wait_ge(sems[it[1]], it[2])
                    else:
                        ins = it[1](e)
                        ins.then_inc(sems[it[2]], it[3])
            return f

        with nc.Block() as block:
            block.tensor(replay(ops["pe"]))
            block.vector(replay(ops["dve"]))
            block.scalar(replay(ops["act"]))
            block.gpsimd(replay(ops["pool"]))
            block.sync(replay(ops["sp"]))


class Rot:
    def __init__(self, n):
        self.n = n
        self.i = -1

    def next(self):
        self.i = (self.i + 1) % self.n
        return self.i


def _bf(a):
    return np.ascontiguousarray(a).astype(ml_dtypes.bfloat16)


def make_consts(S):
    NT = S // 128
    NB = S // MB
    c = {}
    c["ident_bf"] = _bf(np.eye(128, dtype=np.float32))
    c["ident_f"] = np.eye(128, dtype=np.float32)
    ka = np.zeros((18, S), np.float32)
    ka[0, :] = 1.0
    for n in range(NB):
        ka[1 + n, n * MB:(n + 1) * MB] = 1.0
    ka[17, :] = ((np.arange(S) // 128) % 2).astype(np.float32)
    c["kaug_static"] = _bf(ka)
    mt = np.zeros((4, 128, 512), np.float32)
    for i in range(4):
        for j in range(4):
            if i // 2 != j // 2:
                continue
            blk = mt[i, :, j * 128:(j + 1) * 128]
            if i > j:
                blk[:] = -BIG
            elif i == j:
                kk = np.arange(128)[:, None]
                qq = np.arange(128)[None, :]
                blk[kk > qq] = -BIG
    c["masktri"] = _bf(mt)
    tile = np.arange(NT)[:, None]
    n = np.arange(16)[None, :]
    valid = (n < tile // 2).astype(np.float32)
    own = (n == tile // 2).astype(np.float32)
    c["valid01"] = np.broadcast_to(valid[None], (128, NT, 16)).astype(np.float32).copy()
    c["own01"] = np.broadcast_to(own[None], (128, NT, 16)).astype(np.float32).copy()
    c["maskv"] = ((c["valid01"] - 1.0) * 1.0e30).astype(np.float32)
    slopes = np.array([2.0 ** (-8.0 * (i + 1) / NH) for i in range(NH)], np.float32)
    stat = np.zeros((NH, 128, NT, 16), np.float32)
    for h in range(NH):
        st = -slopes[h] * (128.0 * tile - 256.0 * n)
        st = np.where(n <= tile // 2, st, 0.0)
        stat[h] = st[None]
    c["stat"] = stat
    p = np.arange(128, dtype=np.float32)
    c["qlo"] = _bf(np.stack([-slopes[h] * p for h in range(NH)], axis=1))
    c["qhi"] = _bf(np.stack([128.0 * slopes[h] * np.ones(128, np.float32) for h in range(NH)], axis=1))
    kb = np.zeros((128, NH, 2), np.float32)
    for h in range(NH):
        for half in range(2):
            kb[:, h, half] = slopes[h] * (p + 128.0 * half)
    c["kbias"] = kb
    c["iota16"] = np.broadcast_to(np.arange(16, dtype=np.float32)[None], (128, 16)).copy()
    c["epsc"] = np.full((128, 1), EPS, np.float32)
    return c


def act(P, out, in_, func, r, w, **kw):
    P.op("act", lambda e: e.activation(out=out, in_=in_, func=func, **kw), r, w)


def tt(P, eng, out, in0, in1, op, r, w):
    P.op(eng, lambda e: e.tensor_tensor(out=out, in0=in0, in1=in1, op=op), r, w)


def ts(P, eng, out, in0, s1, s2, op0, op1, r, w, **kw):
    if s2 is None:
        P.op(eng, lambda e: e.tensor_scalar(out=out, in0=in0, scalar1=s1, scalar2=None, op0=op0, **kw), r, w)
    else:
        P.op(eng, lambda e: e.tensor_scalar(out=out, in0=in0, scalar1=s1, scalar2=s2, op0=op0, op1=op1, **kw), r, w)


def cp(P, eng, out, in_, r, w):
    if eng == "act":
        P.op("act", lambda e: e.copy(out=out, in_=in_), r, w)
    else:
        P.op(eng, lambda e: e.tensor_copy(out=out, in_=in_), r, w)


def mm(P, out, lhsT, rhs, start, stop, r, w):
    P.op("pe", lambda e: e.matmul(out, lhsT, rhs, start=start, stop=stop), r, w)


def tr(P, out, in_, ident, r, w):
    P.op("pe", lambda e: e.transpose(out, in_, ident), r, w)


def dma(P, q, out, in_, r, w):
    P.dma(q, lambda e: e.dma_start(out=out, in_=in_), r, w)


def rmsnorm_stats(P, xt, junk, ss, std, rstd, epsc, tag):
    act(P, junk, xt, AF.Square, [tag + "x"], [tag + "junk", tag + "ss"], accum_out=ss)
    act(P, std, ss, AF.Sqrt, [tag + "ss", "epsc"], [tag + "std"], bias=epsc, scale=1.0 / D)
    P.op("dve", lambda e: e.reciprocal(out=rstd, in_=std), [tag + "std"], [tag + "rstd"])


class Ctx:
    pass


def declare_io(nc, S, debug=False):
    c = Ctx()
    c.S = S

    def inp(name, shape, dt=F32):
        return nc.dram_tensor(name, list(shape), dt, kind="ExternalInput").ap()

    def scr(name, shape, dt):
        return nc.dram_tensor(name, list(shape), dt, kind="ExternalOutput" if debug else "Internal").ap()

    NT = S // 128
    c.x = inp("x", [S, D])
    c.w_in = inp("w_in", [D, NCOL])
    c.g1c = inp("g1c", [128, 8])
    c.conv_wT = inp("conv_wT", [128, 4, CW])
    c.conv_bc = inp("conv_bc", [128, 4])
    c.ln_gc = inp("ln_gc", [128, 4])
    c.ln_bc = inp("ln_bc", [128, 4])
    c.b_pwc = inp("b_pwc", [128, 8])
    c.w_pw = inp("w_conv_pw", [CC, D])
    c.w_ao = inp("w_attn_out", [NH * DH, D])
    c.w_out = inp("w_out", [D, D])
    c.w_pq = inp("w_peer_q", [D, PH * 256])
    c.g2rep = inp("g2rep", [128, D])
    c.gfrep = inp("gfrep", [128, D])
    c.keys1T = inp("keys1T", [128, NK])
    c.keys2T = inp("keys2T", [128, NK])
    c.peer_u = inp("peer_u", [NK * NK, D])
    c.peer_v = inp("peer_v", [NK * NK, D])
    c.ident_bf = inp("ident_bf", [128, 128], BF16)
    c.ident_f = inp("ident_f", [128, 128])
    c.kaug_static = inp("kaug_static", [18, S], BF16)
    c.qhi = inp("qhi", [128, NH], BF16)
    c.masktri = inp("masktri", [4, 128, 512], BF16)
    c.valid01 = inp("valid01", [128, NT, 16])
    c.own01 = inp("own01", [128, NT, 16])
    c.maskv = inp("maskv", [128, NT, 16])
    c.stat = inp("stat", [NH, 128, NT, 16])
    c.qlo = inp("qlo", [128, NH], BF16)
    c.kbias = inp("kbias", [128, NH, 2])
    c.iota16 = inp("iota16", [128, 16])
    c.epsc = inp("epsc", [128, 1])
    c.out = nc.dram_tensor("out", [S, D], F32, kind="ExternalOutput").ap()
    c.zcT = scr("s_zcT", [CC, S], BF16)
    c.qT = scr("s_qT", [NH * DH, S], BF16)
    c.kT = scr("s_kT", [NH * DH, S], BF16)
    c.vS = scr("s_v", [S, NH * DH], BF16)
    c.sgc = scr("s_sgc", [D, S], BF16)
    c.sga = scr("s_sga", [D, S], BF16)
    c.oT = scr("s_oT", [NH * DH, S], BF16)
    c.x1 = scr("s_x1", [S, D], F32)
    c.uv = nc.dram_tensor("s_uv", [NK * NK, 2, D], BF16, kind="Internal").ap()
    c.dbg = scr("s_dbg", [4, 128, 2048], F32) if debug else None
    return c


def phase_a(nc, P, c):
    S = c.S
    NBLK = S // 512
    with ExitStack() as st:
        sb = lambda name, shape, dt: st.enter_context(nc.sbuf_tensor(name, list(shape), dt))
        ps = lambda name, shape, dt: st.enter_context(nc.psum_tensor(name, list(shape), dt))
        wbf = sb("a_wbf", [128, 8, NCOL], BF16)
        wst = [sb(f"a_wst{i}", [128, NCOL], F32) for i in range(2)]
        g1c = sb("a_g1c", [128, 8], F32)
        epsc = sb("a_eps", [128, 1], F32)
        ident = sb("a_ident", [128, 128], BF16)
        xt = [sb(f"a_xt{i}", [128, D], F32) for i in range(3)]
        junk = sb("a_junk", [128, D], F32)
        hn = [sb(f"a_hn{i}", [128, D], BF16) for i in range(2)]
        ss = [sb(f"a_ss{i}", [128, 1], F32) for i in range(2)]
        std = [sb(f"a_std{i}", [128, 1], F32) for i in range(2)]
        rstd = [sb(f"a_rstd{i}", [128, 1], F32) for i in range(2)]
        hnT = [sb(f"a_hnT{i}", [128, 8, 512], BF16) for i in range(2)]
        sig = [sb(f"a_sig{i}", [128, 512], F32) for i in range(2)]
        stg = [sb(f"a_stg{i}", [128, 512], BF16) for i in range(8)]
        tp = ps("a_tp", [128, 8, 128], BF16)
        acc = [ps(f"a_acc{i}", [128, 512], F32) for i in range(6)]

        dma(P, "sp", g1c[:], c.g1c, [], ["g1c"])
        dma(P, "sp", epsc[:], c.epsc, [], ["epsc"])
        dma(P, "sp", ident[:], c.ident_bf, [], ["ident"])
        w_in_v = c.w_in.rearrange("(k p) n -> k p n", p=128)
        for k in range(8):
            dma(P, "sp", wst[k % 2][:], w_in_v[k], [], [f"wst{k%2}"])
            act(P, wbf[:, k, :], wst[k % 2][:], AF.Copy, [f"wst{k%2}", "g1c"], [f"wbf{k}"], scale=g1c[:, k:k + 1])
        wall = [f"wbf{k}" for k in range(8)]

        xr, hr, sr, ar, gr = Rot(3), Rot(2), Rot(2), Rot(6), Rot(8)

        CH = 1024
        conv_list = [(ti, r0) for r0 in range(0, NK * NK, CH) for ti in range(2)]
        nstores = [0]
        c.conv_rest = conv_list[len(conv_list) // 3:]
        del conv_list[len(conv_list) // 3:]
        per = max(1, (NBLK * 36) // max(1, len(conv_list)))

        def conv_tick(force=False):
            nstores[0] += 1
            if conv_list and (force or nstores[0] % per == 0):
                ti, r0 = conv_list.pop(0)
                tab = (c.peer_u, c.peer_v)[ti]
                dma(P, "pool", c.uv[r0:r0 + CH, ti, :], tab[r0:r0 + CH, :], [], [])
        x_v = c.x.rearrange("(t p) d -> t p d", p=128)

        def store(dst, src_ps, tok, kind, scale=None):
            g = gr.next()
            npart = dst.shape[0]
            if kind == "sigmoid":
                act(P, stg[g][0:npart, :], src_ps, AF.Sigmoid, [tok], [f"stg{g}"])
            elif kind == "scale":
                act(P, stg[g][0:npart, :], src_ps, AF.Copy, [tok], [f"stg{g}"], scale=scale)
            else:
                cp(P, "dve", stg[g][0:npart, :], src_ps, [tok], [f"stg{g}"])
            dma(P, "pool", dst, stg[g][0:npart, :], [f"stg{g}"], [])
            conv_tick()

        for b in range(NBLK):
            hb = hr.next()
            for j in range(4):
                t = b * 4 + j
                xi = xr.next()
                si = sr.next()
                dma(P, "sp", xt[xi][:], x_v[t], [], [f"xt{xi}x"])
                rmsnorm_stats(P, xt[xi][:], junk[:], ss[si][:], std[si][:], rstd[si][:], epsc[:], f"xt{xi}")
                act(P, hn[si][:], xt[xi][:], AF.Copy, [f"xt{xi}x", f"xt{xi}rstd"], [f"hn{si}"], scale=rstd[si][:])
                for k in range(8):
                    tr(P, tp[:, k, :], hn[si][:, k * 128:(k + 1) * 128], ident[:], [f"hn{si}", "ident"], ["tp"])
                cp(P, "dve", hnT[hb][:, :, j * 128:(j + 1) * 128], tp[:], ["tp"], [f"hnT{hb}"])
            cols = slice(b * 512, (b + 1) * 512)

            def proj(c0, m):
                a = ar.next()
                for k in range(8):
                    mm(P, acc[a][0:m, :], wbf[:, k, c0:c0 + m], hnT[hb][:, k, :], k == 0, k == 7,
                       wall + [f"hnT{hb}"], [f"acc{a}"])
                return acc[a], f"acc{a}"

            for cc in range(4):
                pa, ta = proj(cc * 128, 128)
                pg, tg = proj(CC + cc * 128, 128)
                s_i = sr.next()
                act(P, sig[s_i][:], pg[:], AF.Sigmoid, [tg], [f"sig{s_i}"])
                g = gr.next()
                tt(P, "dve", stg[g][:], pa[:], sig[s_i][:], ALU.mult, [ta, f"sig{s_i}"], [f"stg{g}"])
                dma(P, "pool", c.zcT[cc * 128:(cc + 1) * 128, cols], stg[g][:], [f"stg{g}"], [])
                conv_tick()
            for cc in range(4):
                pq, tq = proj(2 * CC + cc * 128, 128)
                store(c.qT[cc * 128:(cc + 1) * 128, cols], pq[:], tq, "scale", scale=DH ** -0.5)
            for cc in range(4):
                pk, tk = proj(2 * CC + 512 + cc * 128, 128)
                store(c.kT[cc * 128:(cc + 1) * 128, cols], pk[:], tk, "copy")
            for cc in range(8):
                pg, tg = proj(2 * CC + 1536 + cc * 128, 128)
                store(c.sgc[cc * 128:(cc + 1) * 128, cols], pg[:], tg, "sigmoid")
            for cc in range(8):
                pg, tg = proj(2 * CC + 1536 + D + cc * 128, 128)
                store(c.sga[cc * 128:(cc + 1) * 128, cols], pg[:], tg, "sigmoid")
            for j in range(4):
                a = ar.next()
                for k in range(8):
                    mm(P, acc[a][:], hnT[hb][:, k, j * 128:(j + 1) * 128], wbf[:, k, 2 * CC + 1024:2 * CC + 1536],
                       k == 0, k == 7, wall + [f"hnT{hb}"], [f"acc{a}"])
                t = b * 4 + j
                store(c.vS[t * 128:(t + 1) * 128, :], acc[a][:], f"acc{a}", "copy")
        while conv_list:
            conv_tick(force=True)
        P.barrier()
        P.emit()


def host_inputs(S, x_b, w, consts):
    f = lambda a: np.ascontiguousarray(np.asarray(a, dtype=np.float32))
    col = lambda v, n: f(np.asarray(v).reshape(n, 128).T)
    m = {
        "x": f(x_b),
        "w_in": f(w["w_in"][0]),
        "g1c": col(w["g_norm1"][0], 8),
        "conv_wT": f(np.asarray(w["conv_w"][0]).reshape(CW, 4, 128).transpose(2, 1, 0)),
        "conv_bc": col(w["conv_b"][0], 4),
        "ln_gc": col(w["conv_ln_g"][0], 4),
        "ln_bc": col(w["conv_ln_b"][0], 4),
        "b_pwc": col(w["b_conv_pw"][0], 8),
        "w_conv_pw": f(w["w_conv_pw"][0]),
        "w_attn_out": f(w["w_attn_out"][0]),
        "w_out": f(w["w_out"][0]),
        "w_peer_q": f(w["w_peer_q"][0]),
        "g2rep": f(np.broadcast_to(np.asarray(w["g_norm2"][0])[None, :], (128, D))),
        "gfrep": f(np.broadcast_to(np.asarray(w["g_final"])[None, :], (128, D))),
        "keys1T": f(np.asarray(w["peer_keys1"][0]).T),
        "keys2T": f(np.asarray(w["peer_keys2"][0]).T),
        "peer_u": f(w["peer_u"][0]),
        "peer_v": f(w["peer_v"][0]),
    }
    m.update(consts)
    return m


def phase_b(nc, P, c, preload=None):
    S = c.S
    NT = S // 128
    NQB = S // 512
    with ExitStack() as st:
        sb = lambda name, shape, dt: st.enter_context(nc.sbuf_tensor(name, list(shape), dt))
        ps = lambda name, shape, dt: st.enter_context(nc.psum_tensor(name, list(shape), dt))
        kaug = [sb(f"b_kaug{i}", [128, S], BF16) for i in range(2)]
        qaug = [sb(f"b_qaug{i}", [128, S], BF16) for i in range(2)]
        vall = sb("b_vall", [128, NT, NH * DH], BF16)
        vaug = [sb(f"b_vaug{i}", [128, NT, DH + 1], BF16) for i in range(2)]
        augtok = sb("b_augtok", [128, NT, 82], BF16)
        valid01 = sb("b_valid", [128, NT, 16], F32)
        own01 = sb("b_own", [128, NT, 16], F32)
        maskv = sb("b_maskv", [128, NT, 16], F32)
        stat = [sb(f"b_stat{i}", [128, NT, 16], F32) for i in range(2)]
        kbias = sb("b_kbias", [128, NH, 2], F32)
        qlo = sb("b_qlo", [128, NH], BF16)
        qhi = sb("b_qhi", [128, NH], BF16)
        masktri = sb("b_masktri", [128, 4, 512], BF16)
        ident = sb("b_ident", [128, 128], BF16)
        onesf = sb("b_onesf", [128, 64], F32)
        kms = sb("b_kms", [128, 16], F32)
        kmb = [sb(f"b_kmb{i}", [128, 16], BF16) for i in range(2)]
        bsm = sb("b_bsm", [128, NT, 16], F32)
        m8 = sb("b_m8", [128, NT, 8], F32)
        sel = sb("b_sel", [128, NT, 16], F32)
        PT = [sb(f"b_PT{i}", [128, 2, 512], BF16) for i in range(3)]
        rden = [sb(f"b_rden{i}", [128, 512], F32) for i in range(2)]
        bc_sb = sb("b_bcsb", [128, 512], F32)
        oT_sb = [sb(f"b_oT{i}", [128, 512], BF16) for i in range(2)]
        st_ps = [ps(f"b_st{i}", [128, 2, 512], F32) for i in range(2)]
        o_ps = [ps(f"b_o{i}", [128, 512], F32) for i in range(2)]
        shps = ps("b_sh", [128, 512], F32)
        bs_ps = shps[:, 0:NT * 16].rearrange("p (t n) -> p t n", n=16)
        tp = shps[:].bitcast(BF16).rearrange("p (a b) -> p a b", b=128)
        bc_ps = ps("b_bc", [128, 512], F32)

        for i in range(2):
            dma(P, "sp", kaug[i][64:82, :], c.kaug_static, [], [f"kaug_s{i}"])
        dma(P, "sp", vall[:], c.vS.rearrange("(t p) n -> p t n", p=128), [], ["vall"])
        dma(P, "sp", valid01[:], c.valid01, [], ["valid01"])
        dma(P, "sp", own01[:], c.own01, [], ["own01"])
        dma(P, "sp", maskv[:], c.maskv, [], ["maskv"])
        dma(P, "sp", kbias[:], c.kbias, [], ["kbias"])
        dma(P, "sp", qlo[:], c.qlo, [], ["qlo"])
        dma(P, "sp", qhi[:], c.qhi, [], ["qhi"])
        dma(P, "sp", masktri[:], c.masktri.rearrange("i p n -> p i n"), [], ["masktri"])
        dma(P, "sp", ident[:], c.ident_bf, [], ["ident"])
        P.op("dve", lambda e: e.memset(onesf[:], 1.0), [], ["onesf"])
        P.op("dve", lambda e: e.memset(augtok[:], 0.0), [], ["augtok"])
        for i in range(2):
            P.op("dve", (lambda i: lambda e: e.memset(vaug[i][:], 1.0))(i), [], [f"vaug{i}"])

        def prologue(h):
            hp = h % 2
            hs = slice(h * DH, (h + 1) * DH)
            KA, QA, VA, ST, KM = f"kaug{hp}", f"qaug{hp}", f"vaug{hp}", f"stat{hp}", f"kmb{hp}"
            dma(P, "sp", kaug[hp][0:64, :], c.kT[hs, :], [], [KA])
            dma(P, "sp", qaug[hp][0:64, :], c.qT[hs, :], [], [QA])
            dma(P, "sp", stat[hp][:], c.stat[h], [], [ST])
            cp(P, "pool", vaug[hp][:, :, 0:DH], vall[:, :, hs], ["vall"], [VA])
            yield
            P.op("dve", lambda e: e.tensor_reduce(out=kms[0:64, 0:S // MB], in_=kaug[hp][0:64, :].rearrange("p (n k) -> p n k", k=MB),
                                                  axis=AX.X, op=ALU.add), [KA], ["kms"])
            P.op("act", lambda e: e.mul(out=kmb[hp][0:64, 0:S // MB], in_=kms[0:64, 0:S // MB], mul=1.0 / MB), ["kms"], [KM])
            if S // MB < 16:
                P.op("dve", lambda e: e.memset(kmb[hp][0:64, S // MB:16], 0.0), [], [KM])
            yield
            for t0 in range(0, NT, 8):
                for t in range(t0, t0 + 8):
                    mm(P, bs_ps[:, t, :], qaug[hp][0:64, t * 128:(t + 1) * 128], kmb[hp][0:64, :], True, True, [QA, KM], ["shps"])
                yield
            tt(P, "dve", bsm[:], bs_ps, maskv[:], ALU.add, ["shps", "maskv"], ["bsm"])
            for t0 in range(0, NT, 8):
                for t in range(t0, t0 + 8):
                    P.op("dve", (lambda t: lambda e: e.max(out=m8[:, t, :], in_=bsm[:, t, :]))(t), ["bsm"], ["m8"])
                yield
            tt(P, "dve", sel[:], bsm[:], m8[:, :, 2:3].to_broadcast([128, NT, 16]), ALU.is_ge, ["bsm", "m8"], ["sel"])
            tt(P, "dve", sel[:], sel[:], valid01[:], ALU.mult, ["sel", "valid01"], ["sel"])
            tt(P, "dve", sel[:], sel[:], own01[:], ALU.add, ["sel", "own01"], ["sel"])
            ts(P, "dve", sel[:], sel[:], -1.0, BIG, ALU.add, ALU.mult, ["sel"], ["sel"])
            tt(P, "dve", augtok[:, :, 65:81], sel[:], stat[hp][:], ALU.add, ["sel", ST], ["augtok"])
            cp(P, "dve", augtok[:, :, 64], qlo[:, h:h + 1].to_broadcast([128, NT]), ["qlo"], ["augtok"])
            cp(P, "dve", augtok[:, :, 81], qhi[:, h:h + 1].to_broadcast([128, NT]), ["qhi"], ["augtok"])
            yield
            for t0 in range(0, NT, 8):
                for j in range(8):
                    tr(P, tp[0:82, j, :], augtok[:, t0 + j, :], ident[:], ["augtok", "ident"], ["shps"])
                cp(P, "dve", qaug[hp][64:82, t0 * 128:(t0 + 8) * 128].rearrange("p (a b) -> p a b", b=128), tp[64:82, :, :],
                   ["shps"], [QA])
                yield

        def advance(gen, n):
            if gen is None:
                return None
            for _ in range(n):
                try:
                    next(gen)
                except StopIteration:
                    return None
            return gen

        sr, pr, orr, osr = Rot(2), Rot(3), Rot(2), Rot(2)
        gen = prologue(0)
        while gen is not None:
            gen = advance(gen, 1)
        if preload is not None:
            preload()
        for h in range(NH):
            hp = h % 2
            hs = slice(h * DH, (h + 1) * DH)
            KA, QA, VA = f"kaug{hp}", f"qaug{hp}", f"vaug{hp}"
            units = [(qb, kp) for qb in range(NQB) for kp in range(2 * qb + 2)]
            NU = len(units)
            sbuf_of = {}
            obuf_of = {}
            pending = []

            def col0(qb, kt):
                return (kt - 4 * qb) * 128 if kt >= 4 * qb else 0

            def emit_s(n):
                qb, kp = units[n]
                si = sr.next()
                sbuf_of[n] = si
                for half in range(2):
                    kt = 2 * kp + half
                    diag = kt >= 4 * qb
                    c0 = col0(qb, kt)
                    mm(P, st_ps[si][:, half, c0:512], kaug[hp][0:82, kt * 128:(kt + 1) * 128],
                       qaug[hp][0:82, qb * 512 + c0:(qb + 1) * 512], True, not diag, [KA, f"kaug_s{hp}", QA], [f"st{si}"])
                    if diag:
                        mm(P, st_ps[si][:, half, c0:c0 + 128], ident[:], masktri[:, kt - 4 * qb, c0:c0 + 128], False, True,
                           ["ident", "masktri"], [f"st{si}"])

            def emit_pv(n):
                qb, kp = units[n]
                nkt = 4 * qb + 4
                si = sbuf_of.pop(n)
                if kp == 0:
                    obuf_of[qb] = orr.next()
                oi = obuf_of[qb]
                pi = pr.next()
                cmin = col0(qb, 2 * kp)
                act(P, PT[pi][:, :, cmin:512], st_ps[si][:, :, cmin:512], AF.Exp, [f"st{si}", "kbias"], [f"PT{pi}"],
                    bias=kbias[:, h, 0:1])
                for half in range(2):
                    kt = 2 * kp + half
                    c0 = col0(qb, kt)
                    mm(P, o_ps[oi][0:65, c0:512], vaug[hp][:, kt, :], PT[pi][:, half, c0:512], kt == 0, kt == nkt - 1,
                       [VA, f"PT{pi}"], [f"o{oi}"])
                if kp == 2 * qb + 1:
                    o = o_ps[oi]
                    ri = qb % 2
                    P.op("dve", lambda e: e.reciprocal(out=rden[ri][64:65, :], in_=o[64:65, :]), [f"o{oi}"], [f"rden{ri}"])

                    def fin2(qb=qb, oi=oi, o=o, ri=ri):
                        mm(P, bc_ps[0:64, :], onesf[64:65, 0:64], rden[ri][64:65, :], True, True, ["onesf", f"rden{ri}"], ["bc_ps"])
                        cp(P, "act", bc_sb[0:64, :], bc_ps[0:64, :], ["bc_ps"], ["bc_sb"])
                        oo = osr.next()
                        tt(P, "dve", oT_sb[oo][0:64, :], o[0:64, :], bc_sb[0:64, :], ALU.mult, [f"o{oi}", "bc_sb"], [f"oT{oo}"])
                        dma(P, "pool", c.oT[hs, qb * 512:(qb + 1) * 512], oT_sb[oo][0:64, :], [f"oT{oo}"], [])
                    pending.append((n + 1, fin2))

            gen = prologue(h + 1) if h + 1 < NH else None
            emit_s(0)
            for n in range(NU):
                if n + 1 < NU:
                    emit_s(n + 1)
                emit_pv(n)
                while pending and pending[0][0] <= n:
                    pending.pop(0)[1]()
                if n >= 4 and n % 2 == 0:
                    gen = advance(gen, 1)
                if n % 16 == 8 and c.conv_rest:
                    ti, r0 = c.conv_rest.pop(0)
                    dma(P, "pool", c.uv[r0:r0 + 1024, ti, :], (c.peer_u, c.peer_v)[ti][r0:r0 + 1024, :], [], [])
            while pending:
                pending.pop(0)[1]()
            while gen is not None:
                gen = advance(gen, 1)
        while c.conv_rest:
            ti, r0 = c.conv_rest.pop(0)
            dma(P, "pool", c.uv[r0:r0 + 1024, ti, :], (c.peer_u, c.peer_v)[ti][r0:r0 + 1024, :], [], [])
        P.barrier()
        P.emit()


def alloc_c_weights(nc, st):
    sb = lambda name, shape, dt: st.enter_context(nc.sbuf_tensor(name, list(shape), dt))
    wpw = sb("c_wpw", [128, 4, D], BF16)
    wao = sb("c_wao", [128, NH // 2, D], BF16)
    wout = sb("c_wout", [128, 8, D], BF16)
    dg = sb("c_dg", [128, 4, CW, 128], BF16)
    identf = sb("c_identf", [128, 128], F32)
    convw = sb("c_convw", [128, 4, CW], F32)
    convb = sb("c_convb", [128, 4], F32)
    lng = sb("c_lng", [128, 4], F32)
    lnb = sb("c_lnb", [128, 4], F32)
    bpw = sb("c_bpw", [128, 8], F32)
    epsc = sb("c_eps", [128, 1], F32)
    onesm = sb("c_onesm", [128, 128], F32)
    return (wpw, wao, wout, dg, identf, convw, convb, lng, lnb, bpw, epsc, onesm)


def load_c_weights(nc, P, c, wts, wst):
    (wpw, wao, wout, dg, identf, convw, convb, lng, lnb, bpw, epsc, onesm) = wts
    for name, t, src in (("identf", identf, c.ident_f), ("convw", convw, c.conv_wT), ("convb", convb, c.conv_bc),
                         ("lng", lng, c.ln_gc), ("lnb", lnb, c.ln_bc), ("bpw", bpw, c.b_pwc), ("epsc", epsc, c.epsc)):
        dma(P, "sp", t[:], src, [], [name])
    P.op("dve", lambda e: e.memset(onesm[:], 1.0 / CC), [], ["onesm"])
    dma(P, "pool", wpw[:], c.w_pw.rearrange("(k p) n -> p k n", p=128), [], ["wpw"])
    dma(P, "pool", wao[:], c.w_ao.rearrange("(k p) n -> p k n", p=128), [], ["wao"])
    dma(P, "pool", wout[:], c.w_out.rearrange("(k p) n -> p k n", p=128), [], ["wout"])
    for cc in range(4):
        tt(P, "dve", dg[:, cc, :, :], identf[:].unsqueeze(1).to_broadcast([128, CW, 128]),
           convw[:, cc, :].unsqueeze(2).to_broadcast([128, CW, 128]), ALU.mult, ["identf", "convw"], ["dg"])


def phase_c(nc, P, c, wts):
    S = c.S
    NBLK = S // 512
    HALO = CW - 1
    with ExitStack() as st:
        sb = lambda name, shape, dt: st.enter_context(nc.sbuf_tensor(name, list(shape), dt))
        ps = lambda name, shape, dt: st.enter_context(nc.psum_tensor(name, list(shape), dt))
        (wpw, wao, wout, dg, identf, convw, convb, lng, lnb, bpw, epsc, onesm) = wts
        zcb = [sb(f"c_zcb{i}", [128, 4, 512 + HALO], BF16) for i in range(2)]
        zconv = sb("c_zconv", [128, 4, 512], F32)
        zsq = sb("c_zsq", [128, 4, 512], F32)
        mean_sb = sb("c_mean", [128, 512], F32)
        msq = sb("c_msq", [128, 512], F32)
        var = sb("c_var", [128, 512], F32)
        rstd = sb("c_rstd", [128, 512], F32)
        t1 = [sb(f"c_t1{i}", [128, 512], F32) for i in range(2)]
        zact = sb("c_zact", [128, 4, 512], BF16)
        oTb = [sb(f"c_oTb{i}", [128, NH // 2, 512], BF16) for i in range(2)]
        sgcb = [sb(f"c_sgcb{i}", [128, 8, 512], BF16) for i in range(2)]
        sgab = [sb(f"c_sgab{i}", [128, 8, 512], BF16) for i in range(2)]
        mixc = [sb(f"c_mixc{i}", [128, 512], F32) for i in range(2)]
        mixa = [sb(f"c_mixa{i}", [128, 512], F32) for i in range(2)]
        mix = sb("c_mix", [128, 8, 512], BF16)
        xt = [sb(f"c_xt{i}", [128, D], F32) for i in range(2)]
        x1t = [sb(f"c_x1t{i}", [128, D], F32) for i in range(2)]
        cv_ps = [ps(f"c_cv{i}", [128, 512], F32) for i in range(2)]
        mean_ps = ps("c_meanps", [128, 512], F32)
        ex2_ps = ps("c_ex2ps", [128, 512], F32)
        y_ps = [ps(f"c_y{i}", [128, 512], F32) for i in range(2)]
        o_ps = [ps(f"c_op{i}", [128, 512], F32) for i in range(2)]

        zc_v = c.zcT.rearrange("(c p) s -> p c s", p=128)
        sgc_v = c.sgc.rearrange("(f p) s -> p f s", p=128)
        sga_v = c.sga.rearrange("(f p) s -> p f s", p=128)
        oT_v = c.oT.rearrange("(h d) s -> d h s", d=2 * DH)
        x_v = c.x.rearrange("(t p) d -> t p d", p=128)
        x1_v = c.x1.rearrange("(t p) d -> t p d", p=128)
        cr, tr1, yr, opr, xr, mr = Rot(2), Rot(2), Rot(2), Rot(2), Rot(2), Rot(2)
        for b in range(NBLK):
            bi = b % 2
            cols = slice(b * 512, (b + 1) * 512)
            if b == 0:
                P.op("dve", lambda e: e.memset(zcb[0][:, :, 0:HALO], 0.0), [], ["zcb0"])
                dma(P, "sp", zcb[0][:, :, HALO:], zc_v[:, :, 0:512], [], ["zcb0"])
            else:
                dma(P, "sp", zcb[bi][:], zc_v[:, :, b * 512 - HALO:(b + 1) * 512], [], [f"zcb{bi}"])
            dma(P, "sp", sgcb[bi][:], sgc_v[:, :, cols], [], [f"sgcb{bi}"])
            dma(P, "sp", sgab[bi][:], sga_v[:, :, cols], [], [f"sgab{bi}"])
            dma(P, "sp", oTb[bi][:], oT_v[:, :, cols], [], [f"oTb{bi}"])
            for cc in range(4):
                ci = cr.next()
                for k in range(CW):
                    mm(P, cv_ps[ci][:], dg[:, cc, k, :], zcb[bi][:, cc, k:k + 512], k == 0, k == CW - 1,
                       ["dg", f"zcb{bi}"], [f"cv{ci}"])
                act(P, zconv[:, cc, :], cv_ps[ci][:], AF.Identity, [f"cv{ci}", "convb"], [f"zconv{cc}"], bias=convb[:, cc:cc + 1])
                act(P, zsq[:, cc, :], zconv[:, cc, :], AF.Square, [f"zconv{cc}"], [f"zsq{cc}"])
            for cc in range(4):
                mm(P, mean_ps[:], onesm[:], zconv[:, cc, :], cc == 0, cc == 3, ["onesm", f"zconv{cc}"], ["mean_ps"])
            for cc in range(4):
                mm(P, ex2_ps[:], onesm[:], zsq[:, cc, :], cc == 0, cc == 3, ["onesm", f"zsq{cc}"], ["ex2_ps"])
            cp(P, "act", mean_sb[:], mean_ps[:], ["mean_ps"], ["mean_sb"])
            act(P, msq[:], mean_ps[:], AF.Square, ["mean_ps"], ["msq"])
            tt(P, "dve", var[:], ex2_ps[:], msq[:], ALU.subtract, ["ex2_ps", "msq"], ["var"])
            act(P, var[:], var[:], AF.Sqrt, ["var", "epsc"], ["var"], bias=epsc[:], scale=1.0)
            P.op("dve", lambda e: e.reciprocal(out=rstd[:], in_=var[:]), ["var"], ["rstd"])
            for cc in range(4):
                ti = tr1.next()
                tt(P, "dve", t1[ti][:], zconv[:, cc, :], mean_sb[:], ALU.subtract, [f"zconv{cc}", "mean_sb"], [f"t1{ti}"])
                tt(P, "dve", t1[ti][:], t1[ti][:], rstd[:], ALU.mult, [f"t1{ti}", "rstd"], [f"t1{ti}"])
                act(P, zact[:, cc, :], t1[ti][:], AF.Silu, [f"t1{ti}", "lng", "lnb"], [f"zact{cc}"],
                    scale=lng[:, cc:cc + 1], bias=lnb[:, cc:cc + 1])
            zall = [f"zact{cc}" for cc in range(4)]
            if c.dbg is not None and b == 0:
                dma(P, "sp", c.dbg[1, :, 0:512], mean_sb[:], ["mean_sb"], [])
                dma(P, "sp", c.dbg[1, :, 512:1024], var[:], ["var"], [])
                dma(P, "sp", c.dbg[1, :, 1024:1536], rstd[:], ["rstd"], [])
            for f in range(8):
                fs = slice(f * 128, (f + 1) * 128)
                yi = yr.next()
                for cc in range(4):
                    mm(P, y_ps[yi][:], wpw[:, cc, fs], zact[:, cc, :], cc == 0, cc == 3, ["wpw"] + zall, [f"y{yi}"])
                mi = mr.next()
                act(P, mixc[mi][:], y_ps[yi][:], AF.Identity, [f"y{yi}", "bpw"], [f"mixc{mi}"], bias=bpw[:, f:f + 1])
                tt(P, "dve", mixc[mi][:], mixc[mi][:], sgcb[bi][:, f, :], ALU.mult, [f"mixc{mi}", f"sgcb{bi}"], [f"mixc{mi}"])
                yi2 = yr.next()
                for h in range(NH // 2):
                    mm(P, y_ps[yi2][:], wao[:, h, fs], oTb[bi][:, h, :], h == 0, h == NH // 2 - 1,
                       ["wao", f"oTb{bi}"], [f"y{yi2}"])
                tt(P, "dve", mixa[mi][:], y_ps[yi2][:], sgab[bi][:, f, :], ALU.mult, [f"y{yi2}", f"sgab{bi}"], [f"mixa{mi}"])
                tt(P, "pool", mix[:, f, :], mixa[mi][:], mixc[mi][:], ALU.add, [f"mixa{mi}", f"mixc{mi}"], [f"mix{f}"])
            mall = [f"mix{f}" for f in range(8)]
            for j in range(4):
                t = b * 4 + j
                xi = xr.next()
                dma(P, "sp", xt[xi][:], x_v[t], [], [f"xt{xi}"])
                for half in range(2):
                    oi = opr.next()
                    hsl = slice(half * 512, (half + 1) * 512)
                    for f in range(8):
                        mm(P, o_ps[oi][:], mix[:, f, j * 128:(j + 1) * 128], wout[:, f, hsl], f == 0, f == 7,
                           mall + ["wout"], [f"op{oi}"])
                    tt(P, "dve", x1t[xi][:, hsl], xt[xi][:, hsl], o_ps[oi][:], ALU.add, [f"xt{xi}", f"op{oi}"], [f"x1t{xi}"])
                dma(P, "pool", x1_v[t], x1t[xi][:], [f"x1t{xi}"], [])
        P.barrier()
        P.emit()


def phase_d(nc, P, c):
    S = c.S
    NT = S // 128
    G = 2
    NB_G = 8
    NSL = PH * 16
    with ExitStack() as st:
        sb = lambda name, shape, dt: st.enter_context(nc.sbuf_tensor(name, list(shape), dt))
        ps = lambda name, shape, dt: st.enter_context(nc.psum_tensor(name, list(shape), dt))
        wq = sb("d_wq", [128, 8, PH * 256], BF16)
        kT = [sb(f"d_keysT{i}", [128, NK], BF16) for i in range(2)]
        kTf = sb("d_keysTf", [128, NK], F32)
        g2rep = sb("d_g2rep", [128, D], F32)
        gfrep = sb("d_gfrep", [128, D], F32)
        identb = sb("d_identb", [128, 128], BF16)
        epsc = sb("d_eps", [128, 1], F32)
        iota16 = sb("d_iota16", [128, 16], F32)
        thr17 = sb("d_thr17", [128, 17], F32)
        x1t = [sb(f"d_x1t{i}", [128, D], F32) for i in range(3)]
        tmp = sb("d_tmp", [128, D], F32)
        hn2 = sb("d_hn2", [128, D], F32)
        hn2b = [sb(f"d_hn2b{i}", [128, D], BF16) for i in range(2)]
        hn2T = sb("d_hn2T", [128, 8, 128], BF16)
        ss = sb("d_ss", [128, 1], F32)
        std = sb("d_std", [128, 1], F32)
        rstd = sb("d_rstd", [128, 1], F32)
        fss = sb("d_fss", [128, 1], F32)
        fstd = sb("d_fstd", [128, 1], F32)
        frstd = sb("d_frstd", [128, 1], F32)
        qT_sb = sb("d_qT", [128, 16, 128], BF16)
        s_sb = sb("d_s", [128, 16, NK], F32)
        gebuf = sb("d_ge", [128, PH * 16 * 17], F32)
        ohbuf = sb("d_oh", [128, PH * 256], F32)
        s2 = gebuf[:, 0:16 * NK].rearrange("p (g n) -> p g n", n=NK)
        ge = gebuf[:].rearrange("p (h k a) -> p h k a", k=16, a=17)
        cand2 = ohbuf[:].rearrange("p (h n) -> p h n", n=256)
        oh = ohbuf[:].rearrange("p (h k a) -> p h k a", k=16, a=16)
        m16 = sb("d_m16", [128, 16, 16], F32)
        i16 = sb("d_i16", [128, 16, 16], U32)
        i16f = sb("d_i16f", [128, 16, 16], F32)
        cand = sb("d_cand", [128, PH, 256], F32)
        sc = sb("d_sc", [128, PH, 16], F32)
        pos = sb("d_pos", [128, PH, 16], U32)
        posf = sb("d_posf", [128, PH, 16], F32)
        af = sb("d_af", [128, PH, 16], F32)
        bf_ = sb("d_bf", [128, PH, 16], F32)
        i1s = sb("d_i1s", [128, PH, 16], F32)
        i2s = sb("d_i2s", [128, PH, 16], F32)
        eidx = [sb(f"d_eidx{i}", [128, NSL], U32) for i in range(2)]
        ee = sb("d_ee", [128, PH, 16], F32)
        zz = sb("d_zz", [128, PH], F32)
        gg = [sb(f"d_gg{i}", [128, NSL], F32) for i in range(2)]
        adot = sb("d_adot", [128, NSL], F32)
        ga = sb("d_ga", [128, NSL], F32)
        cw = sb("d_cw", [128, NSL], F32)
        uvg = [sb(f"d_uvg{i}", [128, G, 2, D], BF16) for i in range(NB_G)]
        prodb = [sb(f"d_prodb{i}", [128, D], BF16) for i in range(4)]
        junkb = sb("d_junkb", [128, D], BF16)
        dgc = [sb(f"d_dgc{i}", [128, 128], BF16) for i in range(8)]
        x2 = sb("d_x2", [128, D], F32)
        ot = sb("d_ot", [128, D], F32)
        tp = ps("d_tp", [128, 8, 128], BF16)
        qs_ps = [ps(f"d_qs{i}", [128, 4, 128], F32) for i in range(2)]
        y_ps = [[ps(f"d_y{i}{h}", [128, 512], F32) for h in range(2)] for i in range(2)]
        fold_ps = ps("d_fold", [128, 512], F32)
        junks = sb("d_junks", [128, 128], BF16)

        for name, t_, src in (("g2rep", g2rep, c.g2rep), ("gfrep", gfrep, c.gfrep),
                              ("identb", identb, c.ident_bf), ("epsc", epsc, c.epsc), ("iota16", iota16, c.iota16)):
            dma(P, "sp", t_[:], src, [], [name])
        for i, src in enumerate((c.keys1T, c.keys2T)):
            dma(P, "pool", kT[i][:], src, [], [f"kT{i}"])
        ts(P, "dve", thr17[:, 0:16], iota16[:], 16.0, None, ALU.mult, None, ["iota16"], ["thr17"])
        P.op("dve", lambda e: e.memset(thr17[:, 16:17], 256.0), [], ["thr17b"])
        dma(P, "pool", wq[:], c.w_pq.rearrange("(k p) n -> p k n", p=128), [], ["wq"])

        x1_v = c.x1.rearrange("(t p) d -> t p d", p=128)
        out_v = c.out.rearrange("(t p) d -> t p d", p=128)
        uv_v = c.uv.rearrange("e s d -> e (s d)")
        m16v = m16[:].rearrange("p (h s) k -> p h s k", s=2)
        i16fv = i16f[:].rearrange("p (h s) k -> p h s k", s=2)
        cand4 = cand[:].rearrange("p h (a b) -> p h a b", b=16)
        B4 = [128, PH, 16, 16]

        def prologue(t):
            par = t % 2
            xp = t % 3
            X, HB, EI, GG = f"x1t{xp}", f"hn2b{par}", f"eidx{par}", f"gg{par}"
            dma(P, "sp", x1t[xp][:], x1_v[t], [], [X])
            yield
            act(P, junkb[:], x1t[xp][:], AF.Square, [X], ["junkb", "ss"], accum_out=ss[:])
            yield
            act(P, std[:], ss[:], AF.Sqrt, ["ss", "epsc"], ["std"], bias=epsc[:], scale=1.0 / D)
            yield
            P.op("dve", lambda e: e.reciprocal(out=rstd[:], in_=std[:]), ["std"], ["rstd"])
            yield
            act(P, tmp[:], x1t[xp][:], AF.Copy, [X, "rstd"], ["tmp"], scale=rstd[:])
            yield
            tt(P, "dve", hn2[:], tmp[:], g2rep[:], ALU.mult, ["tmp", "g2rep"], ["hn2"])
            yield
            cp(P, "act", hn2b[par][:], hn2[:], ["hn2"], [HB])
            yield
            for k in range(8):
                tr(P, tp[:, k, :], hn2b[par][:, k * 128:(k + 1) * 128], identb[:], [HB, "identb"], ["tp"])
            yield
            cp(P, "dve", hn2T[:], tp[:], ["tp"], ["hn2T"])
            yield

            def qmm(g4):
                for gi in range(4):
                    g = g4 * 4 + gi
                    for k in range(8):
                        mm(P, qs_ps[g4 % 2][:, gi, :], wq[:, k, g * 128:(g + 1) * 128], hn2T[:, k, :], k == 0, k == 7,
                           ["wq", "hn2T"], [f"qs{g4 % 2}"])

            def qcp(g4):
                cp(P, "act", qT_sb[:, g4 * 4:(g4 + 1) * 4, :], qs_ps[g4 % 2][:], [f"qs{g4 % 2}"], [f"qT{g4}"])

            def smm(g4):
                for gi in range(4):
                    g = g4 * 4 + gi
                    mm(P, qs_ps[g4 % 2][:, gi, :], qT_sb[:, g, :], kT[g % 2][:], True, True, [f"qT{g4}", f"kT{g%2}"], [f"qs{g4 % 2}"])

            def scp(g4):
                cp(P, "act", s_sb[:, g4 * 4:(g4 + 1) * 4, :], qs_ps[g4 % 2][:], [f"qs{g4 % 2}"], [f"s{g4}"])

            for g4 in range(5):
                if g4 < 4:
                    qmm(g4)
                if g4 >= 1:
                    qcp(g4 - 1)
                yield
            for g4 in range(5):
                if g4 < 4:
                    smm(g4)
                if g4 >= 1:
                    scp(g4 - 1)
                yield
            for g in range(16):
                P.op("dve", (lambda g: lambda e: e.max(out=m16[:, g, 0:8], in_=s_sb[:, g, :]))(g), [f"s{g // 4}"], [f"m16a{g}"])
                if g % 4 == 3:
                    yield
            for g in range(16):
                P.op("dve", (lambda g: lambda e: e.max_index(out=i16[:, g, 0:8], in_max=m16[:, g, 0:8], in_values=s_sb[:, g, :]))(g),
                     [f"s{g // 4}", f"m16a{g}"], [f"i16a{g}"])
                if g % 4 == 3:
                    yield
            for g in range(16):
                P.op("dve", (lambda g: lambda e: e.match_replace(out=s2[:, g, :], in_to_replace=m16[:, g, 0:8],
                                                                 in_values=s_sb[:, g, :], imm_value=NEG))(g),
                     [f"s{g // 4}", f"m16a{g}"], [f"s2_{g}"])
                if g % 4 == 3:
                    yield
            for g in range(16):
                P.op("dve", (lambda g: lambda e: e.max(out=m16[:, g, 8:16], in_=s2[:, g, :]))(g), [f"s2_{g}"], [f"m16b{g}"])
                if g % 4 == 3:
                    yield
            for g in range(16):
                P.op("dve", (lambda g: lambda e: e.max_index(out=i16[:, g, 8:16], in_max=m16[:, g, 8:16], in_values=s2[:, g, :]))(g),
                     [f"s2_{g}", f"m16b{g}"], [f"i16b{g}"])
                if g % 4 == 3:
                    yield
            m16all = [f"m16a{g}" for g in range(16)] + [f"m16b{g}" for g in range(16)]
            i16all = [f"i16a{g}" for g in range(16)] + [f"i16b{g}" for g in range(16)]
            cp(P, "dve", i16f[:], i16[:], i16all, ["i16f"])
            tt(P, "dve", cand4, m16v[:, :, 0, :].unsqueeze(3).to_broadcast(B4), m16v[:, :, 1, :].unsqueeze(2).to_broadcast(B4),
               ALU.add, m16all, ["cand"])
            yield
            for h in range(PH):
                P.op("dve", (lambda h: lambda e: e.max(out=sc[:, h, 0:8], in_=cand[:, h, :]))(h), ["cand"], [f"sca{h}"])
                if h % 4 == 3:
                    yield
            for h in range(PH):
                P.op("dve", (lambda h: lambda e: e.max_index(out=pos[:, h, 0:8], in_max=sc[:, h, 0:8], in_values=cand[:, h, :]))(h),
                     ["cand", f"sca{h}"], [f"posa{h}"])
                if h % 4 == 3:
                    yield
            for h in range(PH):
                P.op("dve", (lambda h: lambda e: e.match_replace(out=cand2[:, h, :], in_to_replace=sc[:, h, 0:8],
                                                                 in_values=cand[:, h, :], imm_value=NEG))(h),
                     ["cand", f"sca{h}"], [f"c2_{h}"])
                if h % 4 == 3:
                    yield
            for h in range(PH):
                P.op("dve", (lambda h: lambda e: e.max(out=sc[:, h, 8:16], in_=cand2[:, h, :]))(h), [f"c2_{h}"], [f"scb{h}"])
                if h % 4 == 3:
                    yield
            for h in range(PH):
                P.op("dve", (lambda h: lambda e: e.max_index(out=pos[:, h, 8:16], in_max=sc[:, h, 8:16], in_values=cand2[:, h, :]))(h),
                     [f"c2_{h}", f"scb{h}"], [f"posb{h}"])
                if h % 4 == 3:
                    yield
            scall = [f"sca{h}" for h in range(PH)] + [f"scb{h}" for h in range(PH)]
            posall = [f"posa{h}" for h in range(PH)] + [f"posb{h}" for h in range(PH)]
            cp(P, "dve", posf[:], pos[:], posall, ["posf"])
            tt(P, "dve", ge, posf[:].unsqueeze(3).to_broadcast([128, PH, 16, 17]),
               thr17[:].unsqueeze(1).unsqueeze(1).to_broadcast([128, PH, 16, 17]), ALU.is_ge, ["posf", "thr17", "thr17b"], ["ge"])
            yield
            tt(P, "dve", oh, ge[:, :, :, 0:16], ge[:, :, :, 1:17], ALU.subtract, ["ge"], ["oh"])
            P.op("dve", lambda e: e.tensor_reduce(out=af[:], in_=ge[:, :, :, 1:17], axis=AX.X, op=ALU.add), ["ge"], ["af"])
            yield
            tt(P, "dve", oh, oh, i16fv[:, :, 0, :].unsqueeze(2).to_broadcast(B4), ALU.mult, ["oh", "i16f"], ["oh"])
            P.op("dve", lambda e: e.tensor_reduce(out=i1s[:], in_=oh, axis=AX.X, op=ALU.add), ["oh"], ["i1s"])
            ts(P, "dve", bf_[:], af[:], -16.0, None, ALU.mult, None, ["af"], ["bf"])
            tt(P, "dve", bf_[:], bf_[:], posf[:], ALU.add, ["bf", "posf"], ["bf"])
            yield
            tt(P, "dve", oh, iota16[:].unsqueeze(1).unsqueeze(1).to_broadcast(B4), bf_[:].unsqueeze(3).to_broadcast(B4),
               ALU.is_equal, ["iota16", "bf", "i1s"], ["oh"])
            tt(P, "dve", oh, oh, i16fv[:, :, 1, :].unsqueeze(2).to_broadcast(B4), ALU.mult, ["oh", "i16f"], ["oh"])
            P.op("dve", lambda e: e.tensor_reduce(out=i2s[:], in_=oh, axis=AX.X, op=ALU.add), ["oh"], ["i2s"])
            yield
            ts(P, "dve", i1s[:], i1s[:], float(NK), None, ALU.mult, None, ["i1s"], ["i1s"])
            tt(P, "dve", i1s[:], i1s[:], i2s[:], ALU.add, ["i1s", "i2s"], ["i1s"])
            cp(P, "dve", eidx[par][:], i1s[:].rearrange("p h k -> p (h k)"), ["i1s"], [EI])
            tt(P, "dve", ee[:], sc[:], sc[:, :, 0:1].to_broadcast([128, PH, 16]), ALU.subtract, scall, ["ee"])
            yield
            act(P, ee[:], ee[:], AF.Exp, ["ee"], ["ee"])
            yield
            P.op("dve", lambda e: e.tensor_reduce(out=zz[:], in_=ee[:], axis=AX.X, op=ALU.add), ["ee"], ["zz"])
            P.op("dve", lambda e: e.reciprocal(out=zz[:], in_=zz[:]), ["zz"], ["zz"])
            tt(P, "dve", gg[par][:].rearrange("p (h k) -> p h k", k=16), ee[:], zz[:].unsqueeze(2).to_broadcast([128, PH, 16]),
               ALU.mult, ["ee", "zz"], [GG])
            yield

        def advance(gen, n):
            if gen is None:
                return None
            for _ in range(n):
                try:
                    next(gen)
                except StopIteration:
                    return None
            return gen

        gen = prologue(0)
        while gen is not None:
            gen = advance(gen, 1)
        br, pr = Rot(NB_G), Rot(4)
        NGRP = NSL // G
        gbuf = {}

        def names(t):
            par = t % 2
            return par, f"x1t{t % 3}", f"hn2b{par}", f"eidx{par}", f"gg{par}"

        def stage_a(t, gi):
            par, X, HB, EI, GG = names(t)
            s0 = gi * G
            bi = br.next()
            gbuf[(t, gi)] = bi
            for j in range(G):
                s = s0 + j
                P.dma("pool", (lambda bi, j, s, par: lambda e: e.indirect_dma_start(
                    out=uvg[bi][:, j, :, :].rearrange("p s d -> p (s d)"), out_offset=None, in_=uv_v,
                    in_offset=bass.IndirectOffsetOnAxis(ap=eidx[par][:, s:s + 1], axis=0)))(bi, j, s, par),
                    [EI], [f"uvg{bi}_{j}"])
            for j in range(G):
                s = s0 + j
                pi = pr.next()
                tt(P, "dve", prodb[pi][:], uvg[bi][:, j, 0, :], hn2b[par][:], ALU.mult, [f"uvg{bi}_{j}", HB], [f"prodb{pi}"])
                if s % 2 == 1:
                    for cc in range(8):
                        mm(P, fold_ps[:, 0:128], identb[:], prodb[pi][:, cc * 128:(cc + 1) * 128], cc == 0, cc == 7,
                           ["identb", f"prodb{pi}"], ["fold"])
                    act(P, junks[:], fold_ps[:, 0:128], AF.Identity, ["fold"], ["junks", f"adot{s}"], accum_out=adot[:, s:s + 1])
                else:
                    act(P, junkb[:], prodb[pi][:], AF.Identity, [f"prodb{pi}"], ["junkb", f"adot{s}"], accum_out=adot[:, s:s + 1])

        def stage_b(t, gi):
            s0 = gi * G
            adg = [f"adot{s0 + j}" for j in range(G)]
            act(P, ga[:, s0:s0 + G], adot[:, s0:s0 + G], AF.Gelu, adg, [f"ga{s0}"])

        def stage_c(t, gi):
            par, X, HB, EI, GG = names(t)
            s0 = gi * G
            tt(P, "dve", cw[:, s0:s0 + G], ga[:, s0:s0 + G], gg[par][:, s0:s0 + G], ALU.mult, [f"ga{s0}", GG], [f"cw{s0}"])

        def stage_d(t, gi):
            par, X, HB, EI, GG = names(t)
            yp = y_ps[par]
            s0 = gi * G
            bi = gbuf.pop((t, gi))
            for j in range(G):
                s = s0 + j
                di = s % 8
                if s % 2 == 0:
                    act(P, dgc[di][:], identb[:], AF.Copy, ["identb", f"cw{s0}"], [f"dgc{di}"], scale=cw[:, s:s + 1])
                else:
                    ts(P, "dve", dgc[di][:], identb[:], cw[:, s:s + 1], None, ALU.mult, None, ["identb", f"cw{s0}"], [f"dgc{di}"])
            for j in range(G):
                s = s0 + j
                di = s % 8
                for half in range(2):
                    mm(P, yp[half][:], dgc[di][:], uvg[bi][:, j, 1, half * 512:(half + 1) * 512], s == 0, s == NSL - 1,
                       [f"dgc{di}", f"uvg{bi}_{j}"], [f"y{par}{half}"])

        def finalize(t):
            par, X, HB, EI, GG = names(t)
            xp = t % 3
            yp = y_ps[par]
            for half in range(2):
                hsl = slice(half * 512, (half + 1) * 512)
                tt(P, "dve", x2[:, hsl], x1t[xp][:, hsl], yp[half][:], ALU.add, [X, f"y{par}{half}"], [f"x2_{half}"])
            yield
            act(P, junkb[:], x2[:], AF.Square, ["x2_0", "x2_1"], ["junkb", "fss"], accum_out=fss[:])
            yield
            act(P, fstd[:], fss[:], AF.Sqrt, ["fss", "epsc"], ["fstd"], bias=epsc[:], scale=1.0 / D)
            yield
            P.op("dve", lambda e: e.reciprocal(out=frstd[:], in_=fstd[:]), ["fstd"], ["frstd"])
            yield
            act(P, ot[:], x2[:], AF.Copy, ["x2_0", "x2_1", "frstd"], ["ot"], scale=frstd[:])
            yield
            tt(P, "dve", ot[:], ot[:], gfrep[:], ALU.mult, ["ot", "gfrep"], ["ot"])
            yield
            dma(P, "sp", out_v[t], ot[:], ["ot"], ["out"])
            yield

        units = [(t, gi) for t in range(NT) for gi in range(NGRP)]
        NU = len(units)
        gen = None
        fgen = None
        for n in range(NU + 3):
            if n < NU:
                t, gi = units[n]
                if gi == 0 and gen is not None:
                    while gen is not None:
                        gen = advance(gen, 1)
                stage_a(t, gi)
            if 1 <= n <= NU:
                stage_b(*units[n - 1])
            if 2 <= n <= NU + 1:
                stage_c(*units[n - 2])
            if n >= 3:
                tc_, gc_ = units[n - 3]
                stage_d(tc_, gc_)
                if gc_ == NGRP - 1:
                    while fgen is not None:
                        fgen = advance(fgen, 1)
                    fgen = finalize(tc_)
            fgen = advance(fgen, 1)
            if n < NU:
                t, gi = units[n]
                if gi == 3 and t + 1 < NT:
                    gen = prologue(t + 1)
                gen = advance(gen, 1)
                if gi == NGRP - 1:
                    while gen is not None:
                        gen = advance(gen, 1)
        while fgen is not None:
            fgen = advance(fgen, 1)
        P.barrier()
        P.emit()


def build_all(nc, P, c, upto="d"):
    phase_a(nc, P, c)
    if upto < "b":
        return
    with ExitStack() as wsc:
        wts = alloc_c_weights(nc, wsc)
        phase_b(nc, P, c, preload=lambda: load_c_weights(nc, P, c, wts, None))
        if upto >= "c":
            phase_c(nc, P, c, wts)
    if upto >= "d":
        phase_d(nc, P, c)


def build_program(S):
    nc = bass.Bass("TRN2", target_bir_lowering=False)
    c = declare_io(nc, S, debug=False)
    with ExitStack() as st:
        P = Prog(nc, st)
        build_all(nc, P, c)
    return nc


def kernel(**inputs):
    x = np.asarray(inputs["x"], dtype=np.float32)
    B, S, _ = x.shape
    assert B == 8
    nc = build_program(S)
    consts = make_consts(S)
    w = {k: np.asarray(v) for k, v in inputs.items() if k != "x"}
    shared = host_inputs(S, x[0], w, consts)
    in_maps = []
    for b in range(B):
        m = dict(shared)
        m["x"] = np.ascontiguousarray(x[b])
        in_maps.append(m)
    res = run_bass_kernel_spmd(nc, in_maps, core_ids=list(range(B)))
    out = np.stack([np.asarray(res.results[b]["out"], dtype=np.float32) for b in range(B)], axis=0)
    return out
```

```python
import numpy as np
import ml_dtypes
from contextlib import ExitStack
import concourse.bass as bass
import concourse.mybir as mybir
from concourse.bass_utils import run_bass_kernel_spmd

F32 = mybir.dt.float32
BF16 = mybir.dt.bfloat16
U32 = mybir.dt.uint32
I32 = mybir.dt.int32
ALU = mybir.AluOpType
AF = mybir.ActivationFunctionType
AX = mybir.AxisListType

D = 1024
NCOL = 4608
NH = 8
DH = 64
CC = 512
CW = 31
MB = 256
PH = 8
NK = 128
TOPK = 16
EPS = 1e-6
BIG = 30000.0
NEG = -1.0e30


class Prog:
    ENG = ("pe", "dve", "act", "pool", "sp")
    DQ = ("sp", "pool", "act")

    def __init__(self, nc, stack, kdma=8):
        self.nc = nc
        self.eng = {"pe": nc.tensor, "dve": nc.vector, "act": nc.scalar, "pool": nc.gpsimd, "sp": nc.sync}
        self.sems = {}
        for e in self.ENG:
            self.sems[("c", e)] = stack.enter_context(nc.semaphore(f"c_{e}"))
        self.kdma = {"sp": kdma, "pool": 16, "act": 4}
        for q in self.DQ:
            for j in range(self.kdma[q]):
                self.sems[("d", q, j)] = stack.enter_context(nc.semaphore(f"d_{q}{j}"))
        self.latest = {k: 0 for k in self.sems}
        self.dnext = {q: 0 for q in self.DQ}
        self.ops = {e: [] for e in self.ENG}
        self.known = {e: {} for e in self.ENG}
        self.lastw = {}
        self.readers = {}
        self.nins = 0

    def _need(self, eng, key, val):
        if self.known[eng].get(key, 0) < val:
            self.known[eng][key] = val
            self.ops[eng].append(("w", key, val))

    def _deps(self, eng, reads, writes, is_dma):
        for b in reads:
            lw = self.lastw.get(b)
            if lw is not None:
                key, val = lw
                if (not is_dma) and key == ("c", eng) and eng == "pe":
                    continue
                self._need(eng, key, val)
        for b in writes:
            lw = self.lastw.get(b)
            if lw is not None:
                key, val = lw
                if is_dma or key != ("c", eng):
                    self._need(eng, key, val)
            for key, val in self.readers.get(b, {}).items():
                if is_dma or key != ("c", eng):
                    self._need(eng, key, val)

    def _commit(self, key, val, reads, writes):
        for b in writes:
            self.lastw[b] = (key, val)
            self.readers[b] = {}
        for b in reads:
            r = self.readers.setdefault(b, {})
            r[key] = max(r.get(key, 0), val)

    def op(self, eng, fn, reads=(), writes=()):
        self._deps(eng, reads, writes, False)
        key = ("c", eng)
        self.latest[key] += 1
        val = self.latest[key]
        self.ops[eng].append(("i", fn, key, 1))
        self._commit(key, val, reads, writes)
        self.nins += 1

    def dma(self, q, fn, reads=(), writes=()):
        self._deps(q, reads, writes, True)
        j = self.dnext[q]
        self.dnext[q] = (j + 1) % self.kdma[q]
        key = ("d", q, j)
        if self.latest[key] > 0:
            self._need(q, key, self.latest[key])
        self.latest[key] += 16
        val = self.latest[key]
        self.ops[q].append(("i", fn, key, 16))
        self._commit(key, val, reads, writes)
        self.nins += 1

    def barrier(self):
        for e in self.ENG:
            for key, val in self.latest.items():
                if val > 0:
                    self._need(e, key, val)
        self.lastw = {}
        self.readers = {}

    def emit(self, name=None):
        ops = self.ops
        self.ops = {e: [] for e in self.ENG}
        sems = self.sems
        nc = self.nc

        def replay(lst):
            def f(e):
                for it in lst:
                    if it[0] == "w":
                        e.wait_ge(sems[it[1]], it[2])
                    else:
                        ins = it[1](e)
                        ins.then_inc(sems[it[2]], it[3])
            return f

        with nc.Block() as block:
            block.tensor(replay(ops["pe"]))
            block.vector(replay(ops["dve"]))
            block.scalar(replay(ops["act"]))
            block.gpsimd(replay(ops["pool"]))
            block.sync(replay(ops["sp"]))


class Rot:
    def __init__(self, n):
        self.n = n
        self.i = -1

    def next(self):
        self.i = (self.i + 1) % self.n
        return self.i


def _bf(a):
    return np.ascontiguousarray(a).astype(ml_dtypes.bfloat16)


def make_consts(S):
    NT = S // 128
    NB = S // MB
    c = {}
    c["ident_bf"] = _bf(np.eye(128, dtype=np.float32))
    c["ident_f"] = np.eye(128, dtype=np.float32)
    ka = np.zeros((18, S), np.float32)
    ka[0, :] = 1.0
    for n in range(NB):
        ka[1 + n, n * MB:(n + 1) * MB] = 1.0
    ka[17, :] = ((np.arange(S) // 128) % 2).astype(np.float32)
    c["kaug_static"] = _bf(ka)
    mt = np.zeros((4, 128, 512), np.float32)
    for i in range(4):
        for j in range(4):
            if i // 2 != j // 2:
                continue
            blk = mt[i, :, j * 128:(j + 1) * 128]
            if i > j:
                blk[:] = -BIG
            elif i == j:
                kk = np.arange(128)[:, None]
                qq = np.arange(128)[None, :]
                blk[kk > qq] = -BIG
    c["masktri"] = _bf(mt)
    tile = np.arange(NT)[:, None]
    n = np.arange(16)[None, :]
    valid = (n < tile // 2).astype(np.float32)
    own = (n == tile // 2).astype(np.float32)
    c["valid01"] = np.broadcast_to(valid[None], (128, NT, 16)).astype(np.float32).copy()
    c["own01"] = np.broadcast_to(own[None], (128, NT, 16)).astype(np.float32).copy()
    c["maskv"] = ((c["valid01"] - 1.0) * 1.0e30).astype(np.float32)
    slopes = np.array([2.0 ** (-8.0 * (i + 1) / NH) for i in range(NH)], np.float32)
    stat = np.zeros((NH, 128, NT, 16), np.float32)
    for h in range(NH):
        st = -slopes[h] * (128.0 * tile - 256.0 * n)
        st = np.where(n <= tile // 2, st, 0.0)
        stat[h] = st[None]
    c["stat"] = stat
    p = np.arange(128, dtype=np.float32)
    c["qlo"] = _bf(np.stack([-slopes[h] * p for h in range(NH)], axis=1))
    c["qhi"] = _bf(np.stack([128.0 * slopes[h] * np.ones(128, np.float32) for h in range(NH)], axis=1))
    kb = np.zeros((128, NH, 2), np.float32)
    for h in range(NH):
        for half in range(2):
            kb[:, h, half] = slopes[h] * (p + 128.0 * half)
    c["kbias"] = kb
    c["iota16"] = np.broadcast_to(np.arange(16, dtype=np.float32)[None], (128, 16)).copy()
    c["epsc"] = np.full((128, 1), EPS, np.float32)
    return c


def act(P, out, in_, func, r, w, **kw):
    P.op("act", lambda e: e.activation(out=out, in_=in_, func=func, **kw), r, w)


def tt(P, eng, out, in0, in1, op, r, w):
    P.op(eng, lambda e: e.tensor_tensor(out=out, in0=in0, in1=in1, op=op), r, w)


def ts(P, eng, out, in0, s1, s2, op0, op1, r, w, **kw):
    if s2 is None:
        P.op(eng, lambda e: e.tensor_scalar(out=out, in0=in0, scalar1=s1, scalar2=None, op0=op0, **kw), r, w)
    else:
        P.op(eng, lambda e: e.tensor_scalar(out=out, in0=in0, scalar1=s1, scalar2=s2, op0=op0, op1=op1, **kw), r, w)


def cp(P, eng, out, in_, r, w):
    if eng == "act":
        P.op("act", lambda e: e.copy(out=out, in_=in_), r, w)
    else:
        P.op(eng, lambda e: e.tensor_copy(out=out, in_=in_), r, w)


def mm(P, out, lhsT, rhs, start, stop, r, w):
    P.op("pe", lambda e: e.matmul(out, lhsT, rhs, start=start, stop=stop), r, w)


def tr(P, out, in_, ident, r, w):
    P.op("pe", lambda e: e.transpose(out, in_, ident), r, w)


def dma(P, q, out, in_, r, w):
    P.dma(q, lambda e: e.dma_start(out=out, in_=in_), r, w)


def rmsnorm_stats(P, xt, junk, ss, std, rstd, epsc, tag):
    act(P, junk, xt, AF.Square, [tag + "x"], [tag + "junk", tag + "ss"], accum_out=ss)
    act(P, std, ss, AF.Sqrt, [tag + "ss", "epsc"], [tag + "std"], bias=epsc, scale=1.0 / D)
    P.op("dve", lambda e: e.reciprocal(out=rstd, in_=std), [tag + "std"], [tag + "rstd"])


class Ctx:
    pass


def declare_io(nc, S, debug=False):
    c = Ctx()
    c.S = S

    def inp(name, shape, dt=F32):
        return nc.dram_tensor(name, list(shape), dt, kind="ExternalInput").ap()

    def scr(name, shape, dt):
        return nc.dram_tensor(name, list(shape), dt, kind="ExternalOutput" if debug else "Internal").ap()

    NT = S // 128
    c.x = inp("x", [S, D])
    c.w_in = inp("w_in", [D, NCOL])
    c.g1c = inp("g1c", [128, 8])
    c.conv_wT = inp("conv_wT", [128, 4, CW])
    c.conv_bc = inp("conv_bc", [128, 4])
    c.ln_gc = inp("ln_gc", [128, 4])
    c.ln_bc = inp("ln_bc", [128, 4])
    c.b_pwc = inp("b_pwc", [128, 8])
    c.w_pw = inp("w_conv_pw", [CC, D])
    c.w_ao = inp("w_attn_out", [NH * DH, D])
    c.w_out = inp("w_out", [D, D])
    c.w_pq = inp("w_peer_q", [D, PH * 256])
    c.g2rep = inp("g2rep", [128, D])
    c.gfrep = inp("gfrep", [128, D])
    c.keys1T = inp("keys1T", [128, NK])
    c.keys2T = inp("keys2T", [128, NK])
    c.peer_u = inp("peer_u", [NK * NK, D])
    c.peer_v = inp("peer_v", [NK * NK, D])
    c.ident_bf = inp("ident_bf", [128, 128], BF16)
    c.ident_f = inp("ident_f", [128, 128])
    c.kaug_static = inp("kaug_static", [18, S], BF16)
    c.qhi = inp("qhi", [128, NH], BF16)
    c.masktri = inp("masktri", [4, 128, 512], BF16)
    c.valid01 = inp("valid01", [128, NT, 16])
    c.own01 = inp("own01", [128, NT, 16])
    c.maskv = inp("maskv", [128, NT, 16])
    c.stat = inp("stat", [NH, 128, NT, 16])
    c.qlo = inp("qlo", [128, NH], BF16)
    c.kbias = inp("kbias", [128, NH, 2])
    c.iota16 = inp("iota16", [128, 16])
    c.epsc = inp("epsc", [128, 1])
    c.out = nc.dram_tensor("out", [S, D], F32, kind="ExternalOutput").ap()
    c.zcT = scr("s_zcT", [CC, S], BF16)
    c.qT = scr("s_qT", [NH * DH, S], BF16)
    c.kT = scr("s_kT", [NH * DH, S], BF16)
    c.vS = scr("s_v", [S, NH * DH], BF16)
    c.sgc = scr("s_sgc", [D, S], BF16)
    c.sga = scr("s_sga", [D, S], BF16)
    c.oT = scr("s_oT", [NH * DH, S], BF16)
    c.x1 = scr("s_x1", [S, D], F32)
    c.uv = nc.dram_tensor("s_uv", [NK * NK, 2, D], BF16, kind="Internal").ap()
    c.dbg = scr("s_dbg", [4, 128, 2048], F32) if debug else None
    return c


def phase_a(nc, P, c):
    S = c.S
    NBLK = S // 512
    with ExitStack() as st:
        sb = lambda name, shape, dt: st.enter_context(nc.sbuf_tensor(name, list(shape), dt))
        ps = lambda name, shape, dt: st.enter_context(nc.psum_tensor(name, list(shape), dt))
        wbf = sb("a_wbf", [128, 8, NCOL], BF16)
        wst = [sb(f"a_wst{i}", [128, NCOL], F32) for i in range(2)]
        g1c = sb("a_g1c", [128, 8], F32)
        epsc = sb("a_eps", [128, 1], F32)
        ident = sb("a_ident", [128, 128], BF16)
        xt = [sb(f"a_xt{i}", [128, D], F32) for i in range(3)]
        junk = sb("a_junk", [128, D], F32)
        hn = [sb(f"a_hn{i}", [128, D], BF16) for i in range(2)]
        ss = [sb(f"a_ss{i}", [128, 1], F32) for i in range(2)]
        std = [sb(f"a_std{i}", [128, 1], F32) for i in range(2)]
        rstd = [sb(f"a_rstd{i}", [128, 1], F32) for i in range(2)]
        hnT = [sb(f"a_hnT{i}", [128, 8, 512], BF16) for i in range(2)]
        sig = [sb(f"a_sig{i}", [128, 512], F32) for i in range(2)]
        stg = [sb(f"a_stg{i}", [128, 512], BF16) for i in range(8)]
        tp = ps("a_tp", [128, 8, 128], BF16)
        acc = [ps(f"a_acc{i}", [128, 512], F32) for i in range(6)]

        dma(P, "sp", g1c[:], c.g1c, [], ["g1c"])
        dma(P, "sp", epsc[:], c.epsc, [], ["epsc"])
        dma(P, "sp", ident[:], c.ident_bf, [], ["ident"])
        w_in_v = c.w_in.rearrange("(k p) n -> k p n", p=128)
        for k in range(8):
            dma(P, "sp", wst[k % 2][:], w_in_v[k], [], [f"wst{k%2}"])
            act(P, wbf[:, k, :], wst[k % 2][:], AF.Copy, [f"wst{k%2}", "g1c"], [f"wbf{k}"], scale=g1c[:, k:k + 1])
        wall = [f"wbf{k}" for k in range(8)]

        xr, hr, sr, ar, gr = Rot(3), Rot(2), Rot(2), Rot(6), Rot(8)

        CH = 1024
        conv_list = [(ti, r0) for r0 in range(0, NK * NK, CH) for ti in range(2)]
        nstores = [0]
        per = max(1, (NBLK * 36) // len(conv_list))

        def conv_tick(force=False):
            nstores[0] += 1
            if conv_list and (force or nstores[0] % per == 0):
                ti, r0 = conv_list.pop(0)
                tab = (c.peer_u, c.peer_v)[ti]
                dma(P, "pool", c.uv[r0:r0 + CH, ti, :], tab[r0:r0 + CH, :], [], [])
        x_v = c.x.rearrange("(t p) d -> t p d", p=128)

        def store(dst, src_ps, tok, kind, scale=None):
            g = gr.next()
            npart = dst.shape[0]
            if kind == "sigmoid":
                act(P, stg[g][0:npart, :], src_ps, AF.Sigmoid, [tok], [f"stg{g}"])
            elif kind == "scale":
                act(P, stg[g][0:npart, :], src_ps, AF.Copy, [tok], [f"stg{g}"], scale=scale)
            else:
                cp(P, "dve", stg[g][0:npart, :], src_ps, [tok], [f"stg{g}"])
            dma(P, "pool", dst, stg[g][0:npart, :], [f"stg{g}"], [])
            conv_tick()

        for b in range(NBLK):
            hb = hr.next()
            for j in range(4):
                t = b * 4 + j
                xi = xr.next()
                si = sr.next()
                dma(P, "sp", xt[xi][:], x_v[t], [], [f"xt{xi}x"])
                rmsnorm_stats(P, xt[xi][:], junk[:], ss[si][:], std[si][:], rstd[si][:], epsc[:], f"xt{xi}")
                act(P, hn[si][:], xt[xi][:], AF.Copy, [f"xt{xi}x", f"xt{xi}rstd"], [f"hn{si}"], scale=rstd[si][:])
                for k in range(8):
                    tr(P, tp[:, k, :], hn[si][:, k * 128:(k + 1) * 128], ident[:], [f"hn{si}", "ident"], ["tp"])
                cp(P, "dve", hnT[hb][:, :, j * 128:(j + 1) * 128], tp[:], ["tp"], [f"hnT{hb}"])
            cols = slice(b * 512, (b + 1) * 512)

            def proj(c0, m):
                a = ar.next()
                for k in range(8):
                    mm(P, acc[a][0:m, :], wbf[:, k, c0:c0 + m], hnT[hb][:, k, :], k == 0, k == 7,
                       wall + [f"hnT{hb}"], [f"acc{a}"])
                return acc[a], f"acc{a}"

            for cc in range(4):
                pa, ta = proj(cc * 128, 128)
                pg, tg = proj(CC + cc * 128, 128)
                s_i = sr.next()
                act(P, sig[s_i][:], pg[:], AF.Sigmoid, [tg], [f"sig{s_i}"])
                g = gr.next()
                tt(P, "dve", stg[g][:], pa[:], sig[s_i][:], ALU.mult, [ta, f"sig{s_i}"], [f"stg{g}"])
                dma(P, "pool", c.zcT[cc * 128:(cc + 1) * 128, cols], stg[g][:], [f"stg{g}"], [])
                conv_tick()
            for cc in range(4):
                pq, tq = proj(2 * CC + cc * 128, 128)
                store(c.qT[cc * 128:(cc + 1) * 128, cols], pq[:], tq, "scale", scale=DH ** -0.5)
            for cc in range(4):
                pk, tk = proj(2 * CC + 512 + cc * 128, 128)
                store(c.kT[cc * 128:(cc + 1) * 128, cols], pk[:], tk, "copy")
            for cc in range(8):
                pg, tg = proj(2 * CC + 1536 + cc * 128, 128)
                store(c.sgc[cc * 128:(cc + 1) * 128, cols], pg[:], tg, "sigmoid")
            for cc in range(8):
                pg, tg = proj(2 * CC + 1536 + D + cc * 128, 128)
                store(c.sga[cc * 128:(cc + 1) * 128, cols], pg[:], tg, "sigmoid")
            for j in range(4):
                a = ar.next()
                for k in range(8):
                    mm(P, acc[a][:], hnT[hb][:, k, j * 128:(j + 1) * 128], wbf[:, k, 2 * CC + 1024:2 * CC + 1536],
                       k == 0, k == 7, wall + [f"hnT{hb}"], [f"acc{a}"])
                t = b * 4 + j
                store(c.vS[t * 128:(t + 1) * 128, :], acc[a][:], f"acc{a}", "copy")
        while conv_list:
            conv_tick(force=True)
        P.barrier()
        P.emit()


def host_inputs(S, x_b, w, consts):
    f = lambda a: np.ascontiguousarray(np.asarray(a, dtype=np.float32))
    col = lambda v, n: f(np.asarray(v).reshape(n, 128).T)
    m = {
        "x": f(x_b),
        "w_in": f(w["w_in"][0]),
        "g1c": col(w["g_norm1"][0], 8),
        "conv_wT": f(np.asarray(w["conv_w"][0]).reshape(CW, 4, 128).transpose(2, 1, 0)),
        "conv_bc": col(w["conv_b"][0], 4),
        "ln_gc": col(w["conv_ln_g"][0], 4),
        "ln_bc": col(w["conv_ln_b"][0], 4),
        "b_pwc": col(w["b_conv_pw"][0], 8),
        "w_conv_pw": f(w["w_conv_pw"][0]),
        "w_attn_out": f(w["w_attn_out"][0]),
        "w_out": f(w["w_out"][0]),
        "w_peer_q": f(w["w_peer_q"][0]),
        "g2rep": f(np.broadcast_to(np.asarray(w["g_norm2"][0])[None, :], (128, D))),
        "gfrep": f(np.broadcast_to(np.asarray(w["g_final"])[None, :], (128, D))),
        "keys1T": f(np.asarray(w["peer_keys1"][0]).T),
        "keys2T": f(np.asarray(w["peer_keys2"][0]).T),
        "peer_u": f(w["peer_u"][0]),
        "peer_v": f(w["peer_v"][0]),
    }
    m.update(consts)
    return m


def phase_b(nc, P, c, preload=None):
    S = c.S
    NT = S // 128
    NQB = S // 512
    with ExitStack() as st:
        sb = lambda name, shape, dt: st.enter_context(nc.sbuf_tensor(name, list(shape), dt))
        ps = lambda name, shape, dt: st.enter_context(nc.psum_tensor(name, list(shape), dt))
        kaug = [sb(f"b_kaug{i}", [128, S], BF16) for i in range(2)]
        qaug = [sb(f"b_qaug{i}", [128, S], BF16) for i in range(2)]
        vall = sb("b_vall", [128, NT, NH * DH], BF16)
        vaug = [sb(f"b_vaug{i}", [128, NT, DH + 1], BF16) for i in range(2)]
        augtok = sb("b_augtok", [128, NT, 82], BF16)
        valid01 = sb("b_valid", [128, NT, 16], F32)
        own01 = sb("b_own", [128, NT, 16], F32)
        maskv = sb("b_maskv", [128, NT, 16], F32)
        stat = [sb(f"b_stat{i}", [128, NT, 16], F32) for i in range(2)]
        kbias = sb("b_kbias", [128, NH, 2], F32)
        qlo = sb("b_qlo", [128, NH], BF16)
        qhi = sb("b_qhi", [128, NH], BF16)
        masktri = sb("b_masktri", [128, 4, 512], BF16)
        ident = sb("b_ident", [128, 128], BF16)
        onesf = sb("b_onesf", [128, 64], F32)
        kms = sb("b_kms", [128, 16], F32)
        kmb = [sb(f"b_kmb{i}", [128, 16], BF16) for i in range(2)]
        bsm = sb("b_bsm", [128, NT, 16], F32)
        m8 = sb("b_m8", [128, NT, 8], F32)
        sel = sb("b_sel", [128, NT, 16], F32)
        PT = [sb(f"b_PT{i}", [128, 2, 512], BF16) for i in range(3)]
        rden = [sb(f"b_rden{i}", [128, 512], F32) for i in range(2)]
        bc_sb = sb("b_bcsb", [128, 512], F32)
        oT_sb = [sb(f"b_oT{i}", [128, 512], BF16) for i in range(2)]
        st_ps = [ps(f"b_st{i}", [128, 2, 512], F32) for i in range(2)]
        o_ps = [ps(f"b_o{i}", [128, 512], F32) for i in range(2)]
        shps = ps("b_sh", [128, 512], F32)
        bs_ps = shps[:, 0:NT * 16].rearrange("p (t n) -> p t n", n=16)
        tp = shps[:].bitcast(BF16).rearrange("p (a b) -> p a b", b=128)
        bc_ps = ps("b_bc", [128, 512], F32)

        for i in range(2):
            dma(P, "sp", kaug[i][64:82, :], c.kaug_static, [], [f"kaug_s{i}"])
        dma(P, "sp", vall[:], c.vS.rearrange("(t p) n -> p t n", p=128), [], ["vall"])
        dma(P, "sp", valid01[:], c.valid01, [], ["valid01"])
        dma(P, "sp", own01[:], c.own01, [], ["own01"])
        dma(P, "sp", maskv[:], c.maskv, [], ["maskv"])
        dma(P, "sp", kbias[:], c.kbias, [], ["kbias"])
        dma(P, "sp", qlo[:], c.qlo, [], ["qlo"])
        dma(P, "sp", qhi[:], c.qhi, [], ["qhi"])
        dma(P, "sp", masktri[:], c.masktri.rearrange("i p n -> p i n"), [], ["masktri"])
        dma(P, "sp", ident[:], c.ident_bf, [], ["ident"])
        P.op("dve", lambda e: e.memset(onesf[:], 1.0), [], ["onesf"])
        P.op("dve", lambda e: e.memset(augtok[:], 0.0), [], ["augtok"])
        for i in range(2):
            P.op("dve", (lambda i: lambda e: e.memset(vaug[i][:], 1.0))(i), [], [f"vaug{i}"])

        def prologue(h):
            hp = h % 2
            hs = slice(h * DH, (h + 1) * DH)
            KA, QA, VA, ST, KM = f"kaug{hp}", f"qaug{hp}", f"vaug{hp}", f"stat{hp}", f"kmb{hp}"
            dma(P, "sp", kaug[hp][0:64, :], c.kT[hs, :], [], [KA])
            dma(P, "sp", qaug[hp][0:64, :], c.qT[hs, :], [], [QA])
            dma(P, "sp", stat[hp][:], c.stat[h], [], [ST])
            cp(P, "pool", vaug[hp][:, :, 0:DH], vall[:, :, hs], ["vall"], [VA])
            yield
            P.op("dve", lambda e: e.tensor_reduce(out=kms[0:64, 0:S // MB], in_=kaug[hp][0:64, :].rearrange("p (n k) -> p n k", k=MB),
                                                  axis=AX.X, op=ALU.add), [KA], ["kms"])
            P.op("act", lambda e: e.mul(out=kmb[hp][0:64, 0:S // MB], in_=kms[0:64, 0:S // MB], mul=1.0 / MB), ["kms"], [KM])
            if S // MB < 16:
                P.op("dve", lambda e: e.memset(kmb[hp][0:64, S // MB:16], 0.0), [], [KM])
            yield
            for t0 in range(0, NT, 8):
                for t in range(t0, t0 + 8):
                    mm(P, bs_ps[:, t, :], qaug[hp][0:64, t * 128:(t + 1) * 128], kmb[hp][0:64, :], True, True, [QA, KM], ["shps"])
                yield
            tt(P, "dve", bsm[:], bs_ps, maskv[:], ALU.add, ["shps", "maskv"], ["bsm"])
            for t0 in range(0, NT, 8):
                for t in range(t0, t0 + 8):
                    P.op("dve", (lambda t: lambda e: e.max(out=m8[:, t, :], in_=bsm[:, t, :]))(t), ["bsm"], ["m8"])
                yield
            tt(P, "dve", sel[:], bsm[:], m8[:, :, 2:3].to_broadcast([128, NT, 16]), ALU.is_ge, ["bsm", "m8"], ["sel"])
            tt(P, "dve", sel[:], sel[:], valid01[:], ALU.mult, ["sel", "valid01"], ["sel"])
            tt(P, "dve", sel[:], sel[:], own01[:], ALU.add, ["sel", "own01"], ["sel"])
            ts(P, "dve", sel[:], sel[:], -1.0, BIG, ALU.add, ALU.mult, ["sel"], ["sel"])
            tt(P, "dve", augtok[:, :, 65:81], sel[:], stat[hp][:], ALU.add, ["sel", ST], ["augtok"])
            cp(P, "dve", augtok[:, :, 64], qlo[:, h:h + 1].to_broadcast([128, NT]), ["qlo"], ["augtok"])
            cp(P, "dve", augtok[:, :, 81], qhi[:, h:h + 1].to_broadcast([128, NT]), ["qhi"], ["augtok"])
            yield
            for t0 in range(0, NT, 8):
                for j in range(8):
                    tr(P, tp[0:82, j, :], augtok[:, t0 + j, :], ident[:], ["augtok", "ident"], ["shps"])
                cp(P, "dve", qaug[hp][64:82, t0 * 128:(t0 + 8) * 128].rearrange("p (a b) -> p a b", b=128), tp[64:82, :, :],
                   ["shps"], [QA])
                yield

        def advance(gen, n):
            if gen is None:
                return None
            for _ in range(n):
                try:
                    next(gen)
                except StopIteration:
                    return None
            return gen

        sr, pr, orr, osr = Rot(2), Rot(3), Rot(2), Rot(2)
        gen = prologue(0)
        while gen is not None:
            gen = advance(gen, 1)
        if preload is not None:
            preload()
        for h in range(NH):
            hp = h % 2
            hs = slice(h * DH, (h + 1) * DH)
            KA, QA, VA = f"kaug{hp}", f"qaug{hp}", f"vaug{hp}"
            units = [(qb, kp) for qb in range(NQB) for kp in range(2 * qb + 2)]
            NU = len(units)
            sbuf_of = {}
            obuf_of = {}
            pending = []

            def col0(qb, kt):
                return (kt - 4 * qb) * 128 if kt >= 4 * qb else 0

            def emit_s(n):
                qb, kp = units[n]
                si = sr.next()
                sbuf_of[n] = si
                for half in range(2):
                    kt = 2 * kp + half
                    diag = kt >= 4 * qb
                    c0 = col0(qb, kt)
                    mm(P, st_ps[si][:, half, c0:512], kaug[hp][0:82, kt * 128:(kt + 1) * 128],
                       qaug[hp][0:82, qb * 512 + c0:(qb + 1) * 512], True, not diag, [KA, f"kaug_s{hp}", QA], [f"st{si}"])
                    if diag:
                        mm(P, st_ps[si][:, half, c0:c0 + 128], ident[:], masktri[:, kt - 4 * qb, c0:c0 + 128], False, True,
                           ["ident", "masktri"], [f"st{si}"])

            def emit_pv(n):
                qb, kp = units[n]
                nkt = 4 * qb + 4
                si = sbuf_of.pop(n)
                if kp == 0:
                    obuf_of[qb] = orr.next()
                oi = obuf_of[qb]
                pi = pr.next()
                cmin = col0(qb, 2 * kp)
                act(P, PT[pi][:, :, cmin:512], st_ps[si][:, :, cmin:512], AF.Exp, [f"st{si}", "kbias"], [f"PT{pi}"],
                    bias=kbias[:, h, 0:1])
                for half in range(2):
                    kt = 2 * kp + half
                    c0 = col0(qb, kt)
                    mm(P, o_ps[oi][0:65, c0:512], vaug[hp][:, kt, :], PT[pi][:, half, c0:512], kt == 0, kt == nkt - 1,
                       [VA, f"PT{pi}"], [f"o{oi}"])
                if kp == 2 * qb + 1:
                    o = o_ps[oi]
                    ri = qb % 2
                    P.op("dve", lambda e: e.reciprocal(out=rden[ri][64:65, :], in_=o[64:65, :]), [f"o{oi}"], [f"rden{ri}"])

                    def fin2(qb=qb, oi=oi, o=o, ri=ri):
                        mm(P, bc_ps[0:64, :], onesf[64:65, 0:64], rden[ri][64:65, :], True, True, ["onesf", f"rden{ri}"], ["bc_ps"])
                        cp(P, "act", bc_sb[0:64, :], bc_ps[0:64, :], ["bc_ps"], ["bc_sb"])
                        oo = osr.next()
                        tt(P, "dve", oT_sb[oo][0:64, :], o[0:64, :], bc_sb[0:64, :], ALU.mult, [f"o{oi}", "bc_sb"], [f"oT{oo}"])
                        dma(P, "pool", c.oT[hs, qb * 512:(qb + 1) * 512], oT_sb[oo][0:64, :], [f"oT{oo}"], [])
                    pending.append((n + 1, fin2))

            gen = prologue(h + 1) if h + 1 < NH else None
            emit_s(0)
            for n in range(NU):
                if n + 1 < NU:
                    emit_s(n + 1)
                for _ in range(2):
                    mm(P, bc_ps[:, :], ident[:], masktri[:, 0, :], True, True, ["ident", "masktri"], ["bc_ps"])
                emit_pv(n)
                while pending and pending[0][0] <= n:
                    pending.pop(0)[1]()
                if n >= 4 and n % 2 == 0:
                    gen = advance(gen, 1)
            while pending:
                pending.pop(0)[1]()
            while gen is not None:
                gen = advance(gen, 1)
        P.barrier()
        P.emit()


def alloc_c_weights(nc, st):
    sb = lambda name, shape, dt: st.enter_context(nc.sbuf_tensor(name, list(shape), dt))
    wpw = sb("c_wpw", [128, 4, D], BF16)
    wao = sb("c_wao", [128, NH // 2, D], BF16)
    wout = sb("c_wout", [128, 8, D], BF16)
    dg = sb("c_dg", [128, 4, CW, 128], BF16)
    identf = sb("c_identf", [128, 128], F32)
    convw = sb("c_convw", [128, 4, CW], F32)
    convb = sb("c_convb", [128, 4], F32)
    lng = sb("c_lng", [128, 4], F32)
    lnb = sb("c_lnb", [128, 4], F32)
    bpw = sb("c_bpw", [128, 8], F32)
    epsc = sb("c_eps", [128, 1], F32)
    onesm = sb("c_onesm", [128, 128], F32)
    return (wpw, wao, wout, dg, identf, convw, convb, lng, lnb, bpw, epsc, onesm)


def load_c_weights(nc, P, c, wts, wst):
    (wpw, wao, wout, dg, identf, convw, convb, lng, lnb, bpw, epsc, onesm) = wts
    for name, t, src in (("identf", identf, c.ident_f), ("convw", convw, c.conv_wT), ("convb", convb, c.conv_bc),
                         ("lng", lng, c.ln_gc), ("lnb", lnb, c.ln_bc), ("bpw", bpw, c.b_pwc), ("epsc", epsc, c.epsc)):
        dma(P, "sp", t[:], src, [], [name])
    P.op("dve", lambda e: e.memset(onesm[:], 1.0 / CC), [], ["onesm"])
    dma(P, "pool", wpw[:], c.w_pw.rearrange("(k p) n -> p k n", p=128), [], ["wpw"])
    dma(P, "pool", wao[:], c.w_ao.rearrange("(k p) n -> p k n", p=128), [], ["wao"])
    dma(P, "pool", wout[:], c.w_out.rearrange("(k p) n -> p k n", p=128), [], ["wout"])
    for cc in range(4):
        tt(P, "dve", dg[:, cc, :, :], identf[:].unsqueeze(1).to_broadcast([128, CW, 128]),
           convw[:, cc, :].unsqueeze(2).to_broadcast([128, CW, 128]), ALU.mult, ["identf", "convw"], ["dg"])


def phase_c(nc, P, c, wts):
    S = c.S
    NBLK = S // 512
    HALO = CW - 1
    with ExitStack() as st:
        sb = lambda name, shape, dt: st.enter_context(nc.sbuf_tensor(name, list(shape), dt))
        ps = lambda name, shape, dt: st.enter_context(nc.psum_tensor(name, list(shape), dt))
        (wpw, wao, wout, dg, identf, convw, convb, lng, lnb, bpw, epsc, onesm) = wts
        zcb = [sb(f"c_zcb{i}", [128, 4, 512 + HALO], BF16) for i in range(2)]
        zconv = sb("c_zconv", [128, 4, 512], F32)
        zsq = sb("c_zsq", [128, 4, 512], F32)
        mean_sb = sb("c_mean", [128, 512], F32)
        msq = sb("c_msq", [128, 512], F32)
        var = sb("c_var", [128, 512], F32)
        rstd = sb("c_rstd", [128, 512], F32)
        t1 = [sb(f"c_t1{i}", [128, 512], F32) for i in range(2)]
        zact = sb("c_zact", [128, 4, 512], BF16)
        oTb = [sb(f"c_oTb{i}", [128, NH // 2, 512], BF16) for i in range(2)]
        sgcb = [sb(f"c_sgcb{i}", [128, 8, 512], BF16) for i in range(2)]
        sgab = [sb(f"c_sgab{i}", [128, 8, 512], BF16) for i in range(2)]
        mixc = [sb(f"c_mixc{i}", [128, 512], F32) for i in range(2)]
        mixa = [sb(f"c_mixa{i}", [128, 512], F32) for i in range(2)]
        mix = sb("c_mix", [128, 8, 512], BF16)
        xt = [sb(f"c_xt{i}", [128, D], F32) for i in range(2)]
        x1t = [sb(f"c_x1t{i}", [128, D], F32) for i in range(2)]
        cv_ps = [ps(f"c_cv{i}", [128, 512], F32) for i in range(2)]
        mean_ps = ps("c_meanps", [128, 512], F32)
        ex2_ps = ps("c_ex2ps", [128, 512], F32)
        y_ps = [ps(f"c_y{i}", [128, 512], F32) for i in range(2)]
        o_ps = [ps(f"c_op{i}", [128, 512], F32) for i in range(2)]

        zc_v = c.zcT.rearrange("(c p) s -> p c s", p=128)
        sgc_v = c.sgc.rearrange("(f p) s -> p f s", p=128)
        sga_v = c.sga.rearrange("(f p) s -> p f s", p=128)
        oT_v = c.oT.rearrange("(h d) s -> d h s", d=2 * DH)
        x_v = c.x.rearrange("(t p) d -> t p d", p=128)
        x1_v = c.x1.rearrange("(t p) d -> t p d", p=128)
        cr, tr1, yr, opr, xr, mr = Rot(2), Rot(2), Rot(2), Rot(2), Rot(2), Rot(2)
        for b in range(NBLK):
            bi = b % 2
            cols = slice(b * 512, (b + 1) * 512)
            if b == 0:
                P.op("dve", lambda e: e.memset(zcb[0][:, :, 0:HALO], 0.0), [], ["zcb0"])
                dma(P, "sp", zcb[0][:, :, HALO:], zc_v[:, :, 0:512], [], ["zcb0"])
            else:
                dma(P, "sp", zcb[bi][:], zc_v[:, :, b * 512 - HALO:(b + 1) * 512], [], [f"zcb{bi}"])
            dma(P, "sp", sgcb[bi][:], sgc_v[:, :, cols], [], [f"sgcb{bi}"])
            dma(P, "sp", sgab[bi][:], sga_v[:, :, cols], [], [f"sgab{bi}"])
            dma(P, "sp", oTb[bi][:], oT_v[:, :, cols], [], [f"oTb{bi}"])
            for cc in range(4):
                ci = cr.next()
                for k in range(CW):
                    mm(P, cv_ps[ci][:], dg[:, cc, k, :], zcb[bi][:, cc, k:k + 512], k == 0, k == CW - 1,
                       ["dg", f"zcb{bi}"], [f"cv{ci}"])
                act(P, zconv[:, cc, :], cv_ps[ci][:], AF.Identity, [f"cv{ci}", "convb"], [f"zconv{cc}"], bias=convb[:, cc:cc + 1])
                act(P, zsq[:, cc, :], zconv[:, cc, :], AF.Square, [f"zconv{cc}"], [f"zsq{cc}"])
            for cc in range(4):
                mm(P, mean_ps[:], onesm[:], zconv[:, cc, :], cc == 0, cc == 3, ["onesm", f"zconv{cc}"], ["mean_ps"])
            for cc in range(4):
                mm(P, ex2_ps[:], onesm[:], zsq[:, cc, :], cc == 0, cc == 3, ["onesm", f"zsq{cc}"], ["ex2_ps"])
            cp(P, "act", mean_sb[:], mean_ps[:], ["mean_ps"], ["mean_sb"])
            act(P, msq[:], mean_ps[:], AF.Square, ["mean_ps"], ["msq"])
            tt(P, "dve", var[:], ex2_ps[:], msq[:], ALU.subtract, ["ex2_ps", "msq"], ["var"])
            act(P, var[:], var[:], AF.Sqrt, ["var", "epsc"], ["var"], bias=epsc[:], scale=1.0)
            P.op("dve", lambda e: e.reciprocal(out=rstd[:], in_=var[:]), ["var"], ["rstd"])
            for cc in range(4):
                ti = tr1.next()
                tt(P, "dve", t1[ti][:], zconv[:, cc, :], mean_sb[:], ALU.subtract, [f"zconv{cc}", "mean_sb"], [f"t1{ti}"])
                tt(P, "dve", t1[ti][:], t1[ti][:], rstd[:], ALU.mult, [f"t1{ti}", "rstd"], [f"t1{ti}"])
                act(P, zact[:, cc, :], t1[ti][:], AF.Silu, [f"t1{ti}", "lng", "lnb"], [f"zact{cc}"],
                    scale=lng[:, cc:cc + 1], bias=lnb[:, cc:cc + 1])
            zall = [f"zact{cc}" for cc in range(4)]
            if c.dbg is not None and b == 0:
                dma(P, "sp", c.dbg[1, :, 0:512], mean_sb[:], ["mean_sb"], [])
                dma(P, "sp", c.dbg[1, :, 512:1024], var[:], ["var"], [])
                dma(P, "sp", c.dbg[1, :, 1024:1536], rstd[:], ["rstd"], [])
            for f in range(8):
                fs = slice(f * 128, (f + 1) * 128)
                yi = yr.next()
                for cc in range(4):
                    mm(P, y_ps[yi][:], wpw[:, cc, fs], zact[:, cc, :], cc == 0, cc == 3, ["wpw"] + zall, [f"y{yi}"])
                mi = mr.next()
                act(P, mixc[mi][:], y_ps[yi][:], AF.Identity, [f"y{yi}", "bpw"], [f"mixc{mi}"], bias=bpw[:, f:f + 1])
                tt(P, "dve", mixc[mi][:], mixc[mi][:], sgcb[bi][:, f, :], ALU.mult, [f"mixc{mi}", f"sgcb{bi}"], [f"mixc{mi}"])
                yi2 = yr.next()
                for h in range(NH // 2):
                    mm(P, y_ps[yi2][:], wao[:, h, fs], oTb[bi][:, h, :], h == 0, h == NH // 2 - 1,
                       ["wao", f"oTb{bi}"], [f"y{yi2}"])
                tt(P, "dve", mixa[mi][:], y_ps[yi2][:], sgab[bi][:, f, :], ALU.mult, [f"y{yi2}", f"sgab{bi}"], [f"mixa{mi}"])
                tt(P, "pool", mix[:, f, :], mixa[mi][:], mixc[mi][:], ALU.add, [f"mixa{mi}", f"mixc{mi}"], [f"mix{f}"])
            mall = [f"mix{f}" for f in range(8)]
            for j in range(4):
                t = b * 4 + j
                xi = xr.next()
                dma(P, "sp", xt[xi][:], x_v[t], [], [f"xt{xi}"])
                for half in range(2):
                    oi = opr.next()
                    hsl = slice(half * 512, (half + 1) * 512)
                    for f in range(8):
                        mm(P, o_ps[oi][:], mix[:, f, j * 128:(j + 1) * 128], wout[:, f, hsl], f == 0, f == 7,
                           mall + ["wout"], [f"op{oi}"])
                    tt(P, "dve", x1t[xi][:, hsl], xt[xi][:, hsl], o_ps[oi][:], ALU.add, [f"xt{xi}", f"op{oi}"], [f"x1t{xi}"])
                dma(P, "pool", x1_v[t], x1t[xi][:], [f"x1t{xi}"], [])
        P.barrier()
        P.emit()


def phase_d(nc, P, c):
    S = c.S
    NT = S // 128
    G = 2
    NB_G = 8
    NSL = PH * 16
    with ExitStack() as st:
        sb = lambda name, shape, dt: st.enter_context(nc.sbuf_tensor(name, list(shape), dt))
        ps = lambda name, shape, dt: st.enter_context(nc.psum_tensor(name, list(shape), dt))
        wq = sb("d_wq", [128, 8, PH * 256], BF16)
        kT = [sb(f"d_keysT{i}", [128, NK], BF16) for i in range(2)]
        kTf = sb("d_keysTf", [128, NK], F32)
        g2rep = sb("d_g2rep", [128, D], F32)
        gfrep = sb("d_gfrep", [128, D], F32)
        identb = sb("d_identb", [128, 128], BF16)
        epsc = sb("d_eps", [128, 1], F32)
        iota16 = sb("d_iota16", [128, 16], F32)
        thr17 = sb("d_thr17", [128, 17], F32)
        x1t = [sb(f"d_x1t{i}", [128, D], F32) for i in range(3)]
        tmp = sb("d_tmp", [128, D], F32)
        hn2 = sb("d_hn2", [128, D], F32)
        hn2b = [sb(f"d_hn2b{i}", [128, D], BF16) for i in range(2)]
        hn2T = sb("d_hn2T", [128, 8, 128], BF16)
        ss = sb("d_ss", [128, 1], F32)
        std = sb("d_std", [128, 1], F32)
        rstd = sb("d_rstd", [128, 1], F32)
        fss = sb("d_fss", [128, 1], F32)
        fstd = sb("d_fstd", [128, 1], F32)
        frstd = sb("d_frstd", [128, 1], F32)
        qT_sb = sb("d_qT", [128, 16, 128], BF16)
        s_sb = sb("d_s", [128, 16, NK], F32)
        gebuf = sb("d_ge", [128, PH * 16 * 17], F32)
        ohbuf = sb("d_oh", [128, PH * 256], F32)
        s2 = gebuf[:, 0:16 * NK].rearrange("p (g n) -> p g n", n=NK)
        ge = gebuf[:].rearrange("p (h k a) -> p h k a", k=16, a=17)
        cand2 = ohbuf[:].rearrange("p (h n) -> p h n", n=256)
        oh = ohbuf[:].rearrange("p (h k a) -> p h k a", k=16, a=16)
        m16 = sb("d_m16", [128, 16, 16], F32)
        i16 = sb("d_i16", [128, 16, 16], U32)
        i16f = sb("d_i16f", [128, 16, 16], F32)
        cand = sb("d_cand", [128, PH, 256], F32)
        sc = sb("d_sc", [128, PH, 16], F32)
        pos = sb("d_pos", [128, PH, 16], U32)
        posf = sb("d_posf", [128, PH, 16], F32)
        af = sb("d_af", [128, PH, 16], F32)
        bf_ = sb("d_bf", [128, PH, 16], F32)
        i1s = sb("d_i1s", [128, PH, 16], F32)
        i2s = sb("d_i2s", [128, PH, 16], F32)
        eidx = [sb(f"d_eidx{i}", [128, NSL], U32) for i in range(2)]
        ee = sb("d_ee", [128, PH, 16], F32)
        zz = sb("d_zz", [128, PH], F32)
        gg = [sb(f"d_gg{i}", [128, NSL], F32) for i in range(2)]
        adot = sb("d_adot", [128, NSL], F32)
        ga = sb("d_ga", [128, NSL], F32)
        cw = sb("d_cw", [128, NSL], F32)
        uvg = [sb(f"d_uvg{i}", [128, G, 2, D], BF16) for i in range(NB_G)]
        prodb = [sb(f"d_prodb{i}", [128, D], BF16) for i in range(4)]
        junkb = sb("d_junkb", [128, D], BF16)
        dgc = [sb(f"d_dgc{i}", [128, 128], BF16) for i in range(8)]
        x2 = sb("d_x2", [128, D], F32)
        ot = sb("d_ot", [128, D], F32)
        tp = ps("d_tp", [128, 8, 128], BF16)
        qs_ps = [ps(f"d_qs{i}", [128, 4, 128], F32) for i in range(2)]
        y_ps = [[ps(f"d_y{i}{h}", [128, 512], F32) for h in range(2)] for i in range(2)]
        fold_ps = ps("d_fold", [128, 512], F32)
        junks = sb("d_junks", [128, 128], BF16)

        for name, t_, src in (("g2rep", g2rep, c.g2rep), ("gfrep", gfrep, c.gfrep),
                              ("identb", identb, c.ident_bf), ("epsc", epsc, c.epsc), ("iota16", iota16, c.iota16)):
            dma(P, "sp", t_[:], src, [], [name])
        for i, src in enumerate((c.keys1T, c.keys2T)):
            dma(P, "pool", kT[i][:], src, [], [f"kT{i}"])
        ts(P, "dve", thr17[:, 0:16], iota16[:], 16.0, None, ALU.mult, None, ["iota16"], ["thr17"])
        P.op("dve", lambda e: e.memset(thr17[:, 16:17], 256.0), [], ["thr17b"])
        dma(P, "pool", wq[:], c.w_pq.rearrange("(k p) n -> p k n", p=128), [], ["wq"])

        x1_v = c.x1.rearrange("(t p) d -> t p d", p=128)
        out_v = c.out.rearrange("(t p) d -> t p d", p=128)
        uv_v = c.uv.rearrange("e s d -> e (s d)")
        m16v = m16[:].rearrange("p (h s) k -> p h s k", s=2)
        i16fv = i16f[:].rearrange("p (h s) k -> p h s k", s=2)
        cand4 = cand[:].rearrange("p h (a b) -> p h a b", b=16)
        B4 = [128, PH, 16, 16]

        def prologue(t):
            par = t % 2
            xp = t % 3
            X, HB, EI, GG = f"x1t{xp}", f"hn2b{par}", f"eidx{par}", f"gg{par}"
            dma(P, "sp", x1t[xp][:], x1_v[t], [], [X])
            yield
            act(P, junkb[:], x1t[xp][:], AF.Square, [X], ["junkb", "ss"], accum_out=ss[:])
            yield
            act(P, std[:], ss[:], AF.Sqrt, ["ss", "epsc"], ["std"], bias=epsc[:], scale=1.0 / D)
            yield
            P.op("dve", lambda e: e.reciprocal(out=rstd[:], in_=std[:]), ["std"], ["rstd"])
            yield
            act(P, tmp[:], x1t[xp][:], AF.Copy, [X, "rstd"], ["tmp"], scale=rstd[:])
            yield
            tt(P, "dve", hn2[:], tmp[:], g2rep[:], ALU.mult, ["tmp", "g2rep"], ["hn2"])
            yield
            cp(P, "act", hn2b[par][:], hn2[:], ["hn2"], [HB])
            yield
            for k in range(8):
                tr(P, tp[:, k, :], hn2b[par][:, k * 128:(k + 1) * 128], identb[:], [HB, "identb"], ["tp"])
            yield
            cp(P, "dve", hn2T[:], tp[:], ["tp"], ["hn2T"])
            yield

            def qmm(g4):
                for gi in range(4):
                    g = g4 * 4 + gi
                    for k in range(8):
                        mm(P, qs_ps[g4 % 2][:, gi, :], wq[:, k, g * 128:(g + 1) * 128], hn2T[:, k, :], k == 0, k == 7,
                           ["wq", "hn2T"], [f"qs{g4 % 2}"])

            def qcp(g4):
                cp(P, "act", qT_sb[:, g4 * 4:(g4 + 1) * 4, :], qs_ps[g4 % 2][:], [f"qs{g4 % 2}"], [f"qT{g4}"])

            def smm(g4):
                for gi in range(4):
                    g = g4 * 4 + gi
                    mm(P, qs_ps[g4 % 2][:, gi, :], qT_sb[:, g, :], kT[g % 2][:], True, True, [f"qT{g4}", f"kT{g%2}"], [f"qs{g4 % 2}"])

            def scp(g4):
                cp(P, "act", s_sb[:, g4 * 4:(g4 + 1) * 4, :], qs_ps[g4 % 2][:], [f"qs{g4 % 2}"], [f"s{g4}"])

            for g4 in range(5):
                if g4 < 4:
                    qmm(g4)
                if g4 >= 1:
                    qcp(g4 - 1)
                yield
            for g4 in range(5):
                if g4 < 4:
                    smm(g4)
                if g4 >= 1:
                    scp(g4 - 1)
                yield
            for g in range(16):
                P.op("dve", (lambda g: lambda e: e.max(out=m16[:, g, 0:8], in_=s_sb[:, g, :]))(g), [f"s{g // 4}"], [f"m16a{g}"])
                if g % 4 == 3:
                    yield
            for g in range(16):
                P.op("dve", (lambda g: lambda e: e.max_index(out=i16[:, g, 0:8], in_max=m16[:, g, 0:8], in_values=s_sb[:, g, :]))(g),
                     [f"s{g // 4}", f"m16a{g}"], [f"i16a{g}"])
                if g % 4 == 3:
                    yield
            for g in range(16):
                P.op("dve", (lambda g: lambda e: e.match_replace(out=s2[:, g, :], in_to_replace=m16[:, g, 0:8],
                                                                 in_values=s_sb[:, g, :], imm_value=NEG))(g),
                     [f"s{g // 4}", f"m16a{g}"], [f"s2_{g}"])
                if g % 4 == 3:
                    yield
            for g in range(16):
                P.op("dve", (lambda g: lambda e: e.max(out=m16[:, g, 8:16], in_=s2[:, g, :]))(g), [f"s2_{g}"], [f"m16b{g}"])
                if g % 4 == 3:
                    yield
            for g in range(16):
                P.op("dve", (lambda g: lambda e: e.max_index(out=i16[:, g, 8:16], in_max=m16[:, g, 8:16], in_values=s2[:, g, :]))(g),
                     [f"s2_{g}", f"m16b{g}"], [f"i16b{g}"])
                if g % 4 == 3:
                    yield
            m16all = [f"m16a{g}" for g in range(16)] + [f"m16b{g}" for g in range(16)]
            i16all = [f"i16a{g}" for g in range(16)] + [f"i16b{g}" for g in range(16)]
            cp(P, "dve", i16f[:], i16[:], i16all, ["i16f"])
            tt(P, "dve", cand4, m16v[:, :, 0, :].unsqueeze(3).to_broadcast(B4), m16v[:, :, 1, :].unsqueeze(2).to_broadcast(B4),
               ALU.add, m16all, ["cand"])
            yield
            for h in range(PH):
                P.op("dve", (lambda h: lambda e: e.max(out=sc[:, h, 0:8], in_=cand[:, h, :]))(h), ["cand"], [f"sca{h}"])
                if h % 4 == 3:
                    yield
            for h in range(PH):
                P.op("dve", (lambda h: lambda e: e.max_index(out=pos[:, h, 0:8], in_max=sc[:, h, 0:8], in_values=cand[:, h, :]))(h),
                     ["cand", f"sca{h}"], [f"posa{h}"])
                if h % 4 == 3:
                    yield
            for h in range(PH):
                P.op("dve", (lambda h: lambda e: e.match_replace(out=cand2[:, h, :], in_to_replace=sc[:, h, 0:8],
                                                                 in_values=cand[:, h, :], imm_value=NEG))(h),
                     ["cand", f"sca{h}"], [f"c2_{h}"])
                if h % 4 == 3:
                    yield
            for h in range(PH):
                P.op("dve", (lambda h: lambda e: e.max(out=sc[:, h, 8:16], in_=cand2[:, h, :]))(h), [f"c2_{h}"], [f"scb{h}"])
                if h % 4 == 3:
                    yield
            for h in range(PH):
                P.op("dve", (lambda h: lambda e: e.max_index(out=pos[:, h, 8:16], in_max=sc[:, h, 8:16], in_values=cand2[:, h, :]))(h),
                     [f"c2_{h}", f"scb{h}"], [f"posb{h}"])
                if h % 4 == 3:
                    yield
            scall = [f"sca{h}" for h in range(PH)] + [f"scb{h}" for h in range(PH)]
            posall = [f"posa{h}" for h in range(PH)] + [f"posb{h}" for h in range(PH)]
            cp(P, "dve", posf[:], pos[:], posall, ["posf"])
            tt(P, "dve", ge, posf[:].unsqueeze(3).to_broadcast([128, PH, 16, 17]),
               thr17[:].unsqueeze(1).unsqueeze(1).to_broadcast([128, PH, 16, 17]), ALU.is_ge, ["posf", "thr17", "thr17b"], ["ge"])
            yield
            tt(P, "dve", oh, ge[:, :, :, 0:16], ge[:, :, :, 1:17], ALU.subtract, ["ge"], ["oh"])
            P.op("dve", lambda e: e.tensor_reduce(out=af[:], in_=ge[:, :, :, 1:17], axis=AX.X, op=ALU.add), ["ge"], ["af"])
            yield
            tt(P, "dve", oh, oh, i16fv[:, :, 0, :].unsqueeze(2).to_broadcast(B4), ALU.mult, ["oh", "i16f"], ["oh"])
            P.op("dve", lambda e: e.tensor_reduce(out=i1s[:], in_=oh, axis=AX.X, op=ALU.add), ["oh"], ["i1s"])
            ts(P, "dve", bf_[:], af[:], -16.0, None, ALU.mult, None, ["af"], ["bf"])
            tt(P, "dve", bf_[:], bf_[:], posf[:], ALU.add, ["bf", "posf"], ["bf"])
            yield
            tt(P, "dve", oh, iota16[:].unsqueeze(1).unsqueeze(1).to_broadcast(B4), bf_[:].unsqueeze(3).to_broadcast(B4),
               ALU.is_equal, ["iota16", "bf", "i1s"], ["oh"])
            tt(P, "dve", oh, oh, i16fv[:, :, 1, :].unsqueeze(2).to_broadcast(B4), ALU.mult, ["oh", "i16f"], ["oh"])
            P.op("dve", lambda e: e.tensor_reduce(out=i2s[:], in_=oh, axis=AX.X, op=ALU.add), ["oh"], ["i2s"])
            yield
            ts(P, "dve", i1s[:], i1s[:], float(NK), None, ALU.mult, None, ["i1s"], ["i1s"])
            tt(P, "dve", i1s[:], i1s[:], i2s[:], ALU.add, ["i1s", "i2s"], ["i1s"])
            cp(P, "dve", eidx[par][:], i1s[:].rearrange("p h k -> p (h k)"), ["i1s"], [EI])
            tt(P, "dve", ee[:], sc[:], sc[:, :, 0:1].to_broadcast([128, PH, 16]), ALU.subtract, scall, ["ee"])
            yield
            act(P, ee[:], ee[:], AF.Exp, ["ee"], ["ee"])
            yield
            P.op("dve", lambda e: e.tensor_reduce(out=zz[:], in_=ee[:], axis=AX.X, op=ALU.add), ["ee"], ["zz"])
            P.op("dve", lambda e: e.reciprocal(out=zz[:], in_=zz[:]), ["zz"], ["zz"])
            tt(P, "dve", gg[par][:].rearrange("p (h k) -> p h k", k=16), ee[:], zz[:].unsqueeze(2).to_broadcast([128, PH, 16]),
               ALU.mult, ["ee", "zz"], [GG])
            yield

        def advance(gen, n):
            if gen is None:
                return None
            for _ in range(n):
                try:
                    next(gen)
                except StopIteration:
                    return None
            return gen

        gen = prologue(0)
        while gen is not None:
            gen = advance(gen, 1)
        br, pr = Rot(NB_G), Rot(4)
        NGRP = NSL // G
        gbuf = {}

        def names(t):
            par = t % 2
            return par, f"x1t{t % 3}", f"hn2b{par}", f"eidx{par}", f"gg{par}"

        def stage_a(t, gi):
            par, X, HB, EI, GG = names(t)
            s0 = gi * G
            bi = br.next()
            gbuf[(t, gi)] = bi
            for j in range(G):
                s = s0 + j
                P.dma("pool", (lambda bi, j, s, par: lambda e: e.indirect_dma_start(
                    out=uvg[bi][:, j, :, :].rearrange("p s d -> p (s d)"), out_offset=None, in_=uv_v,
                    in_offset=bass.IndirectOffsetOnAxis(ap=eidx[par][:, s:s + 1], axis=0)))(bi, j, s, par),
                    [EI], [f"uvg{bi}_{j}"])
            for j in range(G):
                s = s0 + j
                pi = pr.next()
                tt(P, "dve", prodb[pi][:], uvg[bi][:, j, 0, :], hn2b[par][:], ALU.mult, [f"uvg{bi}_{j}", HB], [f"prodb{pi}"])
                if s % 2 == 1:
                    for cc in range(8):
                        mm(P, fold_ps[:, 0:128], identb[:], prodb[pi][:, cc * 128:(cc + 1) * 128], cc == 0, cc == 7,
                           ["identb", f"prodb{pi}"], ["fold"])
                    act(P, junks[:], fold_ps[:, 0:128], AF.Identity, ["fold"], ["junks", f"adot{s}"], accum_out=adot[:, s:s + 1])
                else:
                    act(P, junkb[:], prodb[pi][:], AF.Identity, [f"prodb{pi}"], ["junkb", f"adot{s}"], accum_out=adot[:, s:s + 1])

        def stage_b(t, gi):
            s0 = gi * G
            adg = [f"adot{s0 + j}" for j in range(G)]
            act(P, ga[:, s0:s0 + G], adot[:, s0:s0 + G], AF.Gelu, adg, [f"ga{s0}"])

        def stage_c(t, gi):
            par, X, HB, EI, GG = names(t)
            s0 = gi * G
            tt(P, "dve", cw[:, s0:s0 + G], ga[:, s0:s0 + G], gg[par][:, s0:s0 + G], ALU.mult, [f"ga{s0}", GG], [f"cw{s0}"])

        def stage_d(t, gi):
            par, X, HB, EI, GG = names(t)
            yp = y_ps[par]
            s0 = gi * G
            bi = gbuf.pop((t, gi))
            for j in range(G):
                s = s0 + j
                di = s % 8
                if s % 2 == 0:
                    act(P, dgc[di][:], identb[:], AF.Copy, ["identb", f"cw{s0}"], [f"dgc{di}"], scale=cw[:, s:s + 1])
                else:
                    ts(P, "dve", dgc[di][:], identb[:], cw[:, s:s + 1], None, ALU.mult, None, ["identb", f"cw{s0}"], [f"dgc{di}"])
            for j in range(G):
                s = s0 + j
                di = s % 8
                for half in range(2):
                    mm(P, yp[half][:], dgc[di][:], uvg[bi][:, j, 1, half * 512:(half + 1) * 512], s == 0, s == NSL - 1,
                       [f"dgc{di}", f"uvg{bi}_{j}"], [f"y{par}{half}"])

        def finalize(t):
            par, X, HB, EI, GG = names(t)
            xp = t % 3
            yp = y_ps[par]
            for half in range(2):
                hsl = slice(half * 512, (half + 1) * 512)
                tt(P, "dve", x2[:, hsl], x1t[xp][:, hsl], yp[half][:], ALU.add, [X, f"y{par}{half}"], [f"x2_{half}"])
            yield
            act(P, junkb[:], x2[:], AF.Square, ["x2_0", "x2_1"], ["junkb", "fss"], accum_out=fss[:])
            yield
            act(P, fstd[:], fss[:], AF.Sqrt, ["fss", "epsc"], ["fstd"], bias=epsc[:], scale=1.0 / D)
            yield
            P.op("dve", lambda e: e.reciprocal(out=frstd[:], in_=fstd[:]), ["fstd"], ["frstd"])
            yield
            act(P, ot[:], x2[:], AF.Copy, ["x2_0", "x2_1", "frstd"], ["ot"], scale=frstd[:])
            yield
            tt(P, "dve", ot[:], ot[:], gfrep[:], ALU.mult, ["ot", "gfrep"], ["ot"])
            yield
            dma(P, "sp", out_v[t], ot[:], ["ot"], ["out"])
            yield

        units = [(t, gi) for t in range(NT) for gi in range(NGRP)]
        NU = len(units)
        gen = None
        fgen = None
        for n in range(NU + 3):
            if n < NU:
                t, gi = units[n]
                if gi == 0 and gen is not None:
                    while gen is not None:
                        gen = advance(gen, 1)
                stage_a(t, gi)
            if 1 <= n <= NU:
                stage_b(*units[n - 1])
            if 2 <= n <= NU + 1:
                stage_c(*units[n - 2])
            if n >= 3:
                tc_, gc_ = units[n - 3]
                stage_d(tc_, gc_)
                if gc_ == NGRP - 1:
                    while fgen is not None:
                        fgen = advance(fgen, 1)
                    fgen = finalize(tc_)
            fgen = advance(fgen, 1)
            if n < NU:
                t, gi = units[n]
                if gi == 3 and t + 1 < NT:
                    gen = prologue(t + 1)
                gen = advance(gen, 1)
                if gi == NGRP - 1:
                    while gen is not None:
                        gen = advance(gen, 1)
        while fgen is not None:
            fgen = advance(fgen, 1)
        P.barrier()
        P.emit()


def build_all(nc, P, c, upto="d"):
    phase_a(nc, P, c)
    if upto < "b":
        return
    with ExitStack() as wsc:
        wts = alloc_c_weights(nc, wsc)
        phase_b(nc, P, c, preload=lambda: load_c_weights(nc, P, c, wts, None))
        if upto >= "c":
            phase_c(nc, P, c, wts)
    if upto >= "d":
        phase_d(nc, P, c)


def build_program(S):
    nc = bass.Bass("TRN2", target_bir_lowering=False)
    c = declare_io(nc, S, debug=False)
    with ExitStack() as st:
        P = Prog(nc, st)
        build_all(nc, P, c)
    return nc


def kernel(**inputs):
    x = np.asarray(inputs["x"], dtype=np.float32)
    B, S, _ = x.shape
    assert B == 8
    nc = build_program(S)
    consts = make_consts(S)
    w = {k: np.asarray(v) for k, v in inputs.items() if k != "x"}
    shared = host_inputs(S, x[0], w, consts)
    in_maps = []
    for b in range(B):
        m = dict(shared)
        m["x"] = np.ascontiguousarray(x[b])
        in_maps.append(m)
    res = run_bass_kernel_spmd(nc, in_maps, core_ids=list(range(B)))
    out = np.stack([np.asarray(res.results[b]["out"], dtype=np.float32) for b in range(B)], axis=0)
    return out
```
